# Optimizing a Trainium2 kernel written in Bass

```python
import math
import jax, jax.numpy as jnp
from jax import lax
import numpy as np

D_MODEL = 2048
BATCH = 4
SEQ = 2048
DEPTH = 2

GRID_W = 64
CTX_LEN = 256
MLA_HEADS = 8
MLA_Q_RANK = 768
MLA_KV_RANK = 512
MLA_NOPE = 128
MLA_ROPE = 64
MLA_V = 128
DIFF_HEADS = 4
DIFF_HD = 64
DIFF_V = 2 * DIFF_HD
POOL_WINDOWS = (2, 4, 8, 16)
POOL_GROUP = 128
POOL_W = len(POOL_WINDOWS) * POOL_GROUP
N_BRANCH = 3
SPLIT_WIDTHS = (MLA_KV_RANK, MLA_ROPE, DIFF_HEADS * 2 * DIFF_HD, DIFF_HEADS * DIFF_V,
                MLA_Q_RANK, DIFF_HEADS * 2 * DIFF_HD, POOL_W, N_BRANCH * D_MODEL)
KV_COLS = sum(SPLIT_WIDTHS[:4])
IN_COLS = sum(SPLIT_WIDTHS)
N_GROUPS = 8
EXPERTS_PER_GROUP = 8
N_EXPERTS = N_GROUPS * EXPERTS_PER_GROUP
TOP_K = 2
D_EXPERT = 512
EXPERT_BLOCK = 128
Q_BLOCK = 128
ROPE_BASE = 10000.0
EPS = 1e-6
DN_ALPHA = (2 * DEPTH) ** 0.25
DN_BETA = (8 * DEPTH) ** -0.25

kernel_name = "hybrid_mla_diff_pool_hmoe_dit"


def split_cols(p, widths):
    offs = np.cumsum(widths)[:-1].tolist()
    return jnp.split(p, offs, axis=-1)


def layer_norm(x):
    xf = x.astype(jnp.float32)
    mu = jnp.mean(xf, axis=-1, keepdims=True)
    var = jnp.mean(jnp.square(xf - mu), axis=-1, keepdims=True)
    return ((xf - mu) * lax.rsqrt(var + EPS)).astype(x.dtype)


def rms_norm(x, g):
    xf = x.astype(jnp.float32)
    return (xf * lax.rsqrt(jnp.mean(xf * xf, axis=-1, keepdims=True) + EPS)).astype(x.dtype) * g


def modulate(x, shift, scale):
    return layer_norm(x) * (1.0 + scale) + shift


def deepnorm_residual(x, y, g, b):
    return layer_norm(DN_ALPHA * x + y) * g + b


def axial_rope_table(n_rows, rot_dim):
    quarter = rot_dim // 4
    inv_freq = ROPE_BASE ** (-jnp.arange(quarter, dtype=jnp.float32) / quarter)
    row = jnp.repeat(jnp.arange(n_rows, dtype=jnp.float32), GRID_W)
    col = (jnp.arange(n_rows * GRID_W) % GRID_W).astype(jnp.float32)
    ang = jnp.stack([row[:, None] * inv_freq, col[:, None] * inv_freq], axis=1)
    return jnp.cos(ang), jnp.sin(ang)


def apply_rope(x, cos, sin):
    r, s = x.shape[-1], x.shape[1]
    bshape = (1, s) + (1,) * (x.ndim - 3) + (2, r // 4)
    c, sn = cos.reshape(bshape), sin.reshape(bshape)
    xf = x.astype(jnp.float32).reshape(x.shape[:-1] + (2, 2, r // 4))
    x1, x2 = xf[..., 0, :], xf[..., 1, :]
    out = jnp.stack([x1 * c - x2 * sn, x2 * c + x1 * sn], axis=-2)
    return out.reshape(x.shape).astype(x.dtype)


def multi_map_attention(q, k, v, coef):
    b, sq, h, m, dk = q.shape
    nb = sq // Q_BLOCK
    scale = dk ** -0.5
    qb = jnp.moveaxis(q.reshape(b, nb, Q_BLOCK, h, m, dk), 1, 0)

    def one_block(qblk):
        s = jnp.einsum('bqhmd,bkhmd->bhmqk', qblk, k).astype(jnp.float32) * scale
        w = jnp.einsum('bhmqk,m->bhqk', jax.nn.softmax(s, axis=-1), coef)
        return jnp.einsum('bhqk,bkhd->bqhd', w.astype(v.dtype), v)

    out = lax.map(one_block, qb)
    return jnp.moveaxis(out, 0, 1).reshape(b, sq, h, v.shape[-1])


def pool_mixer(u, w_pool, pool_scale):
    b, s, _ = u.shape
    ug = u.reshape(b, s, len(POOL_WINDOWS), POOL_GROUP)
    t = jnp.arange(s)
    outs = []
    for gi, w in enumerate(POOL_WINDOWS):
        half = w // 2
        xg = ug[:, :, gi, :].astype(jnp.float32)
        xp = jnp.pad(xg, ((0, 0), (half, half), (0, 0)))
        cs = jnp.concatenate([jnp.zeros((b, 1, POOL_GROUP), jnp.float32), jnp.cumsum(xp, axis=1)], axis=1)
        win = cs[:, w:w + s] - cs[:, :s]
        cnt = (jnp.minimum(t + half, s) - jnp.maximum(t - half, 0)).astype(jnp.float32)
        outs.append(win / cnt[None, :, None] - xg)
    pooled = jnp.stack(outs, axis=2).astype(u.dtype)
    y = jnp.einsum('bsgc,gcd->bsgd', pooled, w_pool)
    return y.reshape(b, s, POOL_W) * pool_scale


def token_mixer(u_lat, u_ctx, rope_mla, rope_diff, lam_init, ctx_out,
                w_in, b_gate, g_q, g_kv, w_uq, w_ukv, lam, g_sub, w_pool, pool_scale,
                w_br_mla, w_br_diff, w_br_pool, w_o):
    lam_val = (jnp.exp(jnp.sum(lam[0] * lam[1])) - jnp.exp(jnp.sum(lam[2] * lam[3]))).astype(jnp.float32) + lam_init
    coef_mla = jnp.ones((1,), jnp.float32)
    coef_diff = jnp.stack([jnp.ones((), jnp.float32), -lam_val])

    def keys_values(p, use_rope):
        b, s = p.shape[:2]
        c_kv, k_rot, k_diff, v_diff = split_cols(p[..., :KV_COLS], SPLIT_WIDTHS[:4])
        kv = (rms_norm(c_kv, g_kv) @ w_ukv).reshape(b, s, MLA_HEADS, MLA_NOPE + MLA_V)
        k_rot = k_rot.reshape(b, s, 1, MLA_ROPE)
        k_diff = k_diff.reshape(b, s, DIFF_HEADS, 2, DIFF_HD)
        if use_rope:
            k_rot = apply_rope(k_rot, *rope_mla)
            k_diff = apply_rope(k_diff, *rope_diff)
        k_mla = jnp.concatenate([kv[..., :MLA_NOPE], jnp.broadcast_to(k_rot, (b, s, MLA_HEADS, MLA_ROPE))], axis=-1)
        return (k_mla[:, :, :, None, :], kv[..., MLA_NOPE:], k_diff, v_diff.reshape(b, s, DIFF_HEADS, DIFF_V))

    def stream_out(p, use_rope, k_mla, v_mla, k_diff, v_diff):
        b, s = p.shape[:2]
        c_q, q_diff, pool_in, gate_in = split_cols(p[..., KV_COLS:], SPLIT_WIDTHS[4:])
        q = (rms_norm(c_q, g_q) @ w_uq).reshape(b, s, MLA_HEADS, MLA_NOPE + MLA_ROPE)
        q_nope, q_rot = q[..., :MLA_NOPE], q[..., MLA_NOPE:]
        q_diff = q_diff.reshape(b, s, DIFF_HEADS, 2, DIFF_HD)
        if use_rope:
            q_rot = apply_rope(q_rot, *rope_mla)
            q_diff = apply_rope(q_diff, *rope_diff)
        q_mla = jnp.concatenate([q_nope, q_rot], axis=-1)[:, :, :, None, :]
        o_mla = multi_map_attention(q_mla, k_mla, v_mla, coef_mla).reshape(b, s, MLA_HEADS * MLA_V)
        o_diff = multi_map_attention(q_diff, k_diff, v_diff, coef_diff)
        o_diff = (rms_norm(o_diff, g_sub) * (1.0 - lam_init)).reshape(b, s, DIFF_HEADS * DIFF_V)
        o_pool = pool_mixer(pool_in, w_pool, pool_scale)
        gates = jax.nn.sigmoid(gate_in.reshape(b, s, N_BRANCH, D_MODEL) + b_gate)
        merged = (gates[:, :, 0] * (o_mla @ w_br_mla)
                  + gates[:, :, 1] * (o_diff @ w_br_diff)
                  + gates[:, :, 2] * (o_pool @ w_br_pool))
        return merged @ w_o

    p_lat = u_lat @ w_in
    p_ctx = u_ctx @ (w_in if ctx_out else w_in[:, :KV_COLS])
    kv_lat = keys_values(p_lat, True)
    kv_ctx = keys_values(p_ctx, False)
    kv_all = tuple(jnp.concatenate([kc, kl], axis=1) for kc, kl in zip(kv_ctx, kv_lat))
    out_lat = stream_out(p_lat, True, *kv_all)
    out_ctx = stream_out(p_ctx, False, *kv_ctx) if ctx_out else None
    return out_lat, out_ctx


def expert_ffn(h, expert_idx, weights, w_gu, w_dn):
    n_tok, d = h.shape
    n_assign = n_tok * TOP_K
    flat_e = expert_idx.reshape(n_assign)
    order = jnp.argsort(flat_e)
    sorted_e = flat_e[order]
    sorted_tok = (order // TOP_K).astype(jnp.int32)
    sorted_w = weights.reshape(n_assign)[order]
    counts = jnp.bincount(flat_e, length=N_EXPERTS)
    padded = (counts + EXPERT_BLOCK - 1) // EXPERT_BLOCK * EXPERT_BLOCK
    pad_end = jnp.cumsum(padded)
    pad_start = pad_end - padded
    start = jnp.cumsum(counts) - counts
    dest = pad_start[sorted_e] + jnp.arange(n_assign) - start[sorted_e]
    n_blocks = -(-(n_assign + N_EXPERTS * (EXPERT_BLOCK - 1)) // EXPERT_BLOCK)
    n_rows = n_blocks * EXPERT_BLOCK
    slot_tok = jnp.full((n_rows,), n_tok, jnp.int32).at[dest].set(sorted_tok)
    slot_w = jnp.zeros((n_rows,), jnp.float32).at[dest].set(sorted_w)
    block_exp = jnp.minimum(jnp.searchsorted(pad_end, jnp.arange(n_blocks) * EXPERT_BLOCK, side='right'),
                            N_EXPERTS - 1)
    h_pad = jnp.concatenate([h, jnp.zeros((1, d), h.dtype)], axis=0)
    xb = h_pad[slot_tok].reshape(n_blocks, EXPERT_BLOCK, d)

    def run_block(args):
        xblk, e = args
        gate, up = jnp.split(xblk @ w_gu[e], 2, axis=-1)
        return (jax.nn.silu(gate) * up) @ w_dn[e]

    yb = lax.map(run_block, (xb, block_exp))
    y = yb.reshape(n_rows, d) * slot_w[:, None].astype(h.dtype)
    return jnp.zeros((n_tok + 1, d), h.dtype).at[slot_tok].add(y)[:n_tok]


def hierarchical_moe(h, w_grp, b_grp, w_exp, b_exp, w_gu, w_dn):
    n_tok = h.shape[0]
    grp_prob = jax.nn.softmax((h @ w_grp + b_grp).astype(jnp.float32), axis=-1)
    grp_p, grp_idx = lax.top_k(grp_prob, 1)
    exp_logits = (h @ w_exp + b_exp).astype(jnp.float32).reshape(n_tok, N_GROUPS, EXPERTS_PER_GROUP)
    in_grp = exp_logits[jnp.arange(n_tok), grp_idx[:, 0]]
    top_val, top_loc = lax.top_k(in_grp, TOP_K)
    weights = jax.nn.softmax(top_val, axis=-1) * grp_p
    expert_idx = grp_idx * EXPERTS_PER_GROUP + top_loc
    return expert_ffn(h, expert_idx, weights, w_gu, w_dn)


def setup_inputs(seed: int = 0) -> dict:
    key = jax.random.key(seed)
    ks = iter(jax.random.split(key, 40))
    D, L = D_MODEL, DEPTH

    def nrm(shape, scale):
        return jax.random.normal(next(ks), shape, jnp.float32) * scale

    return {
        "x": nrm((BATCH, SEQ, D), 1.0),
        "c": nrm((BATCH, D), 1.0),
        "ctx": nrm((BATCH, CTX_LEN, D), 1.0),
        "c_ctx": nrm((D,), 1.0),
        "w_ada": nrm((L, D, 6 * D), 0.5 * D ** -0.5),
        "b_ada": nrm((L, 6 * D), 0.01),
        "w_in": nrm((L, D, IN_COLS), D ** -0.5),
        "b_gate": nrm((L, N_BRANCH, D), 0.01),
        "g_q": 1.0 + nrm((L, MLA_Q_RANK), 0.01),
        "g_kv": 1.0 + nrm((L, MLA_KV_RANK), 0.01),
        "w_uq": nrm((L, MLA_Q_RANK, MLA_HEADS * (MLA_NOPE + MLA_ROPE)), MLA_Q_RANK ** -0.5),
        "w_ukv": nrm((L, MLA_KV_RANK, MLA_HEADS * (MLA_NOPE + MLA_V)), MLA_KV_RANK ** -0.5),
        "lam": nrm((L, 4, DIFF_HD), 0.1),
        "g_sub": 1.0 + nrm((L, DIFF_V), 0.01),
        "w_pool": nrm((L, len(POOL_WINDOWS), POOL_GROUP, POOL_GROUP), POOL_GROUP ** -0.5),
        "pool_scale": 1.0 + nrm((L, POOL_W), 0.01),
        "w_br_mla": nrm((L, MLA_HEADS * MLA_V, D), (MLA_HEADS * MLA_V) ** -0.5),
        "w_br_diff": nrm((L, DIFF_HEADS * DIFF_V, D), (DIFF_HEADS * DIFF_V) ** -0.5),
        "w_br_pool": nrm((L, POOL_W, D), POOL_W ** -0.5),
        "w_o": nrm((L, D, D), DN_BETA * D ** -0.5),
        "ln1_g": 1.0 + nrm((L, D), 0.01),
        "ln1_b": nrm((L, D), 0.01),
        "w_grp": nrm((L, D, N_GROUPS), D ** -0.5),
        "b_grp": nrm((L, N_GROUPS), 0.01),
        "w_exp": nrm((L, D, N_EXPERTS), D ** -0.5),
        "b_exp": nrm((L, N_EXPERTS), 0.01),
        "w_gu": nrm((L, N_EXPERTS, D, 2 * D_EXPERT), D ** -0.5),
        "w_dn": nrm((L, N_EXPERTS, D_EXPERT, D), DN_BETA * D_EXPERT ** -0.5),
        "ln2_g": 1.0 + nrm((L, D), 0.01),
        "ln2_b": nrm((L, D), 0.01),
    }


def reference(x, c, ctx, c_ctx, w_ada, b_ada, w_in, b_gate, g_q, g_kv, w_uq, w_ukv, lam, g_sub,
              w_pool, pool_scale, w_br_mla, w_br_diff, w_br_pool, w_o, ln1_g, ln1_b,
              w_grp, b_grp, w_exp, b_exp, w_gu, w_dn, ln2_g, ln2_b):
    n_lat = x.shape[1]
    rows = n_lat // GRID_W
    rope_mla = axial_rope_table(rows, MLA_ROPE)
    rope_diff = axial_rope_table(rows, DIFF_HD)
    x_lat, x_ctx = x, ctx
    for l in range(DEPTH):
        last = l == DEPTH - 1
        lam_init = 0.8 - 0.6 * math.exp(-0.3 * l)
        sh1, sc1, g1, sh2, sc2, g2 = jnp.split(jax.nn.silu(c) @ w_ada[l] + b_ada[l], 6, axis=-1)
        csh1, csc1, cg1, csh2, csc2, cg2 = jnp.split(jax.nn.silu(c_ctx) @ w_ada[l] + b_ada[l], 6, axis=-1)

        u_lat = modulate(x_lat, sh1[:, None], sc1[:, None])
        u_ctx = modulate(x_ctx, csh1, csc1)
        m_lat, m_ctx = token_mixer(u_lat, u_ctx, rope_mla, rope_diff, lam_init, not last,
                                   w_in[l], b_gate[l], g_q[l], g_kv[l], w_uq[l], w_ukv[l], lam[l], g_sub[l],
                                   w_pool[l], pool_scale[l], w_br_mla[l], w_br_diff[l], w_br_pool[l], w_o[l])
        x_lat = deepnorm_residual(x_lat, g1[:, None] * m_lat, ln1_g[l], ln1_b[l])
        if not last:
            x_ctx = deepnorm_residual(x_ctx, cg1 * m_ctx, ln1_g[l], ln1_b[l])

        b, s, d = x_lat.shape
        v_lat = modulate(x_lat, sh2[:, None], sc2[:, None]).reshape(b * s, d)
        if last:
            y_lat = hierarchical_moe(v_lat, w_grp[l], b_grp[l], w_exp[l], b_exp[l], w_gu[l], w_dn[l])
        else:
            v_ctx = modulate(x_ctx, csh2, csc2).reshape(-1, d)
            y = hierarchical_moe(jnp.concatenate([v_lat, v_ctx], axis=0),
                                 w_grp[l], b_grp[l], w_exp[l], b_exp[l], w_gu[l], w_dn[l])
            y_lat = y[:b * s]
            x_ctx = deepnorm_residual(x_ctx, cg2 * y[b * s:].reshape(x_ctx.shape), ln2_g[l], ln2_b[l])
        x_lat = deepnorm_residual(x_lat, g2[:, None] * y_lat.reshape(b, s, d), ln2_g[l], ln2_b[l])
    return x_lat
```

```python
import math
import numpy as np
import ml_dtypes
import concourse.bass as bass
import concourse.mybir as mybir
from concourse.bass_utils import run_bass_kernel_spmd

F32 = mybir.dt.float32
BF16 = mybir.dt.bfloat16
I32 = mybir.dt.int32
ALU = mybir.AluOpType
AF = mybir.ActivationFunctionType
AX = mybir.AxisListType

D = 2048
DC = D // 128
DEPTH = 2
GRID_W = 64
MLA_HEADS = 8
Q_RANK = 768
KV_RANK = 512
NOPE = 128
ROPE = 64
MLA_V = 128
DIFF_HEADS = 4
DIFF_HD = 64
DIFF_V = 128
POOL_WINDOWS = (2, 4, 8, 16)
IN_COLS = 9536
N_GROUPS = 8
EPG = 8
N_EXPERTS = 64
D_EXPERT = 512
EPS = 1e-6
DN_ALPHA = (2 * DEPTH) ** 0.25
C_CKV, C_KROT, C_KDIFF, C_VDIFF, C_CQ, C_QDIFF, C_POOL, C_GATE = 0, 512, 576, 1088, 1600, 2368, 2880, 3392
BIG = 1.0e30


class Buf:
    __slots__ = ("w", "r", "name")

    def __init__(self, name=""):
        self.w = None
        self.r = {}
        self.name = name


class Sched:
    SEM_LIMIT = 30000
    ENGS = ("pe", "act", "dve", "pool", "sp")

    def __init__(self, nc, es):
        self.nc = nc
        self.es = es
        self.prog = {k: [] for k in self.ENGS}
        self.sem = {}
        self.cnt = {}
        self.waited = {k: {} for k in self.ENGS}
        self.last = {}
        self.init = {}
        self.pe_sems = set()
        self.nsem = 0
        for k in self.ENGS:
            self._new_sem(k)
        self.dq = {}
        for q in ("sp", "pool", "act"):
            sems = [self._alloc(f"dq_{q}_{i}") for i in range(6)]
            self.dq[q] = {"sems": sems, "cnt": [0] * len(sems), "i": 0}
        self.ninst = 0

    def _alloc(self, name):
        self.nsem += 1
        return self.es.enter_context(self.nc.semaphore(f"{name}_{self.nsem}"))

    def _new_sem(self, k):
        self.sem[k] = self._alloc(f"s_{k}")
        self.cnt[k] = 0
        if k == "pe":
            self.pe_sems.add(id(self.sem[k]))

    def _wait(self, k, tok):
        sem, val = tok
        sid = id(sem)
        if k == "pe" and sid in self.pe_sems:
            return
        w = self.waited[k]
        if w.get(sid, 0) >= val:
            return
        self.prog[k].append(("w", sem, val))
        w[sid] = val

    def _deps(self, k, reads, writes):
        for b in reads:
            if b.w is not None:
                self._wait(k, b.w)
        for b in writes:
            if b.w is not None:
                self._wait(k, b.w)
            for tok in b.r.values():
                self._wait(k, tok)

    def _commit(self, tok, reads, writes):
        sid = id(tok[0])
        for b in reads:
            b.r[sid] = tok
        for b in writes:
            b.w = tok
            b.r = {}

    def op(self, k, fn, reads=(), writes=(), sig=True):
        self._deps(k, reads, writes)
        if not sig:
            assert k == "pe"
            self.prog[k].append(("n", fn))
            tok = (self.sem[k], self.cnt[k] + 1)
            self._commit(tok, reads, writes)
            self.ninst += 1
            self.unsig = True
            return tok
        pending = k == "pe" and getattr(self, "unsig", False)
        if k == "pe":
            self.unsig = False
        if self.cnt[k] >= self.SEM_LIMIT and not pending:
            self._new_sem(k)
        self.cnt[k] += 1
        self.prog[k].append(("i", fn, self.sem[k], 1))
        tok = (self.sem[k], self.cnt[k])
        self.last[k] = tok
        self._commit(tok, reads, writes)
        self.ninst += 1
        return tok

    def dma(self, q, fn, reads=(), writes=()):
        self._deps(q, reads, writes)
        dq = self.dq[q]
        i = dq["i"]
        dq["i"] = (i + 1) % len(dq["sems"])
        sem = dq["sems"][i]
        if dq["cnt"][i] > 0:
            self._wait(q, (sem, dq["cnt"][i]))
        if dq["cnt"][i] >= self.SEM_LIMIT:
            sem = self._alloc(f"dq_{q}_{i}")
            dq["sems"][i] = sem
            dq["cnt"][i] = 0
        dq["cnt"][i] += 16
        self.prog[q].append(("i", fn, sem, 16))
        tok = (sem, dq["cnt"][i])
        self._commit(tok, reads, writes)
        self.ninst += 1
        return tok

    def finish(self, bufs):
        for b in bufs:
            if b.w is not None:
                self._wait("sp", b.w)

    def emit(self):
        def replay(k):
            def body(eng):
                for f in self.init.get(k, ()):
                    f(eng)
                for ent in self.prog[k]:
                    if ent[0] == "w":
                        eng.wait_ge(ent[1], ent[2])
                    elif ent[0] == "n":
                        ent[1](eng)
                    else:
                        ent[1](eng).then_inc(ent[2], ent[3])
            return body

        with self.nc.Block() as block:
            block.tensor(replay("pe"))
            block.scalar(replay("act"))
            block.vector(replay("dve"))
            block.gpsimd(replay("pool"))
            block.sync(replay("sp"))


class Arena:
    def __init__(self, tile, n):
        self.t = tile
        self.n = n
        self.off = 0

    def reset(self):
        self.off = 0

    def alloc(self, shape_free, name=""):
        n = int(np.prod(shape_free))
        n = (n + 15) // 16 * 16
        assert self.off + n <= self.n, f"arena overflow {name}: {self.off}+{n}>{self.n}"
        ap = self.t[:, self.off:self.off + int(np.prod(shape_free))]
        self.off += n
        if len(shape_free) == 2:
            ap = ap.rearrange("p (a b) -> p a b", b=shape_free[1])
        elif len(shape_free) == 3:
            ap = ap.rearrange("p (a b c) -> p a b c", b=shape_free[1], c=shape_free[2])
        return ap, Buf(name)


def token_blocks(t_ctx, t_lat, bw=512):
    blks = []
    for s0, n in ((0, t_ctx), (t_ctx, t_lat)):
        o = 0
        while o < n:
            w = min(bw, n - o)
            blks.append((s0 + o, w))
            o += w
    return blks


def build_program(cfg):
    T_CTX, T_LAT, NL = cfg["T_CTX"], cfg["T_LAT"], cfg["NL"]
    NG = cfg.get("NG", N_GROUPS)
    NE = NG * EPG
    CAP = cfg.get("CAP", 128)
    stop = cfg.get("stop", "end")
    T = T_CTX + T_LAT
    NT = T // 128
    NTC = T_CTX // 128
    BLKS = token_blocks(T_CTX, T_LAT)
    import contextlib
    es = contextlib.ExitStack()
    nc = bass.Bass("TRN2", target_bir_lowering=False)

    def din(name, shape, dt=F32):
        return nc.dram_tensor(name, list(shape), dt, kind="ExternalInput").ap()

    I = {}
    I["x"] = din("x", [T_LAT, D])
    I["ctx"] = din("ctx", [T_CTX, D])
    I["cvec"] = din("cvec", [2, D])
    L = DEPTH
    for nm, shp in (("w_ada", [L, D, 6 * D]), ("b_ada", [L, 6 * D]), ("w_in", [L, D, IN_COLS]), ("b_gate", [L, 3 * D]),
                    ("g_q", [L, Q_RANK]), ("g_kv", [L, KV_RANK]), ("w_uq", [L, Q_RANK, 1536]), ("w_ukv", [L, KV_RANK, 2048]),
                    ("lam", [L, 4 * DIFF_HD]), ("g_sub", [L, 128]), ("w_pool", [L, 4, 128, 128]), ("pool_scale", [L, 512]),
                    ("w_br_mla", [L, 1024, D]), ("w_br_diff", [L, 512, D]), ("w_br_pool", [L, 512, D]), ("w_o", [L, D, D]),
                    ("ln1_g", [L, D]), ("ln1_b", [L, D]), ("w_grp", [L, D, N_GROUPS]), ("b_grp", [L, N_GROUPS]),
                    ("w_exp", [L, D, N_EXPERTS]), ("b_exp", [L, N_EXPERTS]), ("w_gu", [L, NE, D, 1024]),
                    ("w_dn", [L, NE, 512, D]), ("ln2_g", [L, D]), ("ln2_b", [L, D])):
        I[nm] = din(nm, shp)
    I["ident"] = din("ident", [128, 128])
    I["ropc"] = din("ropc", [128, T])
    I["rops"] = din("rops", [128, T])
    I["rcnt"] = din("rcnt", [4, T])
    I["tril"] = din("tril", [128, 128])
    I["eoff"] = din("eoff", [128, NE])
    OUT = nc.dram_tensor("out", [T_LAT, D], F32, kind="ExternalOutput").ap()
    DBG = None
    if cfg.get("dbg"):
        DBG = nc.dram_tensor("dbg", list(cfg["dbg"]), F32, kind="ExternalOutput").ap()

    def dscr(name, shape, dt=BF16):
        return nc.dram_tensor(name, list(shape), dt).ap()

    XS = dscr("XS", [T, D], F32)
    GROW = dscr("GROW", [DEPTH, 2, 2, D], F32)
    KN = dscr("KN", [8, 128, T])
    KR2 = dscr("KR2", [128, T])
    CKG = dscr("CKG", [4, 128, T])
    CQG = dscr("CQG", [6, 128, T])
    VM = dscr("VM", [NT, 128, 8 * 129])
    QN = dscr("QN", [8, 128, T])
    QR = dscr("QR", [8, 64, T])
    KD = dscr("KD", [4, 128, T])
    QD = dscr("QD", [4, 128, T])
    VD = dscr("VD", [NT, 128, 4 * 129])
    OP = dscr("OP", [4, 128, T])
    GT = dscr("GT", [48, 128, T])
    OM = dscr("OM", [8, 128, T])
    OD = dscr("OD", [4, 128, T])
    MT = dscr("MT", [16, 128, T])
    XSORT = dscr("XSORT", [NE * CAP, D])
    YSORT = dscr("YSORT", [NE * CAP, D], F32)
    dbufs = {k: Buf(k) for k in ("XS", "GROW", "KN", "KR", "CKG", "CQG", "VM", "QN", "QR", "KD", "QD", "VD", "OP", "GT", "OM", "OD",
                                 "MT", "XSORT", "YSORT", "OUT", "DBG")}

    NBF = 58 * 1024
    NF32 = 14 * 1024 + 512
    abf_t = es.enter_context(nc.sbuf_tensor("abf", [128, NBF], BF16))
    af_t = es.enter_context(nc.sbuf_tensor("af32", [128, NF32], F32))
    ABF = Arena(abf_t, NBF)
    AF32 = Arena(af_t, NF32)
    ident = es.enter_context(nc.sbuf_tensor("identf", [128, 128], F32))
    identb = es.enter_context(nc.sbuf_tensor("identb", [128, 128], BF16))
    ones_f = es.enter_context(nc.sbuf_tensor("onesf", [128, 128], F32))
    ones_b = es.enter_context(nc.sbuf_tensor("onesb", [128, 128], BF16))
    tril = es.enter_context(nc.sbuf_tensor("trilf", [128, 128], F32))
    eoff = es.enter_context(nc.sbuf_tensor("eoff_sb", [128, NE], F32))
    zero_b = es.enter_context(nc.sbuf_tensor("zerob", [128, D], BF16))
    mod = es.enter_context(nc.sbuf_tensor("mod_sb", [128, DEPTH, 96, 2], F32))
    mod1 = es.enter_context(nc.sbuf_tensor("mod1", [128, DEPTH, 96, 2], F32))
    smallv = es.enter_context(nc.sbuf_tensor("smallv", [128, DEPTH, 64], F32))
    lamt = es.enter_context(nc.sbuf_tensor("lamt", [128, DEPTH, 8], F32))
    dest_i = es.enter_context(nc.sbuf_tensor("dest_i", [128, NT, 2], I32))
    gatew = es.enter_context(nc.sbuf_tensor("gatew", [128, NT, 2], F32))
    ebase = es.enter_context(nc.sbuf_tensor("ebase", [128, NE], F32))
    cb = {k: Buf(k) for k in ("ident", "identb", "ones", "tril", "eoff", "zero", "mod", "smallv", "lamt", "dest", "gatew", "ebase")}
    psum = [es.enter_context(nc.psum_tensor(f"ps{i}", [128, 512], F32)) for i in range(8)]
    pbuf = [Buf(f"ps{i}") for i in range(8)]
    S = Sched(nc, es)
    pctr = [0]

    def P(n=8, base=0):
        i = base + pctr[0] % n
        pctr[0] += 1
        return psum[i], pbuf[i]

    def barrier():
        toks = list(S.last.values())
        for q in S.dq.values():
            for sem, c in zip(q["sems"], q["cnt"]):
                if c > 0:
                    toks.append((sem, c))
        for k in S.ENGS:
            for t in toks:
                S._wait(k, t)

    def new_phase():
        barrier()
        ABF.reset()
        AF32.reset()

    S.dma("sp", lambda e: e.dma_start(out=ident[:], in_=I["ident"]), writes=[cb["ident"]])
    S.dma("sp", lambda e: e.dma_start(out=tril[:], in_=I["tril"]), writes=[cb["tril"]])
    S.dma("sp", lambda e: e.dma_start(out=eoff[:], in_=I["eoff"]), writes=[cb["eoff"]])
    S.op("dve", lambda e: e.tensor_copy(out=identb[:], in_=ident[:]), reads=[cb["ident"]], writes=[cb["identb"]])
    S.op("pool", lambda e: e.memset(ones_f[:], 1.0), writes=[cb["ones"]])
    S.op("pool", lambda e: e.memset(ones_b[:], 1.0), writes=[cb["ones"]])
    S.op("pool", lambda e: e.memset(zero_b[:], 0.0), writes=[cb["zero"]])

    def load_vec_fm(dst_ap, dst_buf, src2d, n):
        st, sb = AF32.alloc((128,), "vecst")
        S.dma("sp", lambda e: e.dma_start(out=st[0:n, :], in_=src2d), writes=[sb])
        ps, pb = P()
        S.op("pe", lambda e: e.transpose(ps[:, 0:n], st[0:n, :], ident[0:n, 0:n]), reads=[sb, cb["ident"]], writes=[pb])
        S.op("dve", lambda e: e.tensor_copy(out=dst_ap, in_=ps[:, 0:n]), reads=[pb], writes=[dst_buf])

    def phase_adaln():
        new_phase()
        cst, cstb = AF32.alloc((128,), "cst")
        sc, scb = AF32.alloc((32,), "sc")
        S.dma("sp", lambda e: e.dma_start(out=cst[0:32, :], in_=I["cvec"].rearrange("r (k p) -> (r k) p", p=128)), writes=[cstb])
        ps, pb = P()
        S.op("pe", lambda e: e.transpose(ps[:, 0:32], cst[0:32, :], ident[0:32, 0:32]), reads=[cstb, cb["ident"]], writes=[pb])
        S.op("act", lambda e: e.activation(out=sc, in_=ps[:, 0:32], func=AF.Silu), reads=[pb], writes=[scb])
        scv = sc.rearrange("p (r k) -> p k r", r=2)
        mark_ada = AF32.off
        for l in range(NL):
            barrier()
            AF32.off = mark_ada
            bfm, bfb = AF32.alloc((96,), "bada")
            load_vec_fm(bfm, bfb, I["b_ada"][l].rearrange("(n p) -> n p", p=128), 96)
            gsts = [AF32.alloc((256,), f"gst{i}") for i in range(2)]
            brs = [AF32.alloc((256,), f"br{i}") for i in range(2)]
            slabs = [AF32.alloc((16, 256), f"wada{i}") for i in range(2)]
            mps, mpb = psum[7], pbuf[7]
            for sl in range(6 * D // 256):
                w, wb = slabs[sl % 2]
                S.dma("sp", lambda e, w=w, sl=sl, l=l: e.dma_start(
                    out=w, in_=I["w_ada"][l][:, sl * 256:(sl + 1) * 256].rearrange("(k p) n -> p k n", p=128)), writes=[wb])
                for jj in range(2):
                    j = sl * 2 + jj
                    for k in range(16):
                        S.op("pe", lambda e, w=w, jj=jj, k=k, j=j: e.matmul(
                            mps[:, 2 * j:2 * j + 2], lhsT=w[:, k, jj * 128:(jj + 1) * 128], rhs=scv[:, k, :],
                            start=(k == 0), stop=(k == 15)), reads=[wb, scb], writes=[mpb])
                which = (sl * 256) // D
                if which in (2, 5):
                    wi = 0 if which == 2 else 1
                    c0 = sl * 256 - which * D
                    gps, gpb = P(4)
                    gi = (sl // 1) % 2
                    gst, gstb = gsts[gi]
                    br, brb = brs[gi]
                    S.dma("sp", lambda e, br=br, which=which, c0=c0, l=l: e.dma_start(
                        out=br[0:1, :], in_=I["b_ada"][l:l + 1, which * D + c0:which * D + c0 + 256]), writes=[brb])
                    for k in range(16):
                        S.op("pe", lambda e, w=w, k=k, gps=gps: e.matmul(
                            gps[0:2, 0:256], lhsT=scv[:, k, :], rhs=w[:, k, :], start=(k == 0), stop=False),
                            reads=[wb, scb], writes=[gpb])
                    S.op("pe", lambda e, gps=gps, br=br: e.matmul(
                        gps[0:2, 0:256], lhsT=ones_f[0:1, 0:2], rhs=br[0:1, :], start=False, stop=True),
                        reads=[brb, cb["ones"]], writes=[gpb])
                    S.op("dve", lambda e, gps=gps, gst=gst: e.tensor_copy(out=gst[0:2, :], in_=gps[0:2, 0:256]),
                         reads=[gpb], writes=[gstb])
                    S.dma("sp", lambda e, gst=gst, wi=wi, c0=c0, l=l: e.dma_start(
                        out=GROW[l, wi, :, c0:c0 + 256], in_=gst[0:2, :]), reads=[gstb], writes=[dbufs["GROW"]])
            for r in range(2):
                S.op("dve", lambda e, l=l, r=r: e.tensor_tensor(
                    out=mod[:, l, :, r], in0=mps[:, 0:192].rearrange("p (j r) -> p j r", r=2)[:, :, r],
                    in1=bfm, op=ALU.add), reads=[mpb, bfb], writes=[cb["mod"]])
            S.op("dve", lambda e, l=l: e.tensor_scalar_add(out=mod1[:, l], in0=mod[:, l], scalar1=1.0),
                 reads=[cb["mod"]], writes=[cb["mod"]])
            load_vec_fm(smallv[:, l, 0:48], cb["smallv"], I["b_gate"][l].rearrange("(n p) -> n p", p=128), 48)
            load_vec_fm(smallv[:, l, 48:54], cb["smallv"], I["g_q"][l].rearrange("(n p) -> n p", p=128), 6)
            load_vec_fm(smallv[:, l, 54:58], cb["smallv"], I["g_kv"][l].rearrange("(n p) -> n p", p=128), 4)
            load_vec_fm(smallv[:, l, 58:62], cb["smallv"], I["pool_scale"][l].rearrange("(n p) -> n p", p=128), 4)
            load_vec_fm(smallv[:, l, 62:63], cb["smallv"], I["g_sub"][l].rearrange("(n p) -> n p", p=128), 1)
            lt, ltb = AF32.alloc((4 * DIFF_HD,), "lamin")
            S.dma("sp", lambda e, l=l: e.dma_start(out=lt, in_=I["lam"][l].partition_broadcast(128)), writes=[ltb])
            lp, lpb = AF32.alloc((2, DIFF_HD), "lamp")
            ltv = lt.rearrange("p (a b d) -> p a b d", a=2, b=2)
            S.op("dve", lambda e: e.tensor_tensor(out=lp, in0=ltv[:, :, 0, :], in1=ltv[:, :, 1, :], op=ALU.mult),
                 reads=[ltb], writes=[lpb])
            S.op("dve", lambda e, l=l: e.tensor_reduce(out=lamt[:, l, 0:2], in_=lp, axis=AX.X, op=ALU.add),
                 reads=[lpb], writes=[cb["lamt"]])
            S.op("act", lambda e, l=l: e.activation(out=lamt[:, l, 2:4], in_=lamt[:, l, 0:2], func=AF.Exp),
                 reads=[cb["lamt"]], writes=[cb["lamt"]])
            lam_init = 0.8 - 0.6 * math.exp(-0.3 * l)
            S.op("dve", lambda e, l=l: e.tensor_tensor(out=lamt[:, l, 4:5], in0=lamt[:, l, 3:4], in1=lamt[:, l, 2:3],
                                                       op=ALU.subtract), reads=[cb["lamt"]], writes=[cb["lamt"]])
            S.op("dve", lambda e, l=l, li=lam_init: e.tensor_scalar_add(out=lamt[:, l, 5:6], in0=lamt[:, l, 4:5], scalar1=-li),
                 reads=[cb["lamt"]], writes=[cb["lamt"]])
            AF32.off = 0 if False else AF32.off

    def ln_tile(xt, xb, st, stb):
        S.op("dve", lambda e: e.tensor_reduce(out=st[:, 0:1], in_=xt, axis=AX.X, op=ALU.add), reads=[xb], writes=[stb])
        S.op("dve", lambda e: e.tensor_scalar(out=st[:, 1:2], in0=st[:, 0:1], scalar1=-1.0 / D, scalar2=None, op0=ALU.mult),
             reads=[stb], writes=[stb])
        S.op("dve", lambda e: e.tensor_scalar(out=st[:, 0:1], in0=st[:, 0:1], scalar1=1.0 / D, scalar2=None, op0=ALU.mult),
             reads=[stb], writes=[stb])

    def ln_finish(sq, sqb, st, stb):
        S.op("dve", lambda e: e.tensor_reduce(out=st[:, 2:3], in_=sq, axis=AX.X, op=ALU.add), reads=[sqb], writes=[stb])
        S.op("dve", lambda e: e.tensor_scalar(out=st[:, 2:3], in0=st[:, 2:3], scalar1=1.0 / D, scalar2=EPS, op0=ALU.mult,
                                              op1=ALU.add), reads=[stb], writes=[stb])
        S.op("act", lambda e: e.activation(out=st[:, 3:4], in_=st[:, 2:3], func=AF.Sqrt), reads=[stb], writes=[stb])
        S.op("dve", lambda e: e.reciprocal(out=st[:, 2:3], in_=st[:, 3:4]), reads=[stb], writes=[stb])

    def layernorm(xt, xb, xn, xnb, sq, sqb, st, stb):
        ln_tile(xt, xb, st, stb)
        S.op("act", lambda e: e.activation(out=sq, in_=xt, func=AF.Square, bias=st[:, 1:2], scale=1.0),
             reads=[xb, stb], writes=[sqb])
        ln_finish(sq, sqb, st, stb)
        S.op("dve", lambda e: e.tensor_scalar(out=xn, in0=xt, scalar1=st[:, 0:1], scalar2=st[:, 2:3], op0=ALU.subtract,
                                              op1=ALU.mult), reads=[xb, stb], writes=[xnb])

    def is_ctx_tile(tt):
        return tt < NTC

    def load_w_chunk(dst, dstb, src_cols_ap, q="pool"):
        S.dma(q, lambda e: e.dma_start(out=dst, in_=src_cols_ap.rearrange("(k p) n -> p k n", p=128)), writes=[dstb])

    def make_rot_w(w, wb, wp, wpb):
        wv = w.rearrange("p k (g h j) -> p (k g) h j", h=2, j=16)
        wpv = wp.rearrange("p k (g h j) -> p (k g) h j", h=2, j=16)
        S.op("pool", lambda e: e.tensor_scalar(out=wpv[:, :, 0, :], in0=wv[:, :, 1, :], scalar1=-1.0, scalar2=None,
                                               op0=ALU.mult), reads=[wb], writes=[wpb])
        S.op("pool", lambda e: e.tensor_copy(out=wpv[:, :, 1, :], in_=wv[:, :, 0, :]), reads=[wb], writes=[wpb])

    def phase_inproj(l):
        new_phase()
        uT, uTb = ABF.alloc((16, T), "uT")
        ssq_kv, ssq_kvb = AF32.alloc((T,), "ssq_kv")
        ssq_q, ssq_qb = AF32.alloc((T,), "ssq_q")
        mark = AF32.off
        xts = [AF32.alloc((D,), f"xt{i}") for i in range(2)]
        xn, xnb = AF32.alloc((D,), "xn")
        sq, sqb = AF32.alloc((D,), "sq")
        st, stb = AF32.alloc((4,), "st")
        for tt in range(NT):
            xt, xb = xts[tt % 2]
            S.dma("sp", lambda e, xt=xt, tt=tt: e.dma_start(out=xt, in_=XS[tt * 128:(tt + 1) * 128, :]),
                  reads=[dbufs["XS"]], writes=[xb])
            layernorm(xt, xb, xn, xnb, sq, sqb, st, stb)
            r = 1 if is_ctx_tile(tt) else 0
            for k in range(16):
                if k % 4 == 0:
                    ps, pb = P()
                S.op("pe", lambda e, ps=ps, k=k: e.transpose(ps[:, (k % 4) * 128:(k % 4 + 1) * 128],
                                                             xn[:, k * 128:(k + 1) * 128], ident[:]),
                     reads=[xnb, cb["ident"]], writes=[pb])
                S.op("act", lambda e, ps=ps, k=k, tt=tt, r=r: e.activation(
                    out=uT[:, k, tt * 128:(tt + 1) * 128], in_=ps[:, (k % 4) * 128:(k % 4 + 1) * 128], func=AF.Identity,
                    scale=mod1[:, l, 16 + k, r:r + 1], bias=mod[:, l, k, r:r + 1]), reads=[pb, cb["mod"]], writes=[uTb])
        AF32.off = mark
        if stop == "uT":
            return uT, uTb
        barrier()
        ropc, ropcb = AF32.alloc((T,), "ropc")
        rops, ropsb = AF32.alloc((T,), "rops")
        S.dma("sp", lambda e: e.dma_start(out=ropc, in_=I["ropc"]), writes=[ropcb])
        S.dma("sp", lambda e: e.dma_start(out=rops, in_=I["rops"]), writes=[ropsb])
        wch = [ABF.alloc((16, 128), f"wch{i}") for i in range(2)]
        wchp = [ABF.alloc((16, 128), f"wchp{i}") for i in range(2)]
        stg = [ABF.alloc((512,), f"stg{i}") for i in range(3)]
        sqs = [AF32.alloc((512,), f"sqs{i}") for i in range(2)]
        tmp = [AF32.alloc((512,), f"tmpa{i}") for i in range(2)]
        ctr = [0, 0, 0]

        def proj_chunk(c0, ncols, evac, rot=False, dup=False):
            if ctr[0] >= cfg.get("m2cut", 10 ** 9):
                return
            i = ctr[0] % 2
            ctr[0] += 1
            w, wb = wch[i]
            nw = ncols * (2 if dup else 1)
            load_w_chunk(w[:, :, 0:ncols], wb, I["w_in"][l][:, c0:c0 + ncols])
            if dup:
                load_w_chunk(w[:, :, ncols:2 * ncols], wb, I["w_in"][l][:, c0:c0 + ncols])
            if rot:
                wp, wpb = wchp[i]
                make_rot_w(w[:, :, 0:nw], wb, wp[:, :, 0:nw], wpb)
            for (b0, bw) in BLKS:
                ps, pb = P(6)
                for k in range(16):
                    S.op("pe", lambda e, ps=ps, k=k, b0=b0, bw=bw: e.matmul(
                        ps[0:nw, 0:bw], lhsT=w[:, k, 0:nw], rhs=uT[:, k, b0:b0 + bw], start=(k == 0), stop=(k == 15)),
                        reads=[wb, uTb], writes=[pb], sig=(k == 15))
                ps2, pb2 = None, None
                if rot:
                    ps2, pb2 = P(6)
                    for k in range(16):
                        S.op("pe", lambda e, ps2=ps2, k=k, b0=b0, bw=bw: e.matmul(
                            ps2[0:nw, 0:bw], lhsT=wp[:, k, 0:nw], rhs=uT[:, k, b0:b0 + bw], start=(k == 0), stop=(k == 15)),
                            reads=[wpb, uTb], writes=[pb2], sig=(k == 15))
                evac(ps, pb, ps2, pb2, b0, bw, nw)

        def next_stg():
            s = stg[ctr[1] % 3]
            ctr[1] += 1
            return s

        def evac_rope(dram_rows, dname):
            def f(ps, pb, ps2, pb2, b0, bw, nw):
                t1, t1b = tmp[0]
                t2, t2b = tmp[1]
                sg, sgb = next_stg()
                S.op("dve", lambda e: e.tensor_tensor(out=t1[0:nw, 0:bw], in0=ps[0:nw, 0:bw], in1=ropc[0:nw, b0:b0 + bw],
                                                      op=ALU.mult), reads=[pb, ropcb], writes=[t1b])
                S.op("dve", lambda e: e.tensor_tensor(out=t2[0:nw, 0:bw], in0=ps2[0:nw, 0:bw], in1=rops[0:nw, b0:b0 + bw],
                                                      op=ALU.mult), reads=[pb2, ropsb], writes=[t2b])
                S.op("pool", lambda e: e.tensor_tensor(out=sg[0:nw, 0:bw], in0=t1[0:nw, 0:bw], in1=t2[0:nw, 0:bw],
                                                       op=ALU.add), reads=[t1b, t2b], writes=[sgb])
                S.dma("sp", lambda e: e.dma_start(out=dram_rows[0:nw, b0:b0 + bw], in_=sg[0:nw, 0:bw]), reads=[sgb],
                      writes=[dbufs[dname]])
            return f

        def evac_rms(dram_rows, dname, gcol, ssq, ssqb, first):
            def f(ps, pb, ps2, pb2, b0, bw, nw):
                em = cfg.get("evacmode", 9)
                if em < 1:
                    return
                sg, sgb = next_stg()
                s2, s2b = sqs[ctr[2] % 2]
                ctr[2] += 1
                S.op("act", lambda e: e.activation(out=s2[:, 0:bw], in_=ps[:, 0:bw], func=AF.Square), reads=[pb], writes=[s2b])
                if em < 2:
                    return
                S.op("act", lambda e: e.activation(out=sg[:, 0:bw], in_=ps[:, 0:bw], func=AF.Identity,
                                                   scale=smallv[:, l, gcol:gcol + 1]), reads=[pb, cb["smallv"]], writes=[sgb])
                S.dma("sp", lambda e: e.dma_start(out=dram_rows[:, b0:b0 + bw], in_=sg[:, 0:bw]), reads=[sgb],
                      writes=[dbufs[dname]])
                if em < 3:
                    return
                pq, pqb = P(2, 6)
                S.op("pe", lambda e: e.matmul(pq[:, 0:bw], lhsT=ones_f[:], rhs=s2[:, 0:bw], start=True, stop=True),
                     reads=[s2b, cb["ones"]], writes=[pqb])
                if first:
                    S.op("dve", lambda e: e.tensor_copy(out=ssq[:, b0:b0 + bw], in_=pq[:, 0:bw]), reads=[pqb], writes=[ssqb])
                else:
                    S.op("dve", lambda e: e.tensor_tensor(out=ssq[:, b0:b0 + bw], in0=ssq[:, b0:b0 + bw], in1=pq[:, 0:bw],
                                                          op=ALU.add), reads=[pqb, ssqb], writes=[ssqb])
            return f

        for kc in range(4):
            proj_chunk(C_CKV + kc * 128, 128, evac_rms(CKG[kc], "CKG", 54 + kc, ssq_kv, ssq_kvb, kc == 0))
        proj_chunk(C_KROT, 64, evac_rope(KR2, "KR"), rot=True, dup=True)
        for h in range(4):
            proj_chunk(C_KDIFF + h * 128, 128, evac_rope(KD[h], "KD"), rot=True)
        for kc in range(6):
            proj_chunk(C_CQ + kc * 128, 128, evac_rms(CQG[kc], "CQG", 48 + kc, ssq_q, ssq_qb, kc == 0))
        for h in range(4):
            proj_chunk(C_QDIFF + h * 128, 128, evac_rope(QD[h], "QD"), rot=True)

        def evac_gate(j):
            def f(ps, pb, ps2, pb2, b0, bw, nw):
                sg, sgb = next_stg()
                S.op("act", lambda e: e.activation(out=sg[:, 0:bw], in_=ps[:, 0:bw], func=AF.Sigmoid,
                                                   bias=smallv[:, l, j:j + 1], scale=1.0), reads=[pb, cb["smallv"]], writes=[sgb])
                S.dma("sp", lambda e: e.dma_start(out=GT[j][:, b0:b0 + bw], in_=sg[:, 0:bw]), reads=[sgb], writes=[dbufs["GT"]])
            return f
        for j in range(48):
            proj_chunk(C_GATE + j * 128, 128, evac_gate(j))

        if "m2cut" in cfg:
            return ssq_kv, ssq_kvb, ssq_q, ssq_qb, mark
        wv, wvb = ABF.alloc((16, 512), "wvd")
        load_w_chunk(wv, wvb, I["w_in"][l][:, C_VDIFF:C_VDIFF + 512])
        vst = [ABF.alloc((4, 129), f"vst{i}") for i in range(2)]
        for i in range(2):
            S.op("pool", lambda e, i=i: e.memset(vst[i][0], 1.0), writes=[vst[i][1]])
        for tt in range(NT):
            ps, pb = P(6)
            for k in range(16):
                S.op("pe", lambda e, ps=ps, k=k, tt=tt: e.matmul(ps[:, 0:512], lhsT=uT[:, k, tt * 128:(tt + 1) * 128],
                                                                 rhs=wv[:, k, :], start=(k == 0), stop=(k == 15)),
                     reads=[uTb, wvb], writes=[pb])
            v, vb = vst[tt % 2]
            S.op("act", lambda e, ps=ps, v=v: e.activation(out=v[:, :, 0:128], in_=ps[:, 0:512].rearrange("p (h d) -> p h d", d=128),
                                                           func=AF.Copy), reads=[pb], writes=[vb])
            S.dma("sp", lambda e, v=v, tt=tt: e.dma_start(out=VD[tt], in_=v.rearrange("p h d -> p (h d)")), reads=[vb],
                  writes=[dbufs["VD"]])

        barrier()
        AF32.off = mark
        PADW = 16
        TP = T + 4 * PADW
        xp, xpb = AF32.alloc((TP,), "xp")
        b1, b1b = AF32.alloc((TP,), "b1")
        rc, rcb = AF32.alloc((T,), "rc")
        pooled, pooledb = ABF.alloc((T,), "pooled")
        wpl, wplb = ABF.alloc((128,), "wpool")

        def segs():
            return ((PADW, 0, T_CTX), (3 * PADW + T_CTX, T_CTX, T_LAT))
        for gi, wwin in enumerate(POOL_WINDOWS):
            half = wwin // 2
            S.op("pool", lambda e: e.memset(xp, 0.0), writes=[xpb])
            S.dma("sp", lambda e, gi=gi: e.dma_start(out=rc, in_=I["rcnt"][gi:gi + 1, :].partition_broadcast(128)), writes=[rcb])
            S.dma("pool", lambda e, gi=gi: e.dma_start(out=wpl, in_=I["w_pool"][l, gi]), writes=[wplb])

            def evac_pool(ps, pb, ps2, pb2, b0, bw, nw):
                po = PADW + b0 if b0 < T_CTX else 3 * PADW + b0
                S.op("act", lambda e: e.activation(out=xp[:, po:po + bw], in_=ps[:, 0:bw], func=AF.Copy), reads=[pb], writes=[xpb])
            proj_chunk(C_POOL + gi * 128, 128, evac_pool)
            src, srcb, dst, dstb = xp, xpb, b1, b1b
            m = 1
            first = True
            while m < wwin:
                if first:
                    S.op("dve", lambda e, m=m: e.tensor_tensor(out=b1[:, 0:TP - m], in0=xp[:, 0:TP - m], in1=xp[:, m:TP],
                                                               op=ALU.add), reads=[xpb], writes=[b1b])
                    first = False
                else:
                    S.op("dve", lambda e, m=m: e.tensor_tensor(out=b1[:, 0:TP - m], in0=b1[:, 0:TP - m], in1=b1[:, m:TP],
                                                               op=ALU.add), reads=[b1b], writes=[b1b])
                m *= 2
            for (po, t0, n) in segs():
                S.op("dve", lambda e, po=po, t0=t0, n=n, half=half: e.tensor_tensor(
                    out=b1[:, po - half:po - half + n], in0=b1[:, po - half:po - half + n], in1=rc[:, t0:t0 + n], op=ALU.mult),
                    reads=[b1b, rcb], writes=[b1b])
                S.op("dve", lambda e, po=po, t0=t0, n=n, half=half: e.tensor_tensor(
                    out=pooled[:, t0:t0 + n], in0=b1[:, po - half:po - half + n], in1=xp[:, po:po + n], op=ALU.subtract),
                    reads=[b1b, xpb], writes=[pooledb])
            for (b0, bw) in BLKS:
                ps, pb = P(6)
                S.op("pe", lambda e, ps=ps, b0=b0, bw=bw: e.matmul(ps[:, 0:bw], lhsT=wpl, rhs=pooled[:, b0:b0 + bw], start=True,
                                                                   stop=True), reads=[wplb, pooledb], writes=[pb])
                sg, sgb = next_stg()
                S.op("act", lambda e, ps=ps, sg=sg, bw=bw, gi=gi: e.activation(
                    out=sg[:, 0:bw], in_=ps[:, 0:bw], func=AF.Identity, scale=smallv[:, l, 58 + gi:59 + gi]),
                    reads=[pb, cb["smallv"]], writes=[sgb])
                S.dma("sp", lambda e, sg=sg, b0=b0, bw=bw, gi=gi: e.dma_start(out=OP[gi][:, b0:b0 + bw], in_=sg[:, 0:bw]),
                      reads=[sgb], writes=[dbufs["OP"]])
        return ssq_kv, ssq_kvb, ssq_q, ssq_qb, mark

    def rstd_inplace(ssq, ssqb, n, tmp, tmpb):
        S.op("dve", lambda e: e.tensor_scalar(out=ssq, in0=ssq, scalar1=1.0 / n, scalar2=EPS, op0=ALU.mult, op1=ALU.add),
             reads=[ssqb], writes=[ssqb])
        S.op("act", lambda e: e.activation(out=tmp, in_=ssq, func=AF.Sqrt), reads=[ssqb], writes=[tmpb])
        S.op("dve", lambda e: e.reciprocal(out=ssq, in_=tmp), reads=[tmpb], writes=[ssqb])

    def phase_upproj(l, ssq_kv, ssq_kvb, ssq_q, ssq_qb, mark):
        barrier()
        ABF.reset()
        AF32.off = mark
        tmpT, tmpTb = AF32.alloc((T,), "tmpT")
        rstd_inplace(ssq_kv, ssq_kvb, KV_RANK, tmpT, tmpTb)
        rstd_inplace(ssq_q, ssq_qb, Q_RANK, tmpT, tmpTb)
        rtm, rtmb = AF32.alloc((NT,), "rtm")
        ps, pb = P()
        for tt in range(NT):
            S.op("pe", lambda e, tt=tt: e.transpose(ps[:, tt:tt + 1], ssq_kv[0:1, tt * 128:(tt + 1) * 128], ident[0:1, 0:1]),
                 reads=[ssq_kvb, cb["ident"]], writes=[pb])
        S.op("dve", lambda e: e.tensor_copy(out=rtm, in_=ps[:, 0:NT]), reads=[pb], writes=[rtmb])
        ropc, ropcb = AF32.alloc((T,), "ropc")
        rops, ropsb = AF32.alloc((T,), "rops")
        S.dma("sp", lambda e: e.dma_start(out=ropc, in_=I["ropc"]), writes=[ropcb])
        S.dma("sp", lambda e: e.dma_start(out=rops, in_=I["rops"]), writes=[ropsb])
        t1, t1b = AF32.alloc((512,), "t1")
        t2, t2b = AF32.alloc((512,), "t2")
        ckg, ckgb = ABF.alloc((4, T), "ckg")
        cqg, cqgb = ABF.alloc((6, T), "cqg")
        S.dma("sp", lambda e: e.dma_start(out=ckg, in_=CKG.rearrange("k p t -> p k t")), reads=[dbufs["CKG"]], writes=[ckgb])
        S.dma("sp", lambda e: e.dma_start(out=cqg, in_=CQG.rearrange("k p t -> p k t")), reads=[dbufs["CQG"]], writes=[cqgb])
        wcs = [ABF.alloc((6, 128), f"wc{i}") for i in range(2)]
        wcp, wcpb = ABF.alloc((6, 128), "wcp")
        stg = [ABF.alloc((512,), f"stgu{i}") for i in range(3)]
        wv, wvb = ABF.alloc((4, 1024), "wv")
        vst = [ABF.alloc((8, 129), f"vstm{i}") for i in range(2)]
        ctr = [0, 0]

        def nstg():
            s = stg[ctr[1] % 3]
            ctr[1] += 1
            return s

        def nw():
            s = wcs[ctr[0] % 2]
            ctr[0] += 1
            return s
        for h in range(8):
            w, wb = nw()
            load_w_chunk(w[:, 0:4, :], wb, I["w_ukv"][l][:, h * 256:h * 256 + 128])
            for (b0, bw) in BLKS:
                ps, pb = P(6)
                for k in range(4):
                    S.op("pe", lambda e, ps=ps, k=k, b0=b0, bw=bw, w=w: e.matmul(
                        ps[:, 0:bw], lhsT=w[:, k, :], rhs=ckg[:, k, b0:b0 + bw], start=(k == 0), stop=(k == 3)),
                        reads=[wb, ckgb], writes=[pb])
                sg, sgb = nstg()
                S.op("dve", lambda e, ps=ps, sg=sg, b0=b0, bw=bw: e.tensor_tensor(
                    out=sg[:, 0:bw], in0=ps[:, 0:bw], in1=ssq_kv[:, b0:b0 + bw], op=ALU.mult), reads=[pb, ssq_kvb], writes=[sgb])
                S.dma("sp", lambda e, sg=sg, b0=b0, bw=bw, h=h: e.dma_start(out=KN[h][:, b0:b0 + bw], in_=sg[:, 0:bw]),
                      reads=[sgb], writes=[dbufs["KN"]])
            w, wb = nw()
            load_w_chunk(w, wb, I["w_uq"][l][:, h * 192:h * 192 + 128])
            for (b0, bw) in BLKS:
                ps, pb = P(6)
                for k in range(6):
                    S.op("pe", lambda e, ps=ps, k=k, b0=b0, bw=bw, w=w: e.matmul(
                        ps[:, 0:bw], lhsT=w[:, k, :], rhs=cqg[:, k, b0:b0 + bw], start=(k == 0), stop=(k == 5)),
                        reads=[wb, cqgb], writes=[pb])
                sg, sgb = nstg()
                S.op("dve", lambda e, ps=ps, sg=sg, b0=b0, bw=bw: e.tensor_tensor(
                    out=sg[:, 0:bw], in0=ps[:, 0:bw], in1=ssq_q[:, b0:b0 + bw], op=ALU.mult), reads=[pb, ssq_qb], writes=[sgb])
                S.dma("sp", lambda e, sg=sg, b0=b0, bw=bw, h=h: e.dma_start(out=QN[h][:, b0:b0 + bw], in_=sg[:, 0:bw]),
                      reads=[sgb], writes=[dbufs["QN"]])
            load_w_chunk(wv[:, :, h * 128:(h + 1) * 128], wvb, I["w_ukv"][l][:, h * 256 + 128:h * 256 + 256])
        for hp in range(4):
            w, wb = nw()
            for i in range(2):
                hh = 2 * hp + i
                load_w_chunk(w[:, :, i * 64:(i + 1) * 64], wb, I["w_uq"][l][:, hh * 192 + 128:hh * 192 + 192])
            make_rot_w(w, wb, wcp, wcpb)
            for (b0, bw) in BLKS:
                ps, pb = P(6)
                ps2, pb2 = P(6)
                for k in range(6):
                    S.op("pe", lambda e, ps=ps, k=k, b0=b0, bw=bw, w=w: e.matmul(
                        ps[:, 0:bw], lhsT=w[:, k, :], rhs=cqg[:, k, b0:b0 + bw], start=(k == 0), stop=(k == 5)),
                        reads=[wb, cqgb], writes=[pb])
                for k in range(6):
                    S.op("pe", lambda e, ps2=ps2, k=k, b0=b0, bw=bw: e.matmul(
                        ps2[:, 0:bw], lhsT=wcp[:, k, :], rhs=cqg[:, k, b0:b0 + bw], start=(k == 0), stop=(k == 5)),
                        reads=[wcpb, cqgb], writes=[pb2])
                sg, sgb = nstg()
                S.op("dve", lambda e, ps=ps, b0=b0, bw=bw: e.tensor_tensor(out=t1[:, 0:bw], in0=ps[:, 0:bw], in1=ropc[:, b0:b0 + bw],
                                                                        op=ALU.mult), reads=[pb, ropcb], writes=[t1b])
                S.op("dve", lambda e, ps2=ps2, b0=b0, bw=bw: e.tensor_tensor(out=t2[:, 0:bw], in0=ps2[:, 0:bw], in1=rops[:, b0:b0 + bw],
                                                                         op=ALU.mult), reads=[pb2, ropsb], writes=[t2b])
                S.op("pool", lambda e, bw=bw: e.tensor_tensor(out=t1[:, 0:bw], in0=t1[:, 0:bw], in1=t2[:, 0:bw], op=ALU.add),
                     reads=[t1b, t2b], writes=[t1b])
                S.op("dve", lambda e, sg=sg, b0=b0, bw=bw: e.tensor_tensor(out=sg[:, 0:bw], in0=t1[:, 0:bw], in1=ssq_q[:, b0:b0 + bw],
                                                                        op=ALU.mult), reads=[t1b, ssq_qb], writes=[sgb])
                for i in range(2):
                    S.dma("sp", lambda e, sg=sg, b0=b0, bw=bw, i=i, hp=hp: e.dma_start(
                        out=QR[2 * hp + i][:, b0:b0 + bw], in_=sg[i * 64:(i + 1) * 64, 0:bw]), reads=[sgb], writes=[dbufs["QR"]])
        for i in range(2):
            S.op("pool", lambda e, i=i: e.memset(vst[i][0], 1.0), writes=[vst[i][1]])
        for tt in range(NT):
            v, vb = vst[tt % 2]
            for half in range(2):
                ps, pb = P(6)
                for k in range(4):
                    S.op("pe", lambda e, ps=ps, k=k, tt=tt, half=half: e.matmul(
                        ps[:, 0:512], lhsT=ckg[:, k, tt * 128:(tt + 1) * 128], rhs=wv[:, k, half * 512:(half + 1) * 512],
                        start=(k == 0), stop=(k == 3)), reads=[ckgb, wvb], writes=[pb])
                S.op("act", lambda e, ps=ps, v=v, half=half, tt=tt: e.activation(
                    out=v[:, half * 4:(half + 1) * 4, 0:128], in_=ps[:, 0:512].rearrange("p (h d) -> p h d", d=128),
                    func=AF.Identity, scale=rtm[:, tt:tt + 1]), reads=[pb, rtmb], writes=[vb])
            S.dma("sp", lambda e, v=v, tt=tt: e.dma_start(out=VM[tt], in_=v.rearrange("p h d -> p (h d)")), reads=[vb],
                  writes=[dbufs["VM"]])

    def phase_attn(l):
        new_phase()
        kr, krb = ABF.alloc((T,), "kr")
        S.dma("sp", lambda e: e.dma_start(out=kr[0:64, :], in_=KR2[0:64, :]), reads=[dbufs["KR"]], writes=[krb])
        ops = [ABF.alloc((T,), f"kq{i}") for i in range(8)]
        vhs = [ABF.alloc((NT, 129), f"vh{i}") for i in range(2)]
        est = [ABF.alloc((512,), f"est{i}") for i in range(4)]
        ostg = [ABF.alloc((512,), f"ostg{i}") for i in range(2)]
        on = [AF32.alloc((128,), f"on{i}") for i in range(4)]
        ona = [AF32.alloc((4, 128), f"ona{i}") for i in range(2)]
        rc, rcb = AF32.alloc((8,), "rc")
        sqd, sqdb = AF32.alloc((128,), "sqd")
        gs, gsb = AF32.alloc((2,), "gs")
        lam_init = 0.8 - 0.6 * math.exp(-0.3 * l)
        S.op("dve", lambda e: e.tensor_scalar(out=gs[:, 0:1], in0=smallv[:, l, 62:63], scalar1=1.0 - lam_init, scalar2=None,
                                              op0=ALU.mult), reads=[cb["smallv"]], writes=[gsb])
        qblocks = []
        for (b0, bw) in BLKS:
            qblocks.append((b0, bw, NTC if b0 < T_CTX else NT))
        ectr = [0]

        rcp, rcpb = AF32.alloc((512,), "rcp")
        o0, o0b = AF32.alloc((512,), "o0f")
        o1, o1b = AF32.alloc((512,), "o1f")
        sq5, sq5b = AF32.alloc((512,), "sq5")
        t5, t5b = AF32.alloc((512,), "t5")
        OB, SB, QB = 4, 5, 6

        def attend(kpart, qpart, vh, vhb, scale, q0, qw, nkt):
            for kti in range(nkt):
                ps, pb = P(3)
                n = len(kpart)
                for i, ((ka, kb_), (qa, qb_)) in enumerate(zip(kpart, qpart)):
                    S.op("pe", lambda e, ps=ps, ka=ka, qa=qa, i=i, kti=kti, n=n: e.matmul(
                        ps[:, 0:qw], lhsT=ka[:, kti * 128:(kti + 1) * 128], rhs=qa[:, q0:q0 + qw], start=(i == 0), stop=(i == n - 1)),
                        reads=[kb_, qb_], writes=[pb], sig=(i == n - 1))
                es_, esb = est[ectr[0] % 4]
                ectr[0] += 1
                S.op("act", lambda e, ps=ps, es_=es_: e.activation(out=es_[:, 0:qw], in_=ps[:, 0:qw], func=AF.Exp, scale=scale),
                     reads=[pb], writes=[esb])
                last = kti == nkt - 1
                S.op("pe", lambda e, es_=es_, kti=kti: e.matmul(psum[OB][:, 0:qw], lhsT=vh[:, kti, 0:128], rhs=es_[:, 0:qw],
                                                              start=(kti == 0), stop=(kti == nkt - 1)),
                     reads=[esb, vhb], writes=[pbuf[OB]], sig=last)
                S.op("pe", lambda e, es_=es_, kti=kti: e.matmul(psum[SB][:, 0:qw], lhsT=ones_b[:], rhs=es_[:, 0:qw],
                                                              start=(kti == 0), stop=(kti == nkt - 1)),
                     reads=[esb, cb["ones"]], writes=[pbuf[SB]], sig=True)

        octr = [0]
        for h in range(MLA_HEADS):
            (kt, ktb), (qt, qtb), (qr, qrb) = ops[(h % 2) * 3:(h % 2) * 3 + 3]
            vh, vhb = vhs[h % 2]
            S.dma("sp", lambda e, kt=kt, h=h: e.dma_start(out=kt, in_=KN[h]), reads=[dbufs["KN"]], writes=[ktb])
            S.dma("sp", lambda e, qt=qt, h=h: e.dma_start(out=qt, in_=QN[h]), reads=[dbufs["QN"]], writes=[qtb])
            S.dma("sp", lambda e, qr=qr, h=h: e.dma_start(out=qr[0:64, :], in_=QR[h]), reads=[dbufs["QR"]], writes=[qrb])
            S.dma("sp", lambda e, vh=vh, h=h: e.dma_start(out=vh, in_=VM.rearrange("n p d -> p n d")[:, :, h * 129:(h + 1) * 129]),
                  reads=[dbufs["VM"]], writes=[vhb])
            for (q0, qw, nkt) in qblocks:
                attend([(kt, ktb), (kr[0:64, :], krb)], [(qt, qtb), (qr[0:64, :], qrb)], vh, vhb, 192.0 ** -0.5, q0, qw, nkt)
                og, ogb = ostg[octr[0] % 2]
                octr[0] += 1
                S.op("dve", lambda e, qw=qw: e.reciprocal(out=rcp[:, 0:qw], in_=psum[SB][:, 0:qw]), reads=[pbuf[SB]], writes=[rcpb])
                S.op("dve", lambda e, qw=qw, og=og: e.tensor_tensor(out=og[:, 0:qw], in0=psum[OB][:, 0:qw], in1=rcp[:, 0:qw], op=ALU.mult),
                     reads=[pbuf[OB], rcpb], writes=[ogb])
                S.dma("sp", lambda e, og=og, q0=q0, qw=qw, h=h: e.dma_start(out=OM[h][:, q0:q0 + qw], in_=og[:, 0:qw]),
                      reads=[ogb], writes=[dbufs["OM"]])
        for h in range(DIFF_HEADS):
            (kd, kdb), (qd, qdb) = ops[6:8] if h % 2 else ops[0:2]
            vh, vhb = vhs[h % 2]
            S.dma("sp", lambda e, kd=kd, h=h: e.dma_start(out=kd, in_=KD[h]), reads=[dbufs["KD"]], writes=[kdb])
            S.dma("sp", lambda e, qd=qd, h=h: e.dma_start(out=qd, in_=QD[h]), reads=[dbufs["QD"]], writes=[qdb])
            S.dma("sp", lambda e, vh=vh, h=h: e.dma_start(out=vh[:, :, :], in_=VD.rearrange("n p d -> p n d")[:, :, h * 129:(h + 1) * 129]),
                  reads=[dbufs["VD"]], writes=[vhb])
            for (q0, qw, nkt) in qblocks:
                for m, (om_, omb_) in enumerate(((o0, o0b), (o1, o1b))):
                    attend([(kd[m * 64:(m + 1) * 64, :], kdb)], [(qd[m * 64:(m + 1) * 64, :], qdb)], vh, vhb, 64.0 ** -0.5, q0, qw, nkt)
                    S.op("dve", lambda e, qw=qw: e.reciprocal(out=rcp[:, 0:qw], in_=psum[SB][:, 0:qw]), reads=[pbuf[SB]], writes=[rcpb])
                    S.op("dve", lambda e, qw=qw, om_=om_: e.tensor_tensor(out=om_[:, 0:qw], in0=psum[OB][:, 0:qw], in1=rcp[:, 0:qw],
                                                                        op=ALU.mult), reads=[pbuf[OB], rcpb], writes=[omb_])
                og, ogb = ostg[octr[0] % 2]
                octr[0] += 1
                S.op("dve", lambda e, qw=qw: e.scalar_tensor_tensor(out=o0[:, 0:qw], in0=o1[:, 0:qw], scalar=lamt[:, l, 5:6],
                                                                    in1=o0[:, 0:qw], op0=ALU.mult, op1=ALU.add),
                     reads=[o0b, o1b, cb["lamt"]], writes=[o0b])
                S.op("act", lambda e, qw=qw: e.activation(out=sq5[:, 0:qw], in_=o0[:, 0:qw], func=AF.Square), reads=[o0b], writes=[sq5b])
                S.op("pe", lambda e, qw=qw: e.matmul(psum[QB][:, 0:qw], lhsT=ones_f[:], rhs=sq5[:, 0:qw], start=True, stop=True),
                     reads=[sq5b, cb["ones"]], writes=[pbuf[QB]])
                S.op("dve", lambda e, qw=qw: e.tensor_copy(out=t5[:, 0:qw], in_=psum[QB][:, 0:qw]), reads=[pbuf[QB]], writes=[t5b])
                S.op("dve", lambda e, qw=qw: e.tensor_scalar(out=t5[:, 0:qw], in0=t5[:, 0:qw], scalar1=1.0 / 128, scalar2=EPS,
                                                             op0=ALU.mult, op1=ALU.add), reads=[t5b], writes=[t5b])
                S.op("act", lambda e, qw=qw: e.activation(out=t5[:, 0:qw], in_=t5[:, 0:qw], func=AF.Sqrt), reads=[t5b], writes=[t5b])
                S.op("dve", lambda e, qw=qw: e.reciprocal(out=t5[:, 0:qw], in_=t5[:, 0:qw]), reads=[t5b], writes=[t5b])
                S.op("dve", lambda e, qw=qw: e.tensor_tensor(out=o0[:, 0:qw], in0=o0[:, 0:qw], in1=t5[:, 0:qw], op=ALU.mult),
                     reads=[o0b, t5b], writes=[o0b])
                S.op("act", lambda e, qw=qw, og=og: e.activation(out=og[:, 0:qw], in_=o0[:, 0:qw], func=AF.Identity, scale=gs[:, 0:1]),
                     reads=[o0b, gsb], writes=[ogb])
                S.dma("sp", lambda e, og=og, q0=q0, qw=qw, h=h: e.dma_start(out=OD[h][:, q0:q0 + qw], in_=og[:, 0:qw]),
                      reads=[ogb], writes=[dbufs["OD"]])

    def phase_merge(l):
        new_phase()
        om, omb = ABF.alloc((8, T), "om")
        od, odb = ABF.alloc((4, T), "od")
        opp, oppb = ABF.alloc((4, T), "opp")
        S.dma("sp", lambda e: e.dma_start(out=om, in_=OM.rearrange("k p t -> p k t")), reads=[dbufs["OM"]], writes=[omb])
        S.dma("sp", lambda e: e.dma_start(out=od, in_=OD.rearrange("k p t -> p k t")), reads=[dbufs["OD"]], writes=[odb])
        S.dma("sp", lambda e: e.dma_start(out=opp, in_=OP.rearrange("k p t -> p k t")), reads=[dbufs["OP"]], writes=[oppb])
        wbs = [ABF.alloc((16, 128), f"wb{i}") for i in range(2)]
        gts = [ABF.alloc((3, 512), f"gt{i}") for i in range(2)]
        stg = [ABF.alloc((512,), f"stgm{i}") for i in range(2)]
        ta, tab = AF32.alloc((512,), "ta")
        tb, tbb = AF32.alloc((512,), "tb")
        ctr = 0
        for j in range(16):
            w, wb = wbs[j % 2]
            load_w_chunk(w[:, 0:8, :], wb, I["w_br_mla"][l][:, j * 128:(j + 1) * 128])
            load_w_chunk(w[:, 8:12, :], wb, I["w_br_diff"][l][:, j * 128:(j + 1) * 128])
            load_w_chunk(w[:, 12:16, :], wb, I["w_br_pool"][l][:, j * 128:(j + 1) * 128])
            for (b0, bw) in BLKS:
                g, gb = gts[ctr % 2]
                sg, sgb = stg[ctr % 2]
                ctr += 1
                for i in range(3):
                    S.dma("sp", lambda e, g=g, i=i, b0=b0, bw=bw, j=j: e.dma_start(out=g[:, i, 0:bw], in_=GT[i * 16 + j][:, b0:b0 + bw]),
                          reads=[dbufs["GT"]], writes=[gb])
                pss = []
                for (src_, srcb_, k0, nk) in ((om, omb, 0, 8), (od, odb, 8, 4), (opp, oppb, 12, 4)):
                    ps, pb = P(6)
                    for k in range(nk):
                        S.op("pe", lambda e, ps=ps, k=k, k0=k0, nk=nk, src_=src_, b0=b0, bw=bw, w=w: e.matmul(
                            ps[:, 0:bw], lhsT=w[:, k0 + k, :], rhs=src_[:, k, b0:b0 + bw], start=(k == 0), stop=(k == nk - 1)),
                            reads=[wb, srcb_], writes=[pb], sig=(k == nk - 1))
                    pss.append((ps, pb))
                S.op("dve", lambda e, p=pss[0][0], g=g, bw=bw: e.tensor_tensor(out=ta[:, 0:bw], in0=p[:, 0:bw], in1=g[:, 0, 0:bw],
                                                                            op=ALU.mult), reads=[pss[0][1], gb], writes=[tab])
                S.op("dve", lambda e, p=pss[1][0], g=g, bw=bw: e.tensor_tensor(out=tb[:, 0:bw], in0=p[:, 0:bw], in1=g[:, 1, 0:bw],
                                                                            op=ALU.mult), reads=[pss[1][1], gb], writes=[tbb])
                S.op("pool", lambda e, bw=bw: e.tensor_tensor(out=ta[:, 0:bw], in0=ta[:, 0:bw], in1=tb[:, 0:bw], op=ALU.add),
                     reads=[tab, tbb], writes=[tab])
                S.op("dve", lambda e, p=pss[2][0], g=g, bw=bw: e.tensor_tensor(out=tb[:, 0:bw], in0=p[:, 0:bw], in1=g[:, 2, 0:bw],
                                                                            op=ALU.mult), reads=[pss[2][1], gb], writes=[tbb])
                S.op("pool", lambda e, sg=sg, bw=bw: e.tensor_tensor(out=sg[:, 0:bw], in0=ta[:, 0:bw], in1=tb[:, 0:bw], op=ALU.add),
                     reads=[tab, tbb], writes=[sgb])
                S.dma("sp", lambda e, sg=sg, b0=b0, bw=bw, j=j: e.dma_start(out=MT[j][:, b0:b0 + bw], in_=sg[:, 0:bw]),
                      reads=[sgb], writes=[dbufs["MT"]])

    def deepnorm_tile(src4, xt, xb, t, tb_, sq, sqb, st, stb, gbc, gbcb, lng, lngb, lnb, lnbb):
        for nb, (ya, yb) in enumerate(src4):
            S.op("dve", lambda e, ya=ya, nb=nb: e.tensor_tensor(out=t[:, nb * 512:(nb + 1) * 512], in0=ya,
                                                               in1=gbc[:, nb * 512:(nb + 1) * 512], op=ALU.mult),
                 reads=[yb, gbcb], writes=[tb_])
        S.op("dve", lambda e: e.scalar_tensor_tensor(out=t, in0=xt, scalar=DN_ALPHA, in1=t, op0=ALU.mult, op1=ALU.add),
             reads=[xb, tb_], writes=[tb_])
        layernorm(t, tb_, xt, xb, sq, sqb, st, stb)
        S.op("dve", lambda e: e.tensor_tensor(out=xt, in0=xt, in1=lng, op=ALU.mult), reads=[xb, lngb], writes=[xb])
        S.op("pool", lambda e: e.tensor_tensor(out=xt, in0=xt, in1=lnb, op=ALU.add), reads=[xb, lnbb], writes=[xb])

    def phase_wo(l):
        new_phase()
        wo, wob = ABF.alloc((16, D), "wo")
        for nb in range(4):
            load_w_chunk(wo[:, :, nb * 512:(nb + 1) * 512], wob, I["w_o"][l][:, nb * 512:(nb + 1) * 512])
        mts = [ABF.alloc((16, 128), f"mt{i}") for i in range(2)]
        gl, glb = AF32.alloc((D,), "g1lat")
        gc, gcb = AF32.alloc((D,), "g1ctx")
        lng, lngb = AF32.alloc((D,), "lng")
        lnb, lnbb = AF32.alloc((D,), "lnb")
        S.dma("sp", lambda e: e.dma_start(out=gl, in_=GROW[l, 0, 0:1, :].partition_broadcast(128)), reads=[dbufs["GROW"]], writes=[glb])
        S.dma("sp", lambda e: e.dma_start(out=gc, in_=GROW[l, 0, 1:2, :].partition_broadcast(128)), reads=[dbufs["GROW"]], writes=[gcb])
        S.dma("sp", lambda e: e.dma_start(out=lng, in_=I["ln1_g"][l:l + 1, :].partition_broadcast(128)), writes=[lngb])
        S.dma("sp", lambda e: e.dma_start(out=lnb, in_=I["ln1_b"][l:l + 1, :].partition_broadcast(128)), writes=[lnbb])
        xt, xb = AF32.alloc((D,), "xtw")
        t, tb_ = AF32.alloc((D,), "tw")
        sq, sqb = AF32.alloc((D,), "sqw")
        st, stb = AF32.alloc((4,), "stw")
        for tt in range(NT):
            mt, mtb = mts[tt % 2]
            S.dma("sp", lambda e, mt=mt, tt=tt: e.dma_start(out=mt, in_=MT.rearrange("k p t -> p k t")[:, :, tt * 128:(tt + 1) * 128]),
                  reads=[dbufs["MT"]], writes=[mtb])
            S.dma("sp", lambda e, tt=tt: e.dma_start(out=xt, in_=XS[tt * 128:(tt + 1) * 128, :]), reads=[dbufs["XS"]], writes=[xb])
            src4 = []
            for nb in range(4):
                b = 4 + nb
                for k in range(16):
                    S.op("pe", lambda e, b=b, k=k, nb=nb, mt=mt: e.matmul(psum[b][:, 0:512], lhsT=mt[:, k, :],
                                                                        rhs=wo[:, k, nb * 512:(nb + 1) * 512], start=(k == 0),
                                                                        stop=(k == 15)), reads=[mtb, wob], writes=[pbuf[b]], sig=(k == 15))
                src4.append((psum[b][:, 0:512], pbuf[b]))
            g, gb_ = (gc, gcb) if is_ctx_tile(tt) else (gl, glb)
            deepnorm_tile(src4, xt, xb, t, tb_, sq, sqb, st, stb, g, gb_, lng, lngb, lnb, lnbb)
            S.dma("sp", lambda e, tt=tt: e.dma_start(out=XS[tt * 128:(tt + 1) * 128, :], in_=xt), reads=[xb], writes=[dbufs["XS"]])

    NR = NG + NE
    NSLOT = NE * CAP
    regs = {}

    def _init_pool(eng):
        regs["bc"] = eng.alloc_register("bc")
        eng.reg_mov(regs["bc"], NSLOT - 1)
    S.init.setdefault("pool", []).append(_init_pool)

    def phase_route(l):
        new_phase()
        for i in range(NSLOT // 128):
            S.dma("sp", lambda e, i=i: e.dma_start(out=XSORT[i * 128:(i + 1) * 128, :], in_=zero_b[:]), reads=[cb["zero"]],
                  writes=[dbufs["XSORT"]])
        S.op("pool", lambda e: e.memset(ebase[:], 0.0), writes=[cb["ebase"]])
        wr, wrb = AF32.alloc((16, NR), "wr")
        rb, rbb = AF32.alloc((NR,), "rb")
        S.dma("sp", lambda e: e.dma_start(out=wr[:, :, 0:NG], in_=I["w_grp"][l][:, 0:NG].rearrange("(k p) n -> p k n", p=128), allow_slow_non_contiguous=True), writes=[wrb])
        S.dma("sp", lambda e: e.dma_start(out=wr[:, :, NG:NR], in_=I["w_exp"][l][:, 0:NE].rearrange("(k p) n -> p k n", p=128), allow_slow_non_contiguous=True), writes=[wrb])
        S.dma("sp", lambda e: e.dma_start(out=rb[:, 0:NG], in_=I["b_grp"][l:l + 1, 0:NG].partition_broadcast(128)), writes=[rbb])
        S.dma("sp", lambda e: e.dma_start(out=rb[:, NG:NR], in_=I["b_exp"][l:l + 1, 0:NE].partition_broadcast(128)), writes=[rbb])
        xt, xb = AF32.alloc((D,), "xtr")
        xn, xnb = AF32.alloc((D,), "xnr")
        sq, sqb = AF32.alloc((D,), "sqr")
        vT, vTb = AF32.alloc((16, 128), "vT")
        st, stb = AF32.alloc((4,), "str")
        lg, lgb = AF32.alloc((NR,), "lg")
        ge, geb = AF32.alloc((NG,), "ge")
        pen, penb = AF32.alloc((NG,), "pen")
        msk, mskb = AF32.alloc((NE,), "msk")
        msk2, msk2b = AF32.alloc((NE,), "msk2")
        ohs = [AF32.alloc((NE,), f"oh{i}") for i in range(2)]
        pos, posb = AF32.alloc((NE,), "pos")
        tmpe, tmpeb = AF32.alloc((NE,), "tmpe")
        s_, sb_ = AF32.alloc((16,), "rs")
        vbs = [ABF.alloc((D,), f"vb{i}") for i in range(2)]
        for tt in range(NT):
            r = 1 if is_ctx_tile(tt) else 0
            S.dma("sp", lambda e, tt=tt: e.dma_start(out=xt, in_=XS[tt * 128:(tt + 1) * 128, :]), reads=[dbufs["XS"]], writes=[xb])
            layernorm(xt, xb, xn, xnb, sq, sqb, st, stb)
            for k in range(16):
                if k % 4 == 0:
                    ps, pb = P(4)
                S.op("pe", lambda e, ps=ps, k=k: e.transpose(ps[:, (k % 4) * 128:(k % 4 + 1) * 128], xn[:, k * 128:(k + 1) * 128],
                                                             ident[:]), reads=[xnb, cb["ident"]], writes=[pb])
                S.op("act", lambda e, ps=ps, k=k, r=r: e.activation(
                    out=vT[:, k, :], in_=ps[:, (k % 4) * 128:(k % 4 + 1) * 128], func=AF.Identity,
                    scale=mod1[:, l, 64 + k, r:r + 1], bias=mod[:, l, 48 + k, r:r + 1]), reads=[pb, cb["mod"]], writes=[vTb])
            pr, prb = P(2, 4)
            for k in range(16):
                S.op("pe", lambda e, k=k, pr=pr: e.matmul(pr[:, 0:NR], lhsT=vT[:, k, :], rhs=wr[:, k, :], start=(k == 0), stop=(k == 15)),
                     reads=[vTb, wrb], writes=[prb])
            S.op("dve", lambda e, pr=pr: e.tensor_tensor(out=lg, in0=pr[:, 0:NR], in1=rb, op=ALU.add), reads=[prb, rbb], writes=[lgb])
            vb_, vbb = vbs[tt % 2]
            for k in range(16):
                if k % 4 == 0:
                    ps, pb = P(4)
                S.op("pe", lambda e, ps=ps, k=k: e.transpose(ps[:, (k % 4) * 128:(k % 4 + 1) * 128], vT[:, k, :], ident[:]),
                     reads=[vTb, cb["ident"]], writes=[pb])
                if k % 4 == 3:
                    kb = k // 4
                    if kb % 2 == 0:
                        S.op("act", lambda e, ps=ps, kb=kb, vb_=vb_: e.activation(out=vb_[:, kb * 512:(kb + 1) * 512], in_=ps[:, 0:512],
                                                                                 func=AF.Identity), reads=[pb], writes=[vbb])
                    else:
                        S.op("dve", lambda e, ps=ps, kb=kb, vb_=vb_: e.tensor_copy(out=vb_[:, kb * 512:(kb + 1) * 512], in_=ps[:, 0:512]),
                             reads=[pb], writes=[vbb])
            S.op("dve", lambda e: e.tensor_reduce(out=s_[:, 0:1], in_=lg[:, 0:NG], axis=AX.X, op=ALU.max), reads=[lgb], writes=[sb_])
            S.op("dve", lambda e: e.tensor_scalar(out=ge, in0=lg[:, 0:NG], scalar1=s_[:, 0:1], scalar2=None, op0=ALU.subtract),
                 reads=[lgb, sb_], writes=[geb])
            S.op("act", lambda e: e.activation(out=ge, in_=ge, func=AF.Exp), reads=[geb], writes=[geb])
            S.op("dve", lambda e: e.tensor_reduce(out=s_[:, 1:2], in_=ge, axis=AX.X, op=ALU.add), reads=[geb], writes=[sb_])
            S.op("dve", lambda e: e.reciprocal(out=s_[:, 2:3], in_=s_[:, 1:2]), reads=[sb_], writes=[sb_])
            S.op("dve", lambda e: e.tensor_scalar(out=pen, in0=lg[:, 0:NG], scalar1=s_[:, 0:1], scalar2=None, op0=ALU.is_equal),
                 reads=[lgb, sb_], writes=[penb])
            S.op("dve", lambda e: e.tensor_scalar(out=pen, in0=pen, scalar1=-1.0, scalar2=BIG, op0=ALU.add, op1=ALU.mult),
                 reads=[penb], writes=[penb])
            for g in range(NG):
                S.op("dve", lambda e, g=g: e.tensor_scalar(out=msk[:, g * EPG:(g + 1) * EPG], in0=lg[:, NG + g * EPG:NG + (g + 1) * EPG],
                                                           scalar1=pen[:, g:g + 1], scalar2=None, op0=ALU.add),
                     reads=[lgb, penb], writes=[mskb])
            oh1, oh1b = ohs[0]
            oh2, oh2b = ohs[1]
            S.op("dve", lambda e: e.tensor_reduce(out=s_[:, 3:4], in_=msk, axis=AX.X, op=ALU.max), reads=[mskb], writes=[sb_])
            S.op("dve", lambda e: e.tensor_scalar(out=oh1, in0=msk, scalar1=s_[:, 3:4], scalar2=None, op0=ALU.is_equal),
                 reads=[mskb, sb_], writes=[oh1b])
            S.op("dve", lambda e: e.scalar_tensor_tensor(out=msk2, in0=oh1, scalar=-BIG, in1=msk, op0=ALU.mult, op1=ALU.add),
                 reads=[oh1b, mskb], writes=[msk2b])
            S.op("dve", lambda e: e.tensor_reduce(out=s_[:, 4:5], in_=msk2, axis=AX.X, op=ALU.max), reads=[msk2b], writes=[sb_])
            S.op("dve", lambda e: e.tensor_scalar(out=oh2, in0=msk2, scalar1=s_[:, 4:5], scalar2=None, op0=ALU.is_equal),
                 reads=[msk2b, sb_], writes=[oh2b])
            S.op("dve", lambda e: e.tensor_tensor(out=s_[:, 5:6], in0=s_[:, 3:4], in1=s_[:, 4:5], op=ALU.subtract), reads=[sb_], writes=[sb_])
            S.op("act", lambda e: e.activation(out=s_[:, 6:7], in_=s_[:, 5:6], func=AF.Sigmoid), reads=[sb_], writes=[sb_])
            S.op("act", lambda e: e.activation(out=s_[:, 7:8], in_=s_[:, 5:6], func=AF.Sigmoid, scale=-1.0), reads=[sb_], writes=[sb_])
            S.op("dve", lambda e, tt=tt: e.tensor_scalar(out=gatew[:, tt, :], in0=s_[:, 6:8], scalar1=s_[:, 2:3], scalar2=None,
                                                         op0=ALU.mult), reads=[sb_], writes=[cb["gatew"]])
            for kk, (oh, ohb) in enumerate(ohs):
                pp, ppb = P(2, 6)
                S.op("pe", lambda e, pp=pp, oh=oh: e.matmul(pp[:, 0:NE], lhsT=tril[:], rhs=oh, start=True, stop=True),
                     reads=[ohb, cb["tril"]], writes=[ppb])
                S.op("dve", lambda e, pp=pp: e.tensor_tensor(out=pos, in0=pp[:, 0:NE], in1=ebase[:], op=ALU.add),
                     reads=[ppb, cb["ebase"]], writes=[posb])
                S.op("dve", lambda e, oh=oh: e.tensor_tensor(out=tmpe, in0=oh, in1=pos, op=ALU.mult), reads=[ohb, posb], writes=[tmpeb])
                S.op("dve", lambda e: e.tensor_reduce(out=s_[:, 8:9], in_=tmpe, axis=AX.X, op=ALU.add), reads=[tmpeb], writes=[sb_])
                S.op("dve", lambda e, oh=oh: e.tensor_tensor(out=tmpe, in0=oh, in1=eoff[:], op=ALU.mult), reads=[ohb, cb["eoff"]],
                     writes=[tmpeb])
                S.op("dve", lambda e: e.tensor_reduce(out=s_[:, 9:10], in_=tmpe, axis=AX.X, op=ALU.add), reads=[tmpeb], writes=[sb_])
                S.op("dve", lambda e: e.tensor_scalar(out=s_[:, 10:11], in0=s_[:, 8:9], scalar1=float(CAP) - 0.5, scalar2=1.0e9,
                                                      op0=ALU.is_ge, op1=ALU.mult), reads=[sb_], writes=[sb_])
                S.op("dve", lambda e: e.tensor_tensor(out=s_[:, 8:9], in0=s_[:, 8:9], in1=s_[:, 9:10], op=ALU.add), reads=[sb_], writes=[sb_])
                S.op("dve", lambda e: e.tensor_tensor(out=s_[:, 8:9], in0=s_[:, 8:9], in1=s_[:, 10:11], op=ALU.add), reads=[sb_], writes=[sb_])
                S.op("dve", lambda e, tt=tt, kk=kk: e.tensor_copy(out=dest_i[:, tt, kk:kk + 1], in_=s_[:, 8:9]), reads=[sb_],
                     writes=[cb["dest"]])
                pt, ptb = P(2, 6)
                S.op("pe", lambda e, pt=pt, oh=oh: e.matmul(pt[:, 0:NE], lhsT=ones_f[:], rhs=oh, start=True, stop=True),
                     reads=[ohb, cb["ones"]], writes=[ptb])
                S.op("dve", lambda e, pt=pt: e.tensor_tensor(out=ebase[:], in0=ebase[:], in1=pt[:, 0:NE], op=ALU.add),
                     reads=[ptb, cb["ebase"]], writes=[cb["ebase"]])
                S.dma("pool", lambda e, tt=tt, kk=kk, vb_=vb_: e.indirect_dma_start(
                    out=XSORT, out_offset=bass.IndirectOffsetOnAxis(ap=dest_i[:, tt, kk:kk + 1], axis=0), in_=vb_, in_offset=None,
                    bounds_check=regs["bc"], oob_is_err=False), reads=[vbb, cb["dest"]], writes=[dbufs["XSORT"]])

    def phase_experts(l):
        new_phase()
        wgus = [ABF.alloc((16, 1024), f"wgu{i}") for i in range(2)]
        wdns = [ABF.alloc((4, D), f"wdn{i}") for i in range(2)]
        xes = [ABF.alloc((D,), f"xe{i}") for i in range(2)]
        xeTs = [ABF.alloc((16, 128), f"xeT{i}") for i in range(2)]
        a_s = [ABF.alloc((512,), f"aact{i}") for i in range(2)]
        aTs = [ABF.alloc((4, 128), f"aT{i}") for i in range(2)]
        sgs = [AF32.alloc((512,), f"sgl{i}") for i in range(2)]
        ys = [AF32.alloc((D,), f"ye{i}") for i in range(2)]
        NST = CAP // 128
        tiles = [(ex, s) for ex in range(NE) for s in range(NST)]
        n = len(tiles)
        dctr = [0]

        def load_gu(ex):
            if ex < NE:
                load_w_chunk(wgus[ex % 2][0], wgus[ex % 2][1], I["w_gu"][l, ex])

        dnst = [AF32.alloc((D,), f"dnst{i}") for i in range(2)]
        cctr = [0]

        def load_dn(ex):
            if ex < NE:
                wdn, wdnb = wdns[ex % 2]
                for kc in range(4):
                    sg_, sgb_ = dnst[cctr[0] % 2]
                    eng = "dve" if cctr[0] % 2 == 0 else "act"
                    cctr[0] += 1
                    S.dma("sp", lambda e, sg_=sg_, kc=kc, ex=ex: e.dma_start(out=sg_, in_=I["w_dn"][l, ex][kc * 128:(kc + 1) * 128, :]),
                          writes=[sgb_])
                    if eng == "dve":
                        S.op("dve", lambda e, sg_=sg_, kc=kc, wdn=wdn: e.tensor_copy(out=wdn[:, kc, :], in_=sg_), reads=[sgb_], writes=[wdnb])
                    else:
                        S.op("act", lambda e, sg_=sg_, kc=kc, wdn=wdn: e.activation(out=wdn[:, kc, :], in_=sg_, func=AF.Identity),
                             reads=[sgb_], writes=[wdnb])

        def stA(i):
            ex, s = tiles[i]
            r0 = ex * CAP + s * 128
            xe, xeb = xes[i % 2]
            xeT, xeTb = xeTs[i % 2]
            S.dma("sp", lambda e: e.dma_start(out=xe, in_=XSORT[r0:r0 + 128, :]), reads=[dbufs["XSORT"]], writes=[xeb])
            for kb in range(4):
                ps, pb = psum[kb % 2], pbuf[kb % 2]
                for kk in range(4):
                    k = kb * 4 + kk
                    S.op("pe", lambda e, ps=ps, k=k, kk=kk: e.matmul(ps[:, kk * 128:(kk + 1) * 128], lhsT=xe[:, k * 128:(k + 1) * 128],
                                                                    rhs=identb[:], start=True, stop=True),
                         reads=[xeb, cb["identb"]], writes=[pb], sig=(kk == 3))
                dstv = xeT[:, kb * 4:(kb + 1) * 4, :].rearrange("p a b -> p (a b)")
                if kb % 2 == 0:
                    S.op("act", lambda e, ps=ps, dstv=dstv: e.activation(out=dstv, in_=ps[:, 0:512], func=AF.Identity),
                         reads=[pb], writes=[xeTb])
                else:
                    S.op("dve", lambda e, ps=ps, dstv=dstv: e.tensor_copy(out=dstv, in_=ps[:, 0:512]), reads=[pb], writes=[xeTb])

        def stB(i):
            ex, s = tiles[i]
            wgu, wgub = wgus[ex % 2]
            xeT, xeTb = xeTs[i % 2]
            sg, sgb = sgs[i % 2]
            a_, ab_ = a_s[i % 2]
            for (bk, c0) in ((2, 0), (3, 512)):
                for k in range(16):
                    S.op("pe", lambda e, bk=bk, k=k, c0=c0: e.matmul(psum[bk][:, 0:512], lhsT=xeT[:, k, :], rhs=wgu[:, k, c0:c0 + 512],
                                                                    start=(k == 0), stop=(k == 15)),
                         reads=[xeTb, wgub], writes=[pbuf[bk]], sig=(k == 15))
            S.op("act", lambda e: e.activation(out=sg, in_=psum[2][:, 0:512], func=AF.Silu), reads=[pbuf[2]], writes=[sgb])
            S.op("dve", lambda e: e.tensor_tensor(out=a_, in0=psum[3][:, 0:512], in1=sg, op=ALU.mult), reads=[pbuf[3], sgb], writes=[ab_])
            if s == NST - 1:
                load_gu(ex + 2)

        def stC(i):
            a_, ab_ = a_s[i % 2]
            aT, aTb = aTs[i % 2]
            for k in range(4):
                S.op("pe", lambda e, k=k: e.matmul(psum[4][:, k * 128:(k + 1) * 128], lhsT=a_[:, k * 128:(k + 1) * 128], rhs=identb[:],
                                                  start=True, stop=True), reads=[ab_, cb["identb"]], writes=[pbuf[4]], sig=(k == 3))
            S.op("dve", lambda e: e.tensor_copy(out=aT.rearrange("p a b -> p (a b)"), in_=psum[4][:, 0:512]), reads=[pbuf[4]], writes=[aTb])

        def stD(i):
            ex, s = tiles[i]
            r0 = ex * CAP + s * 128
            wdn, wdnb = wdns[ex % 2]
            aT, aTb = aTs[i % 2]
            y, yb = ys[i % 2]
            for nb in range(4):
                bk = 5 + dctr[0] % 3
                dctr[0] += 1
                for k in range(4):
                    S.op("pe", lambda e, bk=bk, k=k, nb=nb: e.matmul(psum[bk][:, 0:512], lhsT=aT[:, k, :],
                                                                    rhs=wdn[:, k, nb * 512:(nb + 1) * 512], start=(k == 0), stop=(k == 3)),
                         reads=[aTb, wdnb], writes=[pbuf[bk]], sig=(k == 3))
                if nb % 2 == 0:
                    S.op("act", lambda e, bk=bk, nb=nb: e.activation(out=y[:, nb * 512:(nb + 1) * 512], in_=psum[bk][:, 0:512],
                                                                    func=AF.Identity), reads=[pbuf[bk]], writes=[yb])
                else:
                    S.op("dve", lambda e, bk=bk, nb=nb: e.tensor_copy(out=y[:, nb * 512:(nb + 1) * 512], in_=psum[bk][:, 0:512]),
                         reads=[pbuf[bk]], writes=[yb])
            S.dma("sp", lambda e: e.dma_start(out=YSORT[r0:r0 + 128, :], in_=y), reads=[yb], writes=[dbufs["YSORT"]])
            if s == NST - 1:
                load_dn(ex + 2)

        for ex in range(2):
            load_gu(ex)
            load_dn(ex)
        for it in range(n + 3):
            if it < n:
                stA(it)
            if 0 <= it - 1 < n:
                stB(it - 1)
            if 0 <= it - 2 < n:
                stC(it - 2)
            if 0 <= it - 3 < n:
                stD(it - 3)

    def phase_combine(l, last):
        new_phase()
        gl, glb = AF32.alloc((D,), "g2lat")
        gc, gcb = AF32.alloc((D,), "g2ctx")
        lng, lngb = AF32.alloc((D,), "lng2")
        lnb, lnbb = AF32.alloc((D,), "lnb2")
        S.dma("sp", lambda e: e.dma_start(out=gl, in_=GROW[l, 1, 0:1, :].partition_broadcast(128)), reads=[dbufs["GROW"]], writes=[glb])
        S.dma("sp", lambda e: e.dma_start(out=gc, in_=GROW[l, 1, 1:2, :].partition_broadcast(128)), reads=[dbufs["GROW"]], writes=[gcb])
        S.dma("sp", lambda e: e.dma_start(out=lng, in_=I["ln2_g"][l:l + 1, :].partition_broadcast(128)), writes=[lngb])
        S.dma("sp", lambda e: e.dma_start(out=lnb, in_=I["ln2_b"][l:l + 1, :].partition_broadcast(128)), writes=[lnbb])
        A, Ab = AF32.alloc((D,), "yA")
        B, Bb = AF32.alloc((D,), "yB")
        xt, xb = AF32.alloc((D,), "xtc")
        st, stb = AF32.alloc((4,), "stc")
        for tt in range(NT):
            S.op("pool", lambda e: e.memset(A, 0.0), writes=[Ab])
            S.op("pool", lambda e: e.memset(B, 0.0), writes=[Bb])
            for kk, (dst, dstb) in enumerate(((A, Ab), (B, Bb))):
                S.dma("pool", lambda e, tt=tt, kk=kk, dst=dst: e.indirect_dma_start(
                    out=dst, out_offset=None, in_=YSORT, in_offset=bass.IndirectOffsetOnAxis(ap=dest_i[:, tt, kk:kk + 1], axis=0),
                    bounds_check=regs["bc"], oob_is_err=False), reads=[dbufs["YSORT"], cb["dest"]], writes=[dstb])
            S.dma("sp", lambda e, tt=tt: e.dma_start(out=xt, in_=XS[tt * 128:(tt + 1) * 128, :]), reads=[dbufs["XS"]], writes=[xb])
            S.op("dve", lambda e, tt=tt: e.tensor_scalar(out=A, in0=A, scalar1=gatew[:, tt, 0:1], scalar2=None, op0=ALU.mult),
                 reads=[Ab, cb["gatew"]], writes=[Ab])
            S.op("dve", lambda e, tt=tt: e.scalar_tensor_tensor(out=A, in0=B, scalar=gatew[:, tt, 1:2], in1=A, op0=ALU.mult, op1=ALU.add),
                 reads=[Ab, Bb, cb["gatew"]], writes=[Ab])
            g, gb_ = (gc, gcb) if is_ctx_tile(tt) else (gl, glb)
            src4 = [(A[:, nb * 512:(nb + 1) * 512], Ab) for nb in range(4)]
            deepnorm_tile(src4, xt, xb, B, Bb, A, Ab, st, stb, g, gb_, lng, lngb, lnb, lnbb)
            S.dma("sp", lambda e, tt=tt: e.dma_start(out=XS[tt * 128:(tt + 1) * 128, :], in_=xt), reads=[xb], writes=[dbufs["XS"]])
            if last and not is_ctx_tile(tt):
                o0 = (tt - NTC) * 128
                S.dma("sp", lambda e, o0=o0: e.dma_start(out=OUT[o0:o0 + 128, :], in_=xt), reads=[xb], writes=[dbufs["OUT"]])

    def dump(dst_ap, src_ap, srcname):
        new_phase()
        n = src_ap.shape[1]
        t_, tb_ = ABF.alloc((n,), "dumpst")
        nr = src_ap.shape[0]
        S.dma("sp", lambda e: e.dma_start(out=t_[0:nr], in_=src_ap), reads=[dbufs[srcname]], writes=[tb_])
        S.dma("pool", lambda e: e.dma_start(out=dst_ap, in_=t_[0:nr]), reads=[tb_], writes=[dbufs["DBG"]])

    S.dma("sp", lambda e: e.dma_start(out=XS[0:T_CTX, :], in_=I["ctx"]), writes=[dbufs["XS"]])
    S.dma("sp", lambda e: e.dma_start(out=XS[T_CTX:T, :], in_=I["x"]), writes=[dbufs["XS"]])
    done = False
    if stop == "const":
        S.dma("sp", lambda e: e.dma_start(out=DBG[0:128, 0:128], in_=tril[:]), reads=[cb["tril"]], writes=[dbufs["DBG"]])
        done = True
    else:
        phase_adaln()
    if stop == "adaln":
        S.dma("sp", lambda e: e.dma_start(out=DBG[0:128, 0:192], in_=mod[:, 0].rearrange("p j r -> p (j r)")),
              reads=[cb["mod"]], writes=[dbufs["DBG"]])
        S.dma("sp", lambda e: e.dma_start(out=DBG[128:256, 0:64], in_=smallv[:, 0]), reads=[cb["smallv"]], writes=[dbufs["DBG"]])
        S.dma("sp", lambda e: e.dma_start(out=DBG[256:384, 0:8], in_=lamt[:, 0]), reads=[cb["lamt"]], writes=[dbufs["DBG"]])
        S.dma("sp", lambda e: e.dma_start(out=DBG[384:388, 0:D], in_=GROW[0].rearrange("a b d -> (a b) d")),
              reads=[dbufs["GROW"]], writes=[dbufs["DBG"]])
        done = True
    for l in range(0 if done else NL):
        r = phase_inproj(l)
        if stop == "uT":
            uT_, uTb_ = r
            for k in range(16):
                S.dma("pool", lambda e, k=k: e.dma_start(out=DBG[k * 128:(k + 1) * 128, 0:T], in_=uT_[:, k, :]),
                      reads=[uTb_], writes=[dbufs["DBG"]])
            done = True
            break
        if stop == "inproj":
            o = 0
            for nm, ap, rows in (("KD", KD[0], 128), ("KR", KR2, 128), ("CKG", CKG[1], 128), ("QD", QD[3], 128),
                                 ("GT", GT[5], 128), ("OP", OP[2], 128)):
                if nm in cfg.get("dumps", ("KD", "KR", "CKG", "QD", "GT", "OP")):
                    dump(DBG[o:o + rows, 0:T], ap, nm)
                o += rows
            if "VD" in cfg.get("dumps", ("VD",)):
                dump(DBG[o:o + 128, 0:516], VD[1], "VD")
            done = True
            break
        phase_upproj(l, *r)
        phase_attn(l)
        if stop == "attn":
            o = 0
            for nm, ap in (("OM", OM[0]), ("OM", OM[7]), ("OD", OD[0]), ("OD", OD[3]), ("KN", KN[2]), ("QN", QN[5]), ("QR", QR[3])):
                rows = ap.shape[0]
                dump(DBG[o:o + rows, 0:T], ap, nm)
                o += 128
            dump(DBG[o:o + 128, 0:1032], VM[1], "VM")
            done = True
            break
        phase_merge(l)
        phase_wo(l)
        if stop == "mixer":
            S.dma("sp", lambda e: e.dma_start(out=DBG[0:T, 0:D], in_=XS), reads=[dbufs["XS"]], writes=[dbufs["DBG"]])
            done = True
            break
        phase_route(l)
        if stop == "route":
            S.dma("sp", lambda e: e.dma_start(out=DBG[0:128, 0:NT * 2], in_=gatew[:].rearrange("p a b -> p (a b)")),
                  reads=[cb["gatew"]], writes=[dbufs["DBG"]])
            df, dfb = AF32.alloc((NT * 2,), "destf")
            S.op("dve", lambda e: e.tensor_copy(out=df, in_=dest_i[:].rearrange("p a b -> p (a b)")), reads=[cb["dest"]], writes=[dfb])
            S.dma("sp", lambda e: e.dma_start(out=DBG[128:256, 0:NT * 2], in_=df), reads=[dfb], writes=[dbufs["DBG"]])
            done = True
            break
        phase_experts(l)
        phase_combine(l, l == NL - 1)
        if stop == "layer" and l == 0:
            S.dma("sp", lambda e: e.dma_start(out=DBG[0:T, 0:D], in_=XS), reads=[dbufs["XS"]], writes=[dbufs["DBG"]])
            done = True
            break
    S.finish(list(dbufs.values()))
    barrier()
    S.emit()
    return nc, es, S


def host_consts(T_CTX, T_LAT, NE, CAP):
    T = T_CTX + T_LAT
    quarter = ROPE // 4
    inv_freq = (10000.0 ** (-np.arange(quarter, dtype=np.float32) / quarter)).astype(np.float32)
    rows = T_LAT // GRID_W
    row = np.repeat(np.arange(rows, dtype=np.float32), GRID_W)
    col = (np.arange(rows * GRID_W) % GRID_W).astype(np.float32)
    ang = np.stack([row[:, None] * inv_freq, col[:, None] * inv_freq], axis=1)
    cos, sin = np.cos(ang).astype(np.float32), np.sin(ang).astype(np.float32)
    c64 = np.ones((T, 2, 2, quarter), np.float32)
    s64 = np.zeros((T, 2, 2, quarter), np.float32)
    c64[T_CTX:] = cos[:, :, None, :]
    s64[T_CTX:] = sin[:, :, None, :]
    c64 = c64.reshape(T, 64).T
    s64 = s64.reshape(T, 64).T
    ropc = np.ascontiguousarray(np.concatenate([c64, c64], axis=0))
    rops = np.ascontiguousarray(np.concatenate([s64, s64], axis=0))
    rcnt = np.zeros((4, T), np.float32)
    for gi, w in enumerate(POOL_WINDOWS):
        half = w // 2
        for (s0, n) in ((0, T_CTX), (T_CTX, T_LAT)):
            t = np.arange(n)
            cnt = np.minimum(t + half, n) - np.maximum(t - half, 0)
            rcnt[gi, s0:s0 + n] = 1.0 / cnt.astype(np.float32)
    tril = (np.arange(128)[:, None] < np.arange(128)[None, :]).astype(np.float32)
    eoff = np.broadcast_to((np.arange(NE, dtype=np.float32) * CAP)[None, :], (128, NE)).copy()
    return dict(ident=np.eye(128, dtype=np.float32), ropc=ropc, rops=rops, rcnt=rcnt, tril=tril, eoff=eoff)


N_ACTIVE = 4
MOE_CAP = 384


def _per_core_inputs(b, x, c, ctx, c_ctx, weights, consts):
    m = dict(x=np.ascontiguousarray(x[b]), ctx=np.ascontiguousarray(ctx[b]),
             cvec=np.ascontiguousarray(np.stack([c[b], c_ctx])))
    m.update(weights)
    m.update(consts)
    return m


def kernel(x, c, ctx, c_ctx, w_ada, b_ada, w_in, b_gate, g_q, g_kv, w_uq, w_ukv, lam, g_sub, w_pool, pool_scale,
           w_br_mla, w_br_diff, w_br_pool, w_o, ln1_g, ln1_b, w_grp, b_grp, w_exp, b_exp, w_gu, w_dn, ln2_g, ln2_b):
    B, T_LAT, _ = x.shape
    T_CTX = ctx.shape[1]
    weights = dict(w_ada=w_ada, b_ada=b_ada, w_in=w_in, b_gate=np.reshape(b_gate, (DEPTH, 3 * D)), g_q=g_q, g_kv=g_kv,
                   w_uq=w_uq, w_ukv=w_ukv, lam=np.reshape(lam, (DEPTH, 4 * DIFF_HD)), g_sub=g_sub, w_pool=w_pool,
                   pool_scale=pool_scale, w_br_mla=w_br_mla, w_br_diff=w_br_diff, w_br_pool=w_br_pool, w_o=w_o,
                   ln1_g=ln1_g, ln1_b=ln1_b, w_grp=w_grp, b_grp=b_grp, w_exp=w_exp, b_exp=b_exp, w_gu=w_gu, w_dn=w_dn,
                   ln2_g=ln2_g, ln2_b=ln2_b)
    weights = {k: np.ascontiguousarray(np.asarray(v, np.float32)) for k, v in weights.items()}
    cfg = dict(T_CTX=T_CTX, T_LAT=T_LAT, NL=DEPTH, NG=N_GROUPS, CAP=MOE_CAP, stop="end")
    nc, es, S = build_program(cfg)
    consts = host_consts(T_CTX, T_LAT, N_EXPERTS, MOE_CAP)
    in_maps = [_per_core_inputs(b, x, c, ctx, c_ctx, weights, consts) for b in range(B)]
    res = run_bass_kernel_spmd(nc, in_maps, core_ids=list(range(N_ACTIVE)))
    return np.stack([res.results[b]["out"] for b in range(B)], axis=0).astype(np.float32)
```

```python
import math
import numpy as np
import ml_dtypes
import concourse.bass as bass
import concourse.mybir as mybir
from concourse.bass_utils import run_bass_kernel_spmd

F32 = mybir.dt.float32
BF16 = mybir.dt.bfloat16
I32 = mybir.dt.int32
ALU = mybir.AluOpType
AF = mybir.ActivationFunctionType
AX = mybir.AxisListType

D = 2048
DC = D // 128
DEPTH = 2
GRID_W = 64
MLA_HEADS = 8
Q_RANK = 768
KV_RANK = 512
NOPE = 128
ROPE = 64
MLA_V = 128
DIFF_HEADS = 4
DIFF_HD = 64
DIFF_V = 128
POOL_WINDOWS = (2, 4, 8, 16)
IN_COLS = 9536
N_GROUPS = 8
EPG = 8
N_EXPERTS = 64
D_EXPERT = 512
EPS = 1e-6
DN_ALPHA = (2 * DEPTH) ** 0.25
C_CKV, C_KROT, C_KDIFF, C_VDIFF, C_CQ, C_QDIFF, C_POOL, C_GATE = 0, 512, 576, 1088, 1600, 2368, 2880, 3392
BIG = 1.0e30


class Buf:
    __slots__ = ("w", "r", "name")

    def __init__(self, name=""):
        self.w = None
        self.r = {}
        self.name = name


class Sched:
    SEM_LIMIT = 30000
    ENGS = ("pe", "act", "dve", "pool", "sp")

    def __init__(self, nc, es):
        self.nc = nc
        self.es = es
        self.prog = {k: [] for k in self.ENGS}
        self.sem = {}
        self.cnt = {}
        self.waited = {k: {} for k in self.ENGS}
        self.last = {}
        self.init = {}
        self.pe_sems = set()
        self.nsem = 0
        for k in self.ENGS:
            self._new_sem(k)
        self.dq = {}
        for q in ("sp", "pool", "act"):
            sems = [self._alloc(f"dq_{q}_{i}") for i in range(6)]
            self.dq[q] = {"sems": sems, "cnt": [0] * len(sems), "i": 0}
        self.ninst = 0

    def _alloc(self, name):
        self.nsem += 1
        return self.es.enter_context(self.nc.semaphore(f"{name}_{self.nsem}"))

    def _new_sem(self, k):
        self.sem[k] = self._alloc(f"s_{k}")
        self.cnt[k] = 0
        if k == "pe":
            self.pe_sems.add(id(self.sem[k]))

    def _wait(self, k, tok):
        sem, val = tok
        sid = id(sem)
        if k == "pe" and sid in self.pe_sems:
            return
        w = self.waited[k]
        if w.get(sid, 0) >= val:
            return
        self.prog[k].append(("w", sem, val))
        w[sid] = val

    def _deps(self, k, reads, writes):
        for b in reads:
            if b.w is not None:
                self._wait(k, b.w)
        for b in writes:
            if b.w is not None:
                self._wait(k, b.w)
            for tok in b.r.values():
                self._wait(k, tok)

    def _commit(self, tok, reads, writes):
        sid = id(tok[0])
        for b in reads:
            b.r[sid] = tok
        for b in writes:
            b.w = tok
            b.r = {}

    def op(self, k, fn, reads=(), writes=(), sig=True):
        self._deps(k, reads, writes)
        if not sig:
            assert k == "pe"
            self.prog[k].append(("n", fn))
            tok = (self.sem[k], self.cnt[k] + 1)
            self._commit(tok, reads, writes)
            self.ninst += 1
            self.unsig = True
            return tok
        pending = k == "pe" and getattr(self, "unsig", False)
        if k == "pe":
            self.unsig = False
        if self.cnt[k] >= self.SEM_LIMIT and not pending:
            self._new_sem(k)
        self.cnt[k] += 1
        self.prog[k].append(("i", fn, self.sem[k], 1))
        tok = (self.sem[k], self.cnt[k])
        self.last[k] = tok
        self._commit(tok, reads, writes)
        self.ninst += 1
        return tok

    def dma(self, q, fn, reads=(), writes=()):
        self._deps(q, reads, writes)
        dq = self.dq[q]
        i = dq["i"]
        dq["i"] = (i + 1) % len(dq["sems"])
        sem = dq["sems"][i]
        if dq["cnt"][i] > 0:
            self._wait(q, (sem, dq["cnt"][i]))
        if dq["cnt"][i] >= self.SEM_LIMIT:
            sem = self._alloc(f"dq_{q}_{i}")
            dq["sems"][i] = sem
            dq["cnt"][i] = 0
        dq["cnt"][i] += 16
        self.prog[q].append(("i", fn, sem, 16))
        tok = (sem, dq["cnt"][i])
        self._commit(tok, reads, writes)
        self.ninst += 1
        return tok

    def finish(self, bufs):
        for b in bufs:
            if b.w is not None:
                self._wait("sp", b.w)

    def emit(self):
        def replay(k):
            def body(eng):
                for f in self.init.get(k, ()):
                    f(eng)
                for ent in self.prog[k]:
                    if ent[0] == "w":
                        eng.wait_ge(ent[1], ent[2])
                    elif ent[0] == "n":
                        ent[1](eng)
                    else:
                        ent[1](eng).then_inc(ent[2], ent[3])
            return body

        with self.nc.Block() as block:
            block.tensor(replay("pe"))
            block.scalar(replay("act"))
            block.vector(replay("dve"))
            block.gpsimd(replay("pool"))
            block.sync(replay("sp"))


class Arena:
    def __init__(self, tile, n):
        self.t = tile
        self.n = n
        self.off = 0

    def reset(self):
        self.off = 0

    def alloc(self, shape_free, name=""):
        n = int(np.prod(shape_free))
        n = (n + 15) // 16 * 16
        assert self.off + n <= self.n, f"arena overflow {name}: {self.off}+{n}>{self.n}"
        ap = self.t[:, self.off:self.off + int(np.prod(shape_free))]
        self.off += n
        if len(shape_free) == 2:
            ap = ap.rearrange("p (a b) -> p a b", b=shape_free[1])
        elif len(shape_free) == 3:
            ap = ap.rearrange("p (a b c) -> p a b c", b=shape_free[1], c=shape_free[2])
        return ap, Buf(name)


def token_blocks(t_ctx, t_lat, bw=512):
    blks = []
    for s0, n in ((0, t_ctx), (t_ctx, t_lat)):
        o = 0
        while o < n:
            w = min(bw, n - o)
            blks.append((s0 + o, w))
            o += w
    return blks


def build_program(cfg):
    T_CTX, T_LAT, NL = cfg["T_CTX"], cfg["T_LAT"], cfg["NL"]
    NG = cfg.get("NG", N_GROUPS)
    NE = NG * EPG
    CAP = cfg.get("CAP", 128)
    stop = cfg.get("stop", "end")
    T = T_CTX + T_LAT
    NT = T // 128
    NTC = T_CTX // 128
    BLKS = token_blocks(T_CTX, T_LAT)
    import contextlib
    es = contextlib.ExitStack()
    nc = bass.Bass("TRN2", target_bir_lowering=False)

    def din(name, shape, dt=F32):
        return nc.dram_tensor(name, list(shape), dt, kind="ExternalInput").ap()

    I = {}
    I["x"] = din("x", [T_LAT, D])
    I["ctx"] = din("ctx", [T_CTX, D])
    I["cvec"] = din("cvec", [2, D])
    L = DEPTH
    for nm, shp in (("w_ada", [L, D, 6 * D]), ("b_ada", [L, 6 * D]), ("w_in", [L, D, IN_COLS]), ("b_gate", [L, 3 * D]),
                    ("g_q", [L, Q_RANK]), ("g_kv", [L, KV_RANK]), ("w_uq", [L, Q_RANK, 1536]), ("w_ukv", [L, KV_RANK, 2048]),
                    ("lam", [L, 4 * DIFF_HD]), ("g_sub", [L, 128]), ("w_pool", [L, 4, 128, 128]), ("pool_scale", [L, 512]),
                    ("w_br_mla", [L, 1024, D]), ("w_br_diff", [L, 512, D]), ("w_br_pool", [L, 512, D]), ("w_o", [L, D, D]),
                    ("ln1_g", [L, D]), ("ln1_b", [L, D]), ("w_grp", [L, D, N_GROUPS]), ("b_grp", [L, N_GROUPS]),
                    ("w_exp", [L, D, N_EXPERTS]), ("b_exp", [L, N_EXPERTS]), ("w_gu", [L, NE, D, 1024]),
                    ("w_dn", [L, NE, 512, D]), ("ln2_g", [L, D]), ("ln2_b", [L, D])):
        I[nm] = din(nm, shp)
    I["ident"] = din("ident", [128, 128])
    I["ropc"] = din("ropc", [128, T])
    I["rops"] = din("rops", [128, T])
    I["rcnt"] = din("rcnt", [4, T])
    I["tril"] = din("tril", [128, 128])
    I["eoff"] = din("eoff", [128, NE])
    OUT = nc.dram_tensor("out", [T_LAT, D], F32, kind="ExternalOutput").ap()
    DBG = None
    if cfg.get("dbg"):
        DBG = nc.dram_tensor("dbg", list(cfg["dbg"]), F32, kind="ExternalOutput").ap()

    def dscr(name, shape, dt=BF16):
        return nc.dram_tensor(name, list(shape), dt).ap()

    XS = dscr("XS", [T, D], F32)
    GROW = dscr("GROW", [DEPTH, 2, 2, D], F32)
    KN = dscr("KN", [8, 128, T])
    KR2 = dscr("KR2", [128, T])
    CKG = dscr("CKG", [4, 128, T])
    CQG = dscr("CQG", [6, 128, T])
    VM = dscr("VM", [NT, 128, 8 * 129])
    QN = dscr("QN", [8, 128, T])
    QR = dscr("QR", [8, 64, T])
    KD = dscr("KD", [4, 128, T])
    QD = dscr("QD", [4, 128, T])
    VD = dscr("VD", [NT, 128, 4 * 129])
    OP = dscr("OP", [4, 128, T])
    GT = dscr("GT", [48, 128, T])
    OM = dscr("OM", [8, 128, T])
    OD = dscr("OD", [4, 128, T])
    MT = dscr("MT", [16, 128, T])
    XSORT = dscr("XSORT", [NE * CAP, D])
    YSORT = dscr("YSORT", [NE * CAP, D], F32)
    dbufs = {k: Buf(k) for k in ("XS", "GROW", "KN", "KR", "CKG", "CQG", "VM", "QN", "QR", "KD", "QD", "VD", "OP", "GT", "OM", "OD",
                                 "MT", "XSORT", "YSORT", "OUT", "DBG")}

    NBF = 58 * 1024
    NF32 = 14 * 1024 + 512
    abf_t = es.enter_context(nc.sbuf_tensor("abf", [128, NBF], BF16))
    af_t = es.enter_context(nc.sbuf_tensor("af32", [128, NF32], F32))
    ABF = Arena(abf_t, NBF)
    AF32 = Arena(af_t, NF32)
    ident = es.enter_context(nc.sbuf_tensor("identf", [128, 128], F32))
    identb = es.enter_context(nc.sbuf_tensor("identb", [128, 128], BF16))
    ones_f = es.enter_context(nc.sbuf_tensor("onesf", [128, 128], F32))
    ones_b = es.enter_context(nc.sbuf_tensor("onesb", [128, 128], BF16))
    tril = es.enter_context(nc.sbuf_tensor("trilf", [128, 128], F32))
    eoff = es.enter_context(nc.sbuf_tensor("eoff_sb", [128, NE], F32))
    zero_b = es.enter_context(nc.sbuf_tensor("zerob", [128, D], BF16))
    mod = es.enter_context(nc.sbuf_tensor("mod_sb", [128, DEPTH, 96, 2], F32))
    mod1 = es.enter_context(nc.sbuf_tensor("mod1", [128, DEPTH, 96, 2], F32))
    smallv = es.enter_context(nc.sbuf_tensor("smallv", [128, DEPTH, 64], F32))
    lamt = es.enter_context(nc.sbuf_tensor("lamt", [128, DEPTH, 8], F32))
    dest_i = es.enter_context(nc.sbuf_tensor("dest_i", [128, NT, 2], I32))
    gatew = es.enter_context(nc.sbuf_tensor("gatew", [128, NT, 2], F32))
    ebase = es.enter_context(nc.sbuf_tensor("ebase", [128, NE], F32))
    cb = {k: Buf(k) for k in ("ident", "identb", "ones", "tril", "eoff", "zero", "mod", "smallv", "lamt", "dest", "gatew", "ebase")}
    psum = [es.enter_context(nc.psum_tensor(f"ps{i}", [128, 512], F32)) for i in range(8)]
    pbuf = [Buf(f"ps{i}") for i in range(8)]
    S = Sched(nc, es)
    pctr = [0]

    def P(n=8, base=0):
        i = base + pctr[0] % n
        pctr[0] += 1
        return psum[i], pbuf[i]

    def barrier():
        toks = list(S.last.values())
        for q in S.dq.values():
            for sem, c in zip(q["sems"], q["cnt"]):
                if c > 0:
                    toks.append((sem, c))
        for k in S.ENGS:
            for t in toks:
                S._wait(k, t)

    def new_phase():
        barrier()
        ABF.reset()
        AF32.reset()

    S.dma("sp", lambda e: e.dma_start(out=ident[:], in_=I["ident"]), writes=[cb["ident"]])
    S.dma("sp", lambda e: e.dma_start(out=tril[:], in_=I["tril"]), writes=[cb["tril"]])
    S.dma("sp", lambda e: e.dma_start(out=eoff[:], in_=I["eoff"]), writes=[cb["eoff"]])
    S.op("dve", lambda e: e.tensor_copy(out=identb[:], in_=ident[:]), reads=[cb["ident"]], writes=[cb["identb"]])
    S.op("pool", lambda e: e.memset(ones_f[:], 1.0), writes=[cb["ones"]])
    S.op("pool", lambda e: e.memset(ones_b[:], 1.0), writes=[cb["ones"]])
    S.op("pool", lambda e: e.memset(zero_b[:], 0.0), writes=[cb["zero"]])

    def load_vec_fm(dst_ap, dst_buf, src2d, n):
        st, sb = AF32.alloc((128,), "vecst")
        S.dma("sp", lambda e: e.dma_start(out=st[0:n, :], in_=src2d), writes=[sb])
        ps, pb = P()
        S.op("pe", lambda e: e.transpose(ps[:, 0:n], st[0:n, :], ident[0:n, 0:n]), reads=[sb, cb["ident"]], writes=[pb])
        S.op("dve", lambda e: e.tensor_copy(out=dst_ap, in_=ps[:, 0:n]), reads=[pb], writes=[dst_buf])

    def phase_adaln():
        new_phase()
        cst, cstb = AF32.alloc((128,), "cst")
        sc, scb = AF32.alloc((32,), "sc")
        S.dma("sp", lambda e: e.dma_start(out=cst[0:32, :], in_=I["cvec"].rearrange("r (k p) -> (r k) p", p=128)), writes=[cstb])
        ps, pb = P()
        S.op("pe", lambda e: e.transpose(ps[:, 0:32], cst[0:32, :], ident[0:32, 0:32]), reads=[cstb, cb["ident"]], writes=[pb])
        S.op("act", lambda e: e.activation(out=sc, in_=ps[:, 0:32], func=AF.Silu), reads=[pb], writes=[scb])
        scv = sc.rearrange("p (r k) -> p k r", r=2)
        mark_ada = AF32.off
        for l in range(NL):
            barrier()
            AF32.off = mark_ada
            bfm, bfb = AF32.alloc((96,), "bada")
            load_vec_fm(bfm, bfb, I["b_ada"][l].rearrange("(n p) -> n p", p=128), 96)
            gsts = [AF32.alloc((256,), f"gst{i}") for i in range(2)]
            brs = [AF32.alloc((256,), f"br{i}") for i in range(2)]
            slabs = [AF32.alloc((16, 256), f"wada{i}") for i in range(2)]
            mps, mpb = psum[7], pbuf[7]
            for sl in range(6 * D // 256):
                w, wb = slabs[sl % 2]
                S.dma("sp", lambda e, w=w, sl=sl, l=l: e.dma_start(
                    out=w, in_=I["w_ada"][l][:, sl * 256:(sl + 1) * 256].rearrange("(k p) n -> p k n", p=128)), writes=[wb])
                for jj in range(2):
                    j = sl * 2 + jj
                    for k in range(16):
                        S.op("pe", lambda e, w=w, jj=jj, k=k, j=j: e.matmul(
                            mps[:, 2 * j:2 * j + 2], lhsT=w[:, k, jj * 128:(jj + 1) * 128], rhs=scv[:, k, :],
                            start=(k == 0), stop=(k == 15)), reads=[wb, scb], writes=[mpb])
                which = (sl * 256) // D
                if which in (2, 5):
                    wi = 0 if which == 2 else 1
                    c0 = sl * 256 - which * D
                    gps, gpb = P(4)
                    gi = (sl // 1) % 2
                    gst, gstb = gsts[gi]
                    br, brb = brs[gi]
                    S.dma("sp", lambda e, br=br, which=which, c0=c0, l=l: e.dma_start(
                        out=br[0:1, :], in_=I["b_ada"][l:l + 1, which * D + c0:which * D + c0 + 256]), writes=[brb])
                    for k in range(16):
                        S.op("pe", lambda e, w=w, k=k, gps=gps: e.matmul(
                            gps[0:2, 0:256], lhsT=scv[:, k, :], rhs=w[:, k, :], start=(k == 0), stop=False),
                            reads=[wb, scb], writes=[gpb])
                    S.op("pe", lambda e, gps=gps, br=br: e.matmul(
                        gps[0:2, 0:256], lhsT=ones_f[0:1, 0:2], rhs=br[0:1, :], start=False, stop=True),
                        reads=[brb, cb["ones"]], writes=[gpb])
                    S.op("dve", lambda e, gps=gps, gst=gst: e.tensor_copy(out=gst[0:2, :], in_=gps[0:2, 0:256]),
                         reads=[gpb], writes=[gstb])
                    S.dma("sp", lambda e, gst=gst, wi=wi, c0=c0, l=l: e.dma_start(
                        out=GROW[l, wi, :, c0:c0 + 256], in_=gst[0:2, :]), reads=[gstb], writes=[dbufs["GROW"]])
            for r in range(2):
                S.op("dve", lambda e, l=l, r=r: e.tensor_tensor(
                    out=mod[:, l, :, r], in0=mps[:, 0:192].rearrange("p (j r) -> p j r", r=2)[:, :, r],
                    in1=bfm, op=ALU.add), reads=[mpb, bfb], writes=[cb["mod"]])
            S.op("dve", lambda e, l=l: e.tensor_scalar_add(out=mod1[:, l], in0=mod[:, l], scalar1=1.0),
                 reads=[cb["mod"]], writes=[cb["mod"]])
            load_vec_fm(smallv[:, l, 0:48], cb["smallv"], I["b_gate"][l].rearrange("(n p) -> n p", p=128), 48)
            load_vec_fm(smallv[:, l, 48:54], cb["smallv"], I["g_q"][l].rearrange("(n p) -> n p", p=128), 6)
            load_vec_fm(smallv[:, l, 54:58], cb["smallv"], I["g_kv"][l].rearrange("(n p) -> n p", p=128), 4)
            load_vec_fm(smallv[:, l, 58:62], cb["smallv"], I["pool_scale"][l].rearrange("(n p) -> n p", p=128), 4)
            load_vec_fm(smallv[:, l, 62:63], cb["smallv"], I["g_sub"][l].rearrange("(n p) -> n p", p=128), 1)
            lt, ltb = AF32.alloc((4 * DIFF_HD,), "lamin")
            S.dma("sp", lambda e, l=l: e.dma_start(out=lt, in_=I["lam"][l].partition_broadcast(128)), writes=[ltb])
            lp, lpb = AF32.alloc((2, DIFF_HD), "lamp")
            ltv = lt.rearrange("p (a b d) -> p a b d", a=2, b=2)
            S.op("dve", lambda e: e.tensor_tensor(out=lp, in0=ltv[:, :, 0, :], in1=ltv[:, :, 1, :], op=ALU.mult),
                 reads=[ltb], writes=[lpb])
            S.op("dve", lambda e, l=l: e.tensor_reduce(out=lamt[:, l, 0:2], in_=lp, axis=AX.X, op=ALU.add),
                 reads=[lpb], writes=[cb["lamt"]])
            S.op("act", lambda e, l=l: e.activation(out=lamt[:, l, 2:4], in_=lamt[:, l, 0:2], func=AF.Exp),
                 reads=[cb["lamt"]], writes=[cb["lamt"]])
            lam_init = 0.8 - 0.6 * math.exp(-0.3 * l)
            S.op("dve", lambda e, l=l: e.tensor_tensor(out=lamt[:, l, 4:5], in0=lamt[:, l, 3:4], in1=lamt[:, l, 2:3],
                                                       op=ALU.subtract), reads=[cb["lamt"]], writes=[cb["lamt"]])
            S.op("dve", lambda e, l=l, li=lam_init: e.tensor_scalar_add(out=lamt[:, l, 5:6], in0=lamt[:, l, 4:5], scalar1=-li),
                 reads=[cb["lamt"]], writes=[cb["lamt"]])
            AF32.off = 0 if False else AF32.off

    def ln_tile(xt, xb, st, stb):
        S.op("dve", lambda e: e.tensor_reduce(out=st[:, 0:1], in_=xt, axis=AX.X, op=ALU.add), reads=[xb], writes=[stb])
        S.op("dve", lambda e: e.tensor_scalar(out=st[:, 1:2], in0=st[:, 0:1], scalar1=-1.0 / D, scalar2=None, op0=ALU.mult),
             reads=[stb], writes=[stb])
        S.op("dve", lambda e: e.tensor_scalar(out=st[:, 0:1], in0=st[:, 0:1], scalar1=1.0 / D, scalar2=None, op0=ALU.mult),
             reads=[stb], writes=[stb])

    def ln_finish(sq, sqb, st, stb):
        S.op("dve", lambda e: e.tensor_reduce(out=st[:, 2:3], in_=sq, axis=AX.X, op=ALU.add), reads=[sqb], writes=[stb])
        S.op("dve", lambda e: e.tensor_scalar(out=st[:, 2:3], in0=st[:, 2:3], scalar1=1.0 / D, scalar2=EPS, op0=ALU.mult,
                                              op1=ALU.add), reads=[stb], writes=[stb])
        S.op("act", lambda e: e.activation(out=st[:, 3:4], in_=st[:, 2:3], func=AF.Sqrt), reads=[stb], writes=[stb])
        S.op("dve", lambda e: e.reciprocal(out=st[:, 2:3], in_=st[:, 3:4]), reads=[stb], writes=[stb])

    def layernorm(xt, xb, xn, xnb, sq, sqb, st, stb):
        ln_tile(xt, xb, st, stb)
        S.op("act", lambda e: e.activation(out=sq, in_=xt, func=AF.Square, bias=st[:, 1:2], scale=1.0),
             reads=[xb, stb], writes=[sqb])
        ln_finish(sq, sqb, st, stb)
        S.op("dve", lambda e: e.tensor_scalar(out=xn, in0=xt, scalar1=st[:, 0:1], scalar2=st[:, 2:3], op0=ALU.subtract,
                                              op1=ALU.mult), reads=[xb, stb], writes=[xnb])

    def is_ctx_tile(tt):
        return tt < NTC

    def load_w_chunk(dst, dstb, src_cols_ap, q="pool"):
        S.dma(q, lambda e: e.dma_start(out=dst, in_=src_cols_ap.rearrange("(k p) n -> p k n", p=128)), writes=[dstb])

    def make_rot_w(w, wb, wp, wpb):
        wv = w.rearrange("p k (g h j) -> p (k g) h j", h=2, j=16)
        wpv = wp.rearrange("p k (g h j) -> p (k g) h j", h=2, j=16)
        S.op("pool", lambda e: e.tensor_scalar(out=wpv[:, :, 0, :], in0=wv[:, :, 1, :], scalar1=-1.0, scalar2=None,
                                               op0=ALU.mult), reads=[wb], writes=[wpb])
        S.op("pool", lambda e: e.tensor_copy(out=wpv[:, :, 1, :], in_=wv[:, :, 0, :]), reads=[wb], writes=[wpb])

    def phase_inproj(l):
        new_phase()
        uT, uTb = ABF.alloc((16, T), "uT")
        ssq_kv, ssq_kvb = AF32.alloc((T,), "ssq_kv")
        ssq_q, ssq_qb = AF32.alloc((T,), "ssq_q")
        mark = AF32.off
        xts = [AF32.alloc((D,), f"xt{i}") for i in range(2)]
        xn, xnb = AF32.alloc((D,), "xn")
        sq, sqb = AF32.alloc((D,), "sq")
        st, stb = AF32.alloc((4,), "st")
        for tt in range(NT):
            xt, xb = xts[tt % 2]
            S.dma("sp", lambda e, xt=xt, tt=tt: e.dma_start(out=xt, in_=XS[tt * 128:(tt + 1) * 128, :]),
                  reads=[dbufs["XS"]], writes=[xb])
            layernorm(xt, xb, xn, xnb, sq, sqb, st, stb)
            r = 1 if is_ctx_tile(tt) else 0
            for k in range(16):
                if k % 4 == 0:
                    ps, pb = P()
                S.op("pe", lambda e, ps=ps, k=k: e.transpose(ps[:, (k % 4) * 128:(k % 4 + 1) * 128],
                                                             xn[:, k * 128:(k + 1) * 128], ident[:]),
                     reads=[xnb, cb["ident"]], writes=[pb])
                S.op("act", lambda e, ps=ps, k=k, tt=tt, r=r: e.activation(
                    out=uT[:, k, tt * 128:(tt + 1) * 128], in_=ps[:, (k % 4) * 128:(k % 4 + 1) * 128], func=AF.Identity,
                    scale=mod1[:, l, 16 + k, r:r + 1], bias=mod[:, l, k, r:r + 1]), reads=[pb, cb["mod"]], writes=[uTb])
        AF32.off = mark
        if stop == "uT":
            return uT, uTb
        barrier()
        ropc, ropcb = AF32.alloc((T,), "ropc")
        rops, ropsb = AF32.alloc((T,), "rops")
        S.dma("sp", lambda e: e.dma_start(out=ropc, in_=I["ropc"]), writes=[ropcb])
        S.dma("sp", lambda e: e.dma_start(out=rops, in_=I["rops"]), writes=[ropsb])
        wch = [ABF.alloc((16, 128), f"wch{i}") for i in range(2)]
        wchp = [ABF.alloc((16, 128), f"wchp{i}") for i in range(2)]
        stg = [ABF.alloc((512,), f"stg{i}") for i in range(3)]
        sqs = [AF32.alloc((512,), f"sqs{i}") for i in range(2)]
        tmp = [AF32.alloc((512,), f"tmpa{i}") for i in range(2)]
        ctr = [0, 0, 0]

        def proj_chunk(c0, ncols, evac, rot=False, dup=False):
            if ctr[0] >= cfg.get("m2cut", 10 ** 9):
                return
            i = ctr[0] % 2
            ctr[0] += 1
            w, wb = wch[i]
            nw = ncols * (2 if dup else 1)
            load_w_chunk(w[:, :, 0:ncols], wb, I["w_in"][l][:, c0:c0 + ncols])
            if dup:
                load_w_chunk(w[:, :, ncols:2 * ncols], wb, I["w_in"][l][:, c0:c0 + ncols])
            if rot:
                wp, wpb = wchp[i]
                make_rot_w(w[:, :, 0:nw], wb, wp[:, :, 0:nw], wpb)
            for (b0, bw) in BLKS:
                ps, pb = P(6)
                for k in range(16):
                    S.op("pe", lambda e, ps=ps, k=k, b0=b0, bw=bw: e.matmul(
                        ps[0:nw, 0:bw], lhsT=w[:, k, 0:nw], rhs=uT[:, k, b0:b0 + bw], start=(k == 0), stop=(k == 15)),
                        reads=[wb, uTb], writes=[pb], sig=(k == 15))
                ps2, pb2 = None, None
                if rot:
                    ps2, pb2 = P(6)
                    for k in range(16):
                        S.op("pe", lambda e, ps2=ps2, k=k, b0=b0, bw=bw: e.matmul(
                            ps2[0:nw, 0:bw], lhsT=wp[:, k, 0:nw], rhs=uT[:, k, b0:b0 + bw], start=(k == 0), stop=(k == 15)),
                            reads=[wpb, uTb], writes=[pb2], sig=(k == 15))
                evac(ps, pb, ps2, pb2, b0, bw, nw)

        def next_stg():
            s = stg[ctr[1] % 3]
            ctr[1] += 1
            return s

        def evac_rope(dram_rows, dname):
            def f(ps, pb, ps2, pb2, b0, bw, nw):
                t1, t1b = tmp[0]
                t2, t2b = tmp[1]
                sg, sgb = next_stg()
                S.op("dve", lambda e: e.tensor_tensor(out=t1[0:nw, 0:bw], in0=ps[0:nw, 0:bw], in1=ropc[0:nw, b0:b0 + bw],
                                                      op=ALU.mult), reads=[pb, ropcb], writes=[t1b])
                S.op("dve", lambda e: e.tensor_tensor(out=t2[0:nw, 0:bw], in0=ps2[0:nw, 0:bw], in1=rops[0:nw, b0:b0 + bw],
                                                      op=ALU.mult), reads=[pb2, ropsb], writes=[t2b])
                S.op("pool", lambda e: e.tensor_tensor(out=sg[0:nw, 0:bw], in0=t1[0:nw, 0:bw], in1=t2[0:nw, 0:bw],
                                                       op=ALU.add), reads=[t1b, t2b], writes=[sgb])
                S.dma("sp", lambda e: e.dma_start(out=dram_rows[0:nw, b0:b0 + bw], in_=sg[0:nw, 0:bw]), reads=[sgb],
                      writes=[dbufs[dname]])
            return f

        def evac_rms(dram_rows, dname, gcol, ssq, ssqb, first):
            def f(ps, pb, ps2, pb2, b0, bw, nw):
                em = cfg.get("evacmode", 9)
                if em < 1:
                    return
                sg, sgb = next_stg()
                s2, s2b = sqs[ctr[2] % 2]
                ctr[2] += 1
                S.op("act", lambda e: e.activation(out=s2[:, 0:bw], in_=ps[:, 0:bw], func=AF.Square), reads=[pb], writes=[s2b])
                if em < 2:
                    return
                S.op("act", lambda e: e.activation(out=sg[:, 0:bw], in_=ps[:, 0:bw], func=AF.Identity,
                                                   scale=smallv[:, l, gcol:gcol + 1]), reads=[pb, cb["smallv"]], writes=[sgb])
                S.dma("sp", lambda e: e.dma_start(out=dram_rows[:, b0:b0 + bw], in_=sg[:, 0:bw]), reads=[sgb],
                      writes=[dbufs[dname]])
                if em < 3:
                    return
                pq, pqb = P(2, 6)
                S.op("pe", lambda e: e.matmul(pq[:, 0:bw], lhsT=ones_f[:], rhs=s2[:, 0:bw], start=True, stop=True),
                     reads=[s2b, cb["ones"]], writes=[pqb])
                if first:
                    S.op("dve", lambda e: e.tensor_copy(out=ssq[:, b0:b0 + bw], in_=pq[:, 0:bw]), reads=[pqb], writes=[ssqb])
                else:
                    S.op("dve", lambda e: e.tensor_tensor(out=ssq[:, b0:b0 + bw], in0=ssq[:, b0:b0 + bw], in1=pq[:, 0:bw],
                                                          op=ALU.add), reads=[pqb, ssqb], writes=[ssqb])
            return f

        for kc in range(4):
            proj_chunk(C_CKV + kc * 128, 128, evac_rms(CKG[kc], "CKG", 54 + kc, ssq_kv, ssq_kvb, kc == 0))
        proj_chunk(C_KROT, 64, evac_rope(KR2, "KR"), rot=True, dup=True)
        for h in range(4):
            proj_chunk(C_KDIFF + h * 128, 128, evac_rope(KD[h], "KD"), rot=True)
        for kc in range(6):
            proj_chunk(C_CQ + kc * 128, 128, evac_rms(CQG[kc], "CQG", 48 + kc, ssq_q, ssq_qb, kc == 0))
        for h in range(4):
            proj_chunk(C_QDIFF + h * 128, 128, evac_rope(QD[h], "QD"), rot=True)

        def evac_gate(j):
            def f(ps, pb, ps2, pb2, b0, bw, nw):
                sg, sgb = next_stg()
                S.op("act", lambda e: e.activation(out=sg[:, 0:bw], in_=ps[:, 0:bw], func=AF.Sigmoid,
                                                   bias=smallv[:, l, j:j + 1], scale=1.0), reads=[pb, cb["smallv"]], writes=[sgb])
                S.dma("sp", lambda e: e.dma_start(out=GT[j][:, b0:b0 + bw], in_=sg[:, 0:bw]), reads=[sgb], writes=[dbufs["GT"]])
            return f
        for j in range(48):
            proj_chunk(C_GATE + j * 128, 128, evac_gate(j))

        if "m2cut" in cfg:
            return ssq_kv, ssq_kvb, ssq_q, ssq_qb, mark
        wv, wvb = ABF.alloc((16, 512), "wvd")
        load_w_chunk(wv, wvb, I["w_in"][l][:, C_VDIFF:C_VDIFF + 512])
        vst = [ABF.alloc((4, 129), f"vst{i}") for i in range(2)]
        for i in range(2):
            S.op("pool", lambda e, i=i: e.memset(vst[i][0], 1.0), writes=[vst[i][1]])
        for tt in range(NT):
            ps, pb = P(6)
            for k in range(16):
                S.op("pe", lambda e, ps=ps, k=k, tt=tt: e.matmul(ps[:, 0:512], lhsT=uT[:, k, tt * 128:(tt + 1) * 128],
                                                                 rhs=wv[:, k, :], start=(k == 0), stop=(k == 15)),
                     reads=[uTb, wvb], writes=[pb])
            v, vb = vst[tt % 2]
            S.op("act", lambda e, ps=ps, v=v: e.activation(out=v[:, :, 0:128], in_=ps[:, 0:512].rearrange("p (h d) -> p h d", d=128),
                                                           func=AF.Copy), reads=[pb], writes=[vb])
            S.dma("sp", lambda e, v=v, tt=tt: e.dma_start(out=VD[tt], in_=v.rearrange("p h d -> p (h d)")), reads=[vb],
                  writes=[dbufs["VD"]])

        barrier()
        AF32.off = mark
        PADW = 16
        TP = T + 4 * PADW
        xp, xpb = AF32.alloc((TP,), "xp")
        b1, b1b = AF32.alloc((TP,), "b1")
        rc, rcb = AF32.alloc((T,), "rc")
        pooled, pooledb = ABF.alloc((T,), "pooled")
        wpl, wplb = ABF.alloc((128,), "wpool")

        def segs():
            return ((PADW, 0, T_CTX), (3 * PADW + T_CTX, T_CTX, T_LAT))
        for gi, wwin in enumerate(POOL_WINDOWS):
            half = wwin // 2
            S.op("pool", lambda e: e.memset(xp, 0.0), writes=[xpb])
            S.dma("sp", lambda e, gi=gi: e.dma_start(out=rc, in_=I["rcnt"][gi:gi + 1, :].partition_broadcast(128)), writes=[rcb])
            S.dma("pool", lambda e, gi=gi: e.dma_start(out=wpl, in_=I["w_pool"][l, gi]), writes=[wplb])

            def evac_pool(ps, pb, ps2, pb2, b0, bw, nw):
                po = PADW + b0 if b0 < T_CTX else 3 * PADW + b0
                S.op("act", lambda e: e.activation(out=xp[:, po:po + bw], in_=ps[:, 0:bw], func=AF.Copy), reads=[pb], writes=[xpb])
            proj_chunk(C_POOL + gi * 128, 128, evac_pool)
            src, srcb, dst, dstb = xp, xpb, b1, b1b
            m = 1
            first = True
            while m < wwin:
                if first:
                    S.op("dve", lambda e, m=m: e.tensor_tensor(out=b1[:, 0:TP - m], in0=xp[:, 0:TP - m], in1=xp[:, m:TP],
                                                               op=ALU.add), reads=[xpb], writes=[b1b])
                    first = False
                else:
                    S.op("dve", lambda e, m=m: e.tensor_tensor(out=b1[:, 0:TP - m], in0=b1[:, 0:TP - m], in1=b1[:, m:TP],
                                                               op=ALU.add), reads=[b1b], writes=[b1b])
                m *= 2
            for (po, t0, n) in segs():
                S.op("dve", lambda e, po=po, t0=t0, n=n, half=half: e.tensor_tensor(
                    out=b1[:, po - half:po - half + n], in0=b1[:, po - half:po - half + n], in1=rc[:, t0:t0 + n], op=ALU.mult),
                    reads=[b1b, rcb], writes=[b1b])
                S.op("dve", lambda e, po=po, t0=t0, n=n, half=half: e.tensor_tensor(
                    out=pooled[:, t0:t0 + n], in0=b1[:, po - half:po - half + n], in1=xp[:, po:po + n], op=ALU.subtract),
                    reads=[b1b, xpb], writes=[pooledb])
            for (b0, bw) in BLKS:
                ps, pb = P(6)
                S.op("pe", lambda e, ps=ps, b0=b0, bw=bw: e.matmul(ps[:, 0:bw], lhsT=wpl, rhs=pooled[:, b0:b0 + bw], start=True,
                                                                   stop=True), reads=[wplb, pooledb], writes=[pb])
                sg, sgb = next_stg()
                S.op("act", lambda e, ps=ps, sg=sg, bw=bw, gi=gi: e.activation(
                    out=sg[:, 0:bw], in_=ps[:, 0:bw], func=AF.Identity, scale=smallv[:, l, 58 + gi:59 + gi]),
                    reads=[pb, cb["smallv"]], writes=[sgb])
                S.dma("sp", lambda e, sg=sg, b0=b0, bw=bw, gi=gi: e.dma_start(out=OP[gi][:, b0:b0 + bw], in_=sg[:, 0:bw]),
                      reads=[sgb], writes=[dbufs["OP"]])
        return ssq_kv, ssq_kvb, ssq_q, ssq_qb, mark

    def rstd_inplace(ssq, ssqb, n, tmp, tmpb):
        S.op("dve", lambda e: e.tensor_scalar(out=ssq, in0=ssq, scalar1=1.0 / n, scalar2=EPS, op0=ALU.mult, op1=ALU.add),
             reads=[ssqb], writes=[ssqb])
        S.op("act", lambda e: e.activation(out=tmp, in_=ssq, func=AF.Sqrt), reads=[ssqb], writes=[tmpb])
        S.op("dve", lambda e: e.reciprocal(out=ssq, in_=tmp), reads=[tmpb], writes=[ssqb])

    def phase_upproj(l, ssq_kv, ssq_kvb, ssq_q, ssq_qb, mark):
        barrier()
        ABF.reset()
        AF32.off = mark
        tmpT, tmpTb = AF32.alloc((T,), "tmpT")
        rstd_inplace(ssq_kv, ssq_kvb, KV_RANK, tmpT, tmpTb)
        rstd_inplace(ssq_q, ssq_qb, Q_RANK, tmpT, tmpTb)
        rtm, rtmb = AF32.alloc((NT,), "rtm")
        ps, pb = P()
        for tt in range(NT):
            S.op("pe", lambda e, tt=tt: e.transpose(ps[:, tt:tt + 1], ssq_kv[0:1, tt * 128:(tt + 1) * 128], ident[0:1, 0:1]),
                 reads=[ssq_kvb, cb["ident"]], writes=[pb])
        S.op("dve", lambda e: e.tensor_copy(out=rtm, in_=ps[:, 0:NT]), reads=[pb], writes=[rtmb])
        ropc, ropcb = AF32.alloc((T,), "ropc")
        rops, ropsb = AF32.alloc((T,), "rops")
        S.dma("sp", lambda e: e.dma_start(out=ropc, in_=I["ropc"]), writes=[ropcb])
        S.dma("sp", lambda e: e.dma_start(out=rops, in_=I["rops"]), writes=[ropsb])
        t1, t1b = AF32.alloc((512,), "t1")
        t2, t2b = AF32.alloc((512,), "t2")
        ckg, ckgb = ABF.alloc((4, T), "ckg")
        cqg, cqgb = ABF.alloc((6, T), "cqg")
        S.dma("sp", lambda e: e.dma_start(out=ckg, in_=CKG.rearrange("k p t -> p k t")), reads=[dbufs["CKG"]], writes=[ckgb])
        S.dma("sp", lambda e: e.dma_start(out=cqg, in_=CQG.rearrange("k p t -> p k t")), reads=[dbufs["CQG"]], writes=[cqgb])
        wcs = [ABF.alloc((6, 128), f"wc{i}") for i in range(2)]
        wcp, wcpb = ABF.alloc((6, 128), "wcp")
        stg = [ABF.alloc((512,), f"stgu{i}") for i in range(3)]
        wv, wvb = ABF.alloc((4, 1024), "wv")
        vst = [ABF.alloc((8, 129), f"vstm{i}") for i in range(2)]
        ctr = [0, 0]

        def nstg():
            s = stg[ctr[1] % 3]
            ctr[1] += 1
            return s

        def nw():
            s = wcs[ctr[0] % 2]
            ctr[0] += 1
            return s
        for h in range(8):
            w, wb = nw()
            load_w_chunk(w[:, 0:4, :], wb, I["w_ukv"][l][:, h * 256:h * 256 + 128])
            for (b0, bw) in BLKS:
                ps, pb = P(6)
                for k in range(4):
                    S.op("pe", lambda e, ps=ps, k=k, b0=b0, bw=bw, w=w: e.matmul(
                        ps[:, 0:bw], lhsT=w[:, k, :], rhs=ckg[:, k, b0:b0 + bw], start=(k == 0), stop=(k == 3)),
                        reads=[wb, ckgb], writes=[pb])
                sg, sgb = nstg()
                S.op("dve", lambda e, ps=ps, sg=sg, b0=b0, bw=bw: e.tensor_tensor(
                    out=sg[:, 0:bw], in0=ps[:, 0:bw], in1=ssq_kv[:, b0:b0 + bw], op=ALU.mult), reads=[pb, ssq_kvb], writes=[sgb])
                S.dma("sp", lambda e, sg=sg, b0=b0, bw=bw, h=h: e.dma_start(out=KN[h][:, b0:b0 + bw], in_=sg[:, 0:bw]),
                      reads=[sgb], writes=[dbufs["KN"]])
            w, wb = nw()
            load_w_chunk(w, wb, I["w_uq"][l][:, h * 192:h * 192 + 128])
            for (b0, bw) in BLKS:
                ps, pb = P(6)
                for k in range(6):
                    S.op("pe", lambda e, ps=ps, k=k, b0=b0, bw=bw, w=w: e.matmul(
                        ps[:, 0:bw], lhsT=w[:, k, :], rhs=cqg[:, k, b0:b0 + bw], start=(k == 0), stop=(k == 5)),
                        reads=[wb, cqgb], writes=[pb])
                sg, sgb = nstg()
                S.op("dve", lambda e, ps=ps, sg=sg, b0=b0, bw=bw: e.tensor_tensor(
                    out=sg[:, 0:bw], in0=ps[:, 0:bw], in1=ssq_q[:, b0:b0 + bw], op=ALU.mult), reads=[pb, ssq_qb], writes=[sgb])
                S.dma("sp", lambda e, sg=sg, b0=b0, bw=bw, h=h: e.dma_start(out=QN[h][:, b0:b0 + bw], in_=sg[:, 0:bw]),
                      reads=[sgb], writes=[dbufs["QN"]])
            load_w_chunk(wv[:, :, h * 128:(h + 1) * 128], wvb, I["w_ukv"][l][:, h * 256 + 128:h * 256 + 256])
        for hp in range(4):
            w, wb = nw()
            for i in range(2):
                hh = 2 * hp + i
                load_w_chunk(w[:, :, i * 64:(i + 1) * 64], wb, I["w_uq"][l][:, hh * 192 + 128:hh * 192 + 192])
            make_rot_w(w, wb, wcp, wcpb)
            for (b0, bw) in BLKS:
                ps, pb = P(6)
                ps2, pb2 = P(6)
                for k in range(6):
                    S.op("pe", lambda e, ps=ps, k=k, b0=b0, bw=bw, w=w: e.matmul(
                        ps[:, 0:bw], lhsT=w[:, k, :], rhs=cqg[:, k, b0:b0 + bw], start=(k == 0), stop=(k == 5)),
                        reads=[wb, cqgb], writes=[pb])
                for k in range(6):
                    S.op("pe", lambda e, ps2=ps2, k=k, b0=b0, bw=bw: e.matmul(
                        ps2[:, 0:bw], lhsT=wcp[:, k, :], rhs=cqg[:, k, b0:b0 + bw], start=(k == 0), stop=(k == 5)),
                        reads=[wcpb, cqgb], writes=[pb2])
                sg, sgb = nstg()
                S.op("dve", lambda e, ps=ps, b0=b0, bw=bw: e.tensor_tensor(out=t1[:, 0:bw], in0=ps[:, 0:bw], in1=ropc[:, b0:b0 + bw],
                                                                        op=ALU.mult), reads=[pb, ropcb], writes=[t1b])
                S.op("dve", lambda e, ps2=ps2, b0=b0, bw=bw: e.tensor_tensor(out=t2[:, 0:bw], in0=ps2[:, 0:bw], in1=rops[:, b0:b0 + bw],
                                                                         op=ALU.mult), reads=[pb2, ropsb], writes=[t2b])
                S.op("pool", lambda e, bw=bw: e.tensor_tensor(out=t1[:, 0:bw], in0=t1[:, 0:bw], in1=t2[:, 0:bw], op=ALU.add),
                     reads=[t1b, t2b], writes=[t1b])
                S.op("dve", lambda e, sg=sg, b0=b0, bw=bw: e.tensor_tensor(out=sg[:, 0:bw], in0=t1[:, 0:bw], in1=ssq_q[:, b0:b0 + bw],
                                                                        op=ALU.mult), reads=[t1b, ssq_qb], writes=[sgb])
                for i in range(2):
                    S.dma("sp", lambda e, sg=sg, b0=b0, bw=bw, i=i, hp=hp: e.dma_start(
                        out=QR[2 * hp + i][:, b0:b0 + bw], in_=sg[i * 64:(i + 1) * 64, 0:bw]), reads=[sgb], writes=[dbufs["QR"]])
        for i in range(2):
            S.op("pool", lambda e, i=i: e.memset(vst[i][0], 1.0), writes=[vst[i][1]])
        for tt in range(NT):
            v, vb = vst[tt % 2]
            for half in range(2):
                ps, pb = P(6)
                for k in range(4):
                    S.op("pe", lambda e, ps=ps, k=k, tt=tt, half=half: e.matmul(
                        ps[:, 0:512], lhsT=ckg[:, k, tt * 128:(tt + 1) * 128], rhs=wv[:, k, half * 512:(half + 1) * 512],
                        start=(k == 0), stop=(k == 3)), reads=[ckgb, wvb], writes=[pb])
                S.op("act", lambda e, ps=ps, v=v, half=half, tt=tt: e.activation(
                    out=v[:, half * 4:(half + 1) * 4, 0:128], in_=ps[:, 0:512].rearrange("p (h d) -> p h d", d=128),
                    func=AF.Identity, scale=rtm[:, tt:tt + 1]), reads=[pb, rtmb], writes=[vb])
            S.dma("sp", lambda e, v=v, tt=tt: e.dma_start(out=VM[tt], in_=v.rearrange("p h d -> p (h d)")), reads=[vb],
                  writes=[dbufs["VM"]])

    def phase_attn(l):
        new_phase()
        kr, krb = ABF.alloc((T,), "kr")
        S.dma("sp", lambda e: e.dma_start(out=kr[0:64, :], in_=KR2[0:64, :]), reads=[dbufs["KR"]], writes=[krb])
        ops = [ABF.alloc((T,), f"kq{i}") for i in range(8)]
        vhs = [ABF.alloc((NT, 129), f"vh{i}") for i in range(2)]
        est = [ABF.alloc((512,), f"est{i}") for i in range(4)]
        ostg = [ABF.alloc((512,), f"ostg{i}") for i in range(2)]
        on = [AF32.alloc((128,), f"on{i}") for i in range(4)]
        ona = [AF32.alloc((4, 128), f"ona{i}") for i in range(2)]
        rc, rcb = AF32.alloc((8,), "rc")
        sqd, sqdb = AF32.alloc((128,), "sqd")
        gs, gsb = AF32.alloc((2,), "gs")
        lam_init = 0.8 - 0.6 * math.exp(-0.3 * l)
        S.op("dve", lambda e: e.tensor_scalar(out=gs[:, 0:1], in0=smallv[:, l, 62:63], scalar1=1.0 - lam_init, scalar2=None,
                                              op0=ALU.mult), reads=[cb["smallv"]], writes=[gsb])
        qblocks = []
        for (b0, bw) in BLKS:
            qblocks.append((b0, bw, NTC if b0 < T_CTX else NT))
        ectr = [0]

        rcp, rcpb = AF32.alloc((512,), "rcp")
        o0, o0b = AF32.alloc((512,), "o0f")
        o1, o1b = AF32.alloc((512,), "o1f")
        sq5, sq5b = AF32.alloc((512,), "sq5")
        t5, t5b = AF32.alloc((512,), "t5")
        OB, SB, QB = 4, 5, 6

        accs = [AF32.alloc((512,), "accA") + ("dve",), AF32.alloc((512,), "accB") + ("pool",)]

        def attend(kpart, qpart, vh, vhb, scale, q0, qw, nkt):
            def emit_pv(kti, es_, esb):
                S.op("pe", lambda e: e.matmul(psum[OB][:, 0:qw], lhsT=vh[:, kti, 0:128], rhs=es_[:, 0:qw],
                                              start=(kti == 0), stop=(kti == nkt - 1)),
                     reads=[esb, vhb], writes=[pbuf[OB]], sig=True)
            prev = None
            n = len(kpart)
            for kti in range(nkt):
                ps, pb = P(3)
                for i, ((ka, kb_), (qa, qb_)) in enumerate(zip(kpart, qpart)):
                    S.op("pe", lambda e, ps=ps, ka=ka, qa=qa, i=i, kti=kti: e.matmul(
                        ps[:, 0:qw], lhsT=ka[:, kti * 128:(kti + 1) * 128], rhs=qa[:, q0:q0 + qw], start=(i == 0), stop=(i == n - 1)),
                        reads=[kb_, qb_], writes=[pb], sig=(i == n - 1))
                if prev is not None:
                    emit_pv(*prev)
                es_, esb = est[ectr[0] % 4]
                ectr[0] += 1
                S.op("act", lambda e, ps=ps, es_=es_: e.activation(out=es_[:, 0:qw], in_=ps[:, 0:qw], func=AF.Exp, scale=scale),
                     reads=[pb], writes=[esb])
                acc, accb, eng = accs[kti % 2]
                if kti < 2:
                    S.op(eng, lambda e, acc=acc, es_=es_: e.tensor_copy(out=acc[:, 0:qw], in_=es_[:, 0:qw]), reads=[esb], writes=[accb])
                else:
                    S.op(eng, lambda e, acc=acc, es_=es_: e.tensor_tensor(out=acc[:, 0:qw], in0=acc[:, 0:qw], in1=es_[:, 0:qw], op=ALU.add),
                         reads=[esb, accb], writes=[accb])
                prev = (kti, es_, esb)
            emit_pv(*prev)
            parts = accs[0:min(2, nkt)]
            for i, (acc, accb, _) in enumerate(parts):
                S.op("pe", lambda e, acc=acc, i=i: e.matmul(psum[SB][:, 0:qw], lhsT=ones_f[:], rhs=acc[:, 0:qw], start=(i == 0),
                                                          stop=(i == len(parts) - 1)),
                     reads=[accb, cb["ones"]], writes=[pbuf[SB]], sig=(i == len(parts) - 1))

        octr = [0]
        for h in range(MLA_HEADS):
            (kt, ktb), (qt, qtb), (qr, qrb) = ops[(h % 2) * 3:(h % 2) * 3 + 3]
            vh, vhb = vhs[h % 2]
            S.dma("sp", lambda e, kt=kt, h=h: e.dma_start(out=kt, in_=KN[h]), reads=[dbufs["KN"]], writes=[ktb])
            S.dma("sp", lambda e, qt=qt, h=h: e.dma_start(out=qt, in_=QN[h]), reads=[dbufs["QN"]], writes=[qtb])
            S.dma("sp", lambda e, qr=qr, h=h: e.dma_start(out=qr[0:64, :], in_=QR[h]), reads=[dbufs["QR"]], writes=[qrb])
            S.dma("sp", lambda e, vh=vh, h=h: e.dma_start(out=vh, in_=VM.rearrange("n p d -> p n d")[:, :, h * 129:(h + 1) * 129]),
                  reads=[dbufs["VM"]], writes=[vhb])
            for (q0, qw, nkt) in qblocks:
                attend([(kt, ktb), (kr[0:64, :], krb)], [(qt, qtb), (qr[0:64, :], qrb)], vh, vhb, 192.0 ** -0.5, q0, qw, nkt)
                og, ogb = ostg[octr[0] % 2]
                octr[0] += 1
                S.op("dve", lambda e, qw=qw: e.reciprocal(out=rcp[:, 0:qw], in_=psum[SB][:, 0:qw]), reads=[pbuf[SB]], writes=[rcpb])
                S.op("dve", lambda e, qw=qw, og=og: e.tensor_tensor(out=og[:, 0:qw], in0=psum[OB][:, 0:qw], in1=rcp[:, 0:qw], op=ALU.mult),
                     reads=[pbuf[OB], rcpb], writes=[ogb])
                S.dma("sp", lambda e, og=og, q0=q0, qw=qw, h=h: e.dma_start(out=OM[h][:, q0:q0 + qw], in_=og[:, 0:qw]),
                      reads=[ogb], writes=[dbufs["OM"]])
        for h in range(DIFF_HEADS):
            (kd, kdb), (qd, qdb) = ops[6:8] if h % 2 else ops[0:2]
            vh, vhb = vhs[h % 2]
            S.dma("sp", lambda e, kd=kd, h=h: e.dma_start(out=kd, in_=KD[h]), reads=[dbufs["KD"]], writes=[kdb])
            S.dma("sp", lambda e, qd=qd, h=h: e.dma_start(out=qd, in_=QD[h]), reads=[dbufs["QD"]], writes=[qdb])
            S.dma("sp", lambda e, vh=vh, h=h: e.dma_start(out=vh[:, :, :], in_=VD.rearrange("n p d -> p n d")[:, :, h * 129:(h + 1) * 129]),
                  reads=[dbufs["VD"]], writes=[vhb])
            for (q0, qw, nkt) in qblocks:
                for m, (om_, omb_) in enumerate(((o0, o0b), (o1, o1b))):
                    attend([(kd[m * 64:(m + 1) * 64, :], kdb)], [(qd[m * 64:(m + 1) * 64, :], qdb)], vh, vhb, 64.0 ** -0.5, q0, qw, nkt)
                    S.op("dve", lambda e, qw=qw: e.reciprocal(out=rcp[:, 0:qw], in_=psum[SB][:, 0:qw]), reads=[pbuf[SB]], writes=[rcpb])
                    S.op("dve", lambda e, qw=qw, om_=om_: e.tensor_tensor(out=om_[:, 0:qw], in0=psum[OB][:, 0:qw], in1=rcp[:, 0:qw],
                                                                        op=ALU.mult), reads=[pbuf[OB], rcpb], writes=[omb_])
                og, ogb = ostg[octr[0] % 2]
                octr[0] += 1
                S.op("dve", lambda e, qw=qw: e.scalar_tensor_tensor(out=o0[:, 0:qw], in0=o1[:, 0:qw], scalar=lamt[:, l, 5:6],
                                                                    in1=o0[:, 0:qw], op0=ALU.mult, op1=ALU.add),
                     reads=[o0b, o1b, cb["lamt"]], writes=[o0b])
                S.op("act", lambda e, qw=qw: e.activation(out=sq5[:, 0:qw], in_=o0[:, 0:qw], func=AF.Square), reads=[o0b], writes=[sq5b])
                S.op("pe", lambda e, qw=qw: e.matmul(psum[QB][:, 0:qw], lhsT=ones_f[:], rhs=sq5[:, 0:qw], start=True, stop=True),
                     reads=[sq5b, cb["ones"]], writes=[pbuf[QB]])
                S.op("dve", lambda e, qw=qw: e.tensor_copy(out=t5[:, 0:qw], in_=psum[QB][:, 0:qw]), reads=[pbuf[QB]], writes=[t5b])
                S.op("dve", lambda e, qw=qw: e.tensor_scalar(out=t5[:, 0:qw], in0=t5[:, 0:qw], scalar1=1.0 / 128, scalar2=EPS,
                                                             op0=ALU.mult, op1=ALU.add), reads=[t5b], writes=[t5b])
                S.op("act", lambda e, qw=qw: e.activation(out=t5[:, 0:qw], in_=t5[:, 0:qw], func=AF.Sqrt), reads=[t5b], writes=[t5b])
                S.op("dve", lambda e, qw=qw: e.reciprocal(out=t5[:, 0:qw], in_=t5[:, 0:qw]), reads=[t5b], writes=[t5b])
                S.op("dve", lambda e, qw=qw: e.tensor_tensor(out=o0[:, 0:qw], in0=o0[:, 0:qw], in1=t5[:, 0:qw], op=ALU.mult),
                     reads=[o0b, t5b], writes=[o0b])
                S.op("act", lambda e, qw=qw, og=og: e.activation(out=og[:, 0:qw], in_=o0[:, 0:qw], func=AF.Identity, scale=gs[:, 0:1]),
                     reads=[o0b, gsb], writes=[ogb])
                S.dma("sp", lambda e, og=og, q0=q0, qw=qw, h=h: e.dma_start(out=OD[h][:, q0:q0 + qw], in_=og[:, 0:qw]),
                      reads=[ogb], writes=[dbufs["OD"]])

    def phase_merge(l):
        new_phase()
        om, omb = ABF.alloc((8, T), "om")
        od, odb = ABF.alloc((4, T), "od")
        opp, oppb = ABF.alloc((4, T), "opp")
        S.dma("sp", lambda e: e.dma_start(out=om, in_=OM.rearrange("k p t -> p k t")), reads=[dbufs["OM"]], writes=[omb])
        S.dma("sp", lambda e: e.dma_start(out=od, in_=OD.rearrange("k p t -> p k t")), reads=[dbufs["OD"]], writes=[odb])
        S.dma("sp", lambda e: e.dma_start(out=opp, in_=OP.rearrange("k p t -> p k t")), reads=[dbufs["OP"]], writes=[oppb])
        wbs = [ABF.alloc((16, 128), f"wb{i}") for i in range(2)]
        gts = [ABF.alloc((3, 512), f"gt{i}") for i in range(2)]
        stg = [ABF.alloc((512,), f"stgm{i}") for i in range(2)]
        ta, tab = AF32.alloc((512,), "ta")
        tb, tbb = AF32.alloc((512,), "tb")
        ctr = 0
        for j in range(16):
            w, wb = wbs[j % 2]
            load_w_chunk(w[:, 0:8, :], wb, I["w_br_mla"][l][:, j * 128:(j + 1) * 128])
            load_w_chunk(w[:, 8:12, :], wb, I["w_br_diff"][l][:, j * 128:(j + 1) * 128])
            load_w_chunk(w[:, 12:16, :], wb, I["w_br_pool"][l][:, j * 128:(j + 1) * 128])
            for (b0, bw) in BLKS:
                g, gb = gts[ctr % 2]
                sg, sgb = stg[ctr % 2]
                ctr += 1
                for i in range(3):
                    S.dma("sp", lambda e, g=g, i=i, b0=b0, bw=bw, j=j: e.dma_start(out=g[:, i, 0:bw], in_=GT[i * 16 + j][:, b0:b0 + bw]),
                          reads=[dbufs["GT"]], writes=[gb])
                pss = []
                for (src_, srcb_, k0, nk) in ((om, omb, 0, 8), (od, odb, 8, 4), (opp, oppb, 12, 4)):
                    ps, pb = P(6)
                    for k in range(nk):
                        S.op("pe", lambda e, ps=ps, k=k, k0=k0, nk=nk, src_=src_, b0=b0, bw=bw, w=w: e.matmul(
                            ps[:, 0:bw], lhsT=w[:, k0 + k, :], rhs=src_[:, k, b0:b0 + bw], start=(k == 0), stop=(k == nk - 1)),
                            reads=[wb, srcb_], writes=[pb], sig=(k == nk - 1))
                    pss.append((ps, pb))
                S.op("dve", lambda e, p=pss[0][0], g=g, bw=bw: e.tensor_tensor(out=ta[:, 0:bw], in0=p[:, 0:bw], in1=g[:, 0, 0:bw],
                                                                            op=ALU.mult), reads=[pss[0][1], gb], writes=[tab])
                S.op("dve", lambda e, p=pss[1][0], g=g, bw=bw: e.tensor_tensor(out=tb[:, 0:bw], in0=p[:, 0:bw], in1=g[:, 1, 0:bw],
                                                                            op=ALU.mult), reads=[pss[1][1], gb], writes=[tbb])
                S.op("pool", lambda e, bw=bw: e.tensor_tensor(out=ta[:, 0:bw], in0=ta[:, 0:bw], in1=tb[:, 0:bw], op=ALU.add),
                     reads=[tab, tbb], writes=[tab])
                S.op("dve", lambda e, p=pss[2][0], g=g, bw=bw: e.tensor_tensor(out=tb[:, 0:bw], in0=p[:, 0:bw], in1=g[:, 2, 0:bw],
                                                                            op=ALU.mult), reads=[pss[2][1], gb], writes=[tbb])
                S.op("pool", lambda e, sg=sg, bw=bw: e.tensor_tensor(out=sg[:, 0:bw], in0=ta[:, 0:bw], in1=tb[:, 0:bw], op=ALU.add),
                     reads=[tab, tbb], writes=[sgb])
                S.dma("sp", lambda e, sg=sg, b0=b0, bw=bw, j=j: e.dma_start(out=MT[j][:, b0:b0 + bw], in_=sg[:, 0:bw]),
                      reads=[sgb], writes=[dbufs["MT"]])

    def deepnorm_tile(src4, xt, xb, t, tb_, sq, sqb, st, stb, gbc, gbcb, lng, lngb, lnb, lnbb):
        for nb, (ya, yb) in enumerate(src4):
            S.op("dve", lambda e, ya=ya, nb=nb: e.tensor_tensor(out=t[:, nb * 512:(nb + 1) * 512], in0=ya,
                                                               in1=gbc[:, nb * 512:(nb + 1) * 512], op=ALU.mult),
                 reads=[yb, gbcb], writes=[tb_])
        S.op("dve", lambda e: e.scalar_tensor_tensor(out=t, in0=xt, scalar=DN_ALPHA, in1=t, op0=ALU.mult, op1=ALU.add),
             reads=[xb, tb_], writes=[tb_])
        layernorm(t, tb_, xt, xb, sq, sqb, st, stb)
        S.op("dve", lambda e: e.tensor_tensor(out=xt, in0=xt, in1=lng, op=ALU.mult), reads=[xb, lngb], writes=[xb])
        S.op("pool", lambda e: e.tensor_tensor(out=xt, in0=xt, in1=lnb, op=ALU.add), reads=[xb, lnbb], writes=[xb])

    def phase_wo(l):
        new_phase()
        wo, wob = ABF.alloc((16, D), "wo")
        for nb in range(4):
            load_w_chunk(wo[:, :, nb * 512:(nb + 1) * 512], wob, I["w_o"][l][:, nb * 512:(nb + 1) * 512])
        mts = [ABF.alloc((16, 128), f"mt{i}") for i in range(2)]
        gl, glb = AF32.alloc((D,), "g1lat")
        gc, gcb = AF32.alloc((D,), "g1ctx")
        lng, lngb = AF32.alloc((D,), "lng")
        lnb, lnbb = AF32.alloc((D,), "lnb")
        S.dma("sp", lambda e: e.dma_start(out=gl, in_=GROW[l, 0, 0:1, :].partition_broadcast(128)), reads=[dbufs["GROW"]], writes=[glb])
        S.dma("sp", lambda e: e.dma_start(out=gc, in_=GROW[l, 0, 1:2, :].partition_broadcast(128)), reads=[dbufs["GROW"]], writes=[gcb])
        S.dma("sp", lambda e: e.dma_start(out=lng, in_=I["ln1_g"][l:l + 1, :].partition_broadcast(128)), writes=[lngb])
        S.dma("sp", lambda e: e.dma_start(out=lnb, in_=I["ln1_b"][l:l + 1, :].partition_broadcast(128)), writes=[lnbb])
        xt, xb = AF32.alloc((D,), "xtw")
        t, tb_ = AF32.alloc((D,), "tw")
        sq, sqb = AF32.alloc((D,), "sqw")
        st, stb = AF32.alloc((4,), "stw")
        for tt in range(NT):
            mt, mtb = mts[tt % 2]
            S.dma("sp", lambda e, mt=mt, tt=tt: e.dma_start(out=mt, in_=MT.rearrange("k p t -> p k t")[:, :, tt * 128:(tt + 1) * 128]),
                  reads=[dbufs["MT"]], writes=[mtb])
            S.dma("sp", lambda e, tt=tt: e.dma_start(out=xt, in_=XS[tt * 128:(tt + 1) * 128, :]), reads=[dbufs["XS"]], writes=[xb])
            src4 = []
            for nb in range(4):
                b = 4 + nb
                for k in range(16):
                    S.op("pe", lambda e, b=b, k=k, nb=nb, mt=mt: e.matmul(psum[b][:, 0:512], lhsT=mt[:, k, :],
                                                                        rhs=wo[:, k, nb * 512:(nb + 1) * 512], start=(k == 0),
                                                                        stop=(k == 15)), reads=[mtb, wob], writes=[pbuf[b]], sig=(k == 15))
                src4.append((psum[b][:, 0:512], pbuf[b]))
            g, gb_ = (gc, gcb) if is_ctx_tile(tt) else (gl, glb)
            deepnorm_tile(src4, xt, xb, t, tb_, sq, sqb, st, stb, g, gb_, lng, lngb, lnb, lnbb)
            S.dma("sp", lambda e, tt=tt: e.dma_start(out=XS[tt * 128:(tt + 1) * 128, :], in_=xt), reads=[xb], writes=[dbufs["XS"]])

    NR = NG + NE
    NSLOT = NE * CAP
    regs = {}

    def _init_pool(eng):
        regs["bc"] = eng.alloc_register("bc")
        eng.reg_mov(regs["bc"], NSLOT - 1)
    S.init.setdefault("pool", []).append(_init_pool)

    def phase_route(l):
        new_phase()
        for i in range(NSLOT // 128):
            S.dma("sp", lambda e, i=i: e.dma_start(out=XSORT[i * 128:(i + 1) * 128, :], in_=zero_b[:]), reads=[cb["zero"]],
                  writes=[dbufs["XSORT"]])
        S.op("pool", lambda e: e.memset(ebase[:], 0.0), writes=[cb["ebase"]])
        wr, wrb = AF32.alloc((16, NR), "wr")
        rb, rbb = AF32.alloc((NR,), "rb")
        S.dma("sp", lambda e: e.dma_start(out=wr[:, :, 0:NG], in_=I["w_grp"][l][:, 0:NG].rearrange("(k p) n -> p k n", p=128), allow_slow_non_contiguous=True), writes=[wrb])
        S.dma("sp", lambda e: e.dma_start(out=wr[:, :, NG:NR], in_=I["w_exp"][l][:, 0:NE].rearrange("(k p) n -> p k n", p=128), allow_slow_non_contiguous=True), writes=[wrb])
        S.dma("sp", lambda e: e.dma_start(out=rb[:, 0:NG], in_=I["b_grp"][l:l + 1, 0:NG].partition_broadcast(128)), writes=[rbb])
        S.dma("sp", lambda e: e.dma_start(out=rb[:, NG:NR], in_=I["b_exp"][l:l + 1, 0:NE].partition_broadcast(128)), writes=[rbb])
        xt, xb = AF32.alloc((D,), "xtr")
        xn, xnb = AF32.alloc((D,), "xnr")
        sq, sqb = AF32.alloc((D,), "sqr")
        vT, vTb = AF32.alloc((16, 128), "vT")
        st, stb = AF32.alloc((4,), "str")
        lg, lgb = AF32.alloc((NR,), "lg")
        ge, geb = AF32.alloc((NG,), "ge")
        pen, penb = AF32.alloc((NG,), "pen")
        msk, mskb = AF32.alloc((NE,), "msk")
        msk2, msk2b = AF32.alloc((NE,), "msk2")
        ohs = [AF32.alloc((NE,), f"oh{i}") for i in range(2)]
        pos, posb = AF32.alloc((NE,), "pos")
        tmpe, tmpeb = AF32.alloc((NE,), "tmpe")
        s_, sb_ = AF32.alloc((16,), "rs")
        vbs = [ABF.alloc((D,), f"vb{i}") for i in range(2)]
        for tt in range(NT):
            r = 1 if is_ctx_tile(tt) else 0
            S.dma("sp", lambda e, tt=tt: e.dma_start(out=xt, in_=XS[tt * 128:(tt + 1) * 128, :]), reads=[dbufs["XS"]], writes=[xb])
            layernorm(xt, xb, xn, xnb, sq, sqb, st, stb)
            for k in range(16):
                if k % 4 == 0:
                    ps, pb = P(4)
                S.op("pe", lambda e, ps=ps, k=k: e.transpose(ps[:, (k % 4) * 128:(k % 4 + 1) * 128], xn[:, k * 128:(k + 1) * 128],
                                                             ident[:]), reads=[xnb, cb["ident"]], writes=[pb])
                S.op("act", lambda e, ps=ps, k=k, r=r: e.activation(
                    out=vT[:, k, :], in_=ps[:, (k % 4) * 128:(k % 4 + 1) * 128], func=AF.Identity,
                    scale=mod1[:, l, 64 + k, r:r + 1], bias=mod[:, l, 48 + k, r:r + 1]), reads=[pb, cb["mod"]], writes=[vTb])
            pr, prb = P(2, 4)
            for k in range(16):
                S.op("pe", lambda e, k=k, pr=pr: e.matmul(pr[:, 0:NR], lhsT=vT[:, k, :], rhs=wr[:, k, :], start=(k == 0), stop=(k == 15)),
                     reads=[vTb, wrb], writes=[prb])
            S.op("dve", lambda e, pr=pr: e.tensor_tensor(out=lg, in0=pr[:, 0:NR], in1=rb, op=ALU.add), reads=[prb, rbb], writes=[lgb])
            vb_, vbb = vbs[tt % 2]
            for k in range(16):
                if k % 4 == 0:
                    ps, pb = P(4)
                S.op("pe", lambda e, ps=ps, k=k: e.transpose(ps[:, (k % 4) * 128:(k % 4 + 1) * 128], vT[:, k, :], ident[:]),
                     reads=[vTb, cb["ident"]], writes=[pb])
                if k % 4 == 3:
                    kb = k // 4
                    if kb % 2 == 0:
                        S.op("act", lambda e, ps=ps, kb=kb, vb_=vb_: e.activation(out=vb_[:, kb * 512:(kb + 1) * 512], in_=ps[:, 0:512],
                                                                                 func=AF.Identity), reads=[pb], writes=[vbb])
                    else:
                        S.op("dve", lambda e, ps=ps, kb=kb, vb_=vb_: e.tensor_copy(out=vb_[:, kb * 512:(kb + 1) * 512], in_=ps[:, 0:512]),
                             reads=[pb], writes=[vbb])
            S.op("dve", lambda e: e.tensor_reduce(out=s_[:, 0:1], in_=lg[:, 0:NG], axis=AX.X, op=ALU.max), reads=[lgb], writes=[sb_])
            S.op("dve", lambda e: e.tensor_scalar(out=ge, in0=lg[:, 0:NG], scalar1=s_[:, 0:1], scalar2=None, op0=ALU.subtract),
                 reads=[lgb, sb_], writes=[geb])
            S.op("act", lambda e: e.activation(out=ge, in_=ge, func=AF.Exp), reads=[geb], writes=[geb])
            S.op("dve", lambda e: e.tensor_reduce(out=s_[:, 1:2], in_=ge, axis=AX.X, op=ALU.add), reads=[geb], writes=[sb_])
            S.op("dve", lambda e: e.reciprocal(out=s_[:, 2:3], in_=s_[:, 1:2]), reads=[sb_], writes=[sb_])
            S.op("dve", lambda e: e.tensor_scalar(out=pen, in0=lg[:, 0:NG], scalar1=s_[:, 0:1], scalar2=None, op0=ALU.is_equal),
                 reads=[lgb, sb_], writes=[penb])
            S.op("dve", lambda e: e.tensor_scalar(out=pen, in0=pen, scalar1=-1.0, scalar2=BIG, op0=ALU.add, op1=ALU.mult),
                 reads=[penb], writes=[penb])
            for g in range(NG):
                S.op("dve", lambda e, g=g: e.tensor_scalar(out=msk[:, g * EPG:(g + 1) * EPG], in0=lg[:, NG + g * EPG:NG + (g + 1) * EPG],
                                                           scalar1=pen[:, g:g + 1], scalar2=None, op0=ALU.add),
                     reads=[lgb, penb], writes=[mskb])
            oh1, oh1b = ohs[0]
            oh2, oh2b = ohs[1]
            S.op("dve", lambda e: e.tensor_reduce(out=s_[:, 3:4], in_=msk, axis=AX.X, op=ALU.max), reads=[mskb], writes=[sb_])
            S.op("dve", lambda e: e.tensor_scalar(out=oh1, in0=msk, scalar1=s_[:, 3:4], scalar2=None, op0=ALU.is_equal),
                 reads=[mskb, sb_], writes=[oh1b])
            S.op("dve", lambda e: e.scalar_tensor_tensor(out=msk2, in0=oh1, scalar=-BIG, in1=msk, op0=ALU.mult, op1=ALU.add),
                 reads=[oh1b, mskb], writes=[msk2b])
            S.op("dve", lambda e: e.tensor_reduce(out=s_[:, 4:5], in_=msk2, axis=AX.X, op=ALU.max), reads=[msk2b], writes=[sb_])
            S.op("dve", lambda e: e.tensor_scalar(out=oh2, in0=msk2, scalar1=s_[:, 4:5], scalar2=None, op0=ALU.is_equal),
                 reads=[msk2b, sb_], writes=[oh2b])
            S.op("dve", lambda e: e.tensor_tensor(out=s_[:, 5:6], in0=s_[:, 3:4], in1=s_[:, 4:5], op=ALU.subtract), reads=[sb_], writes=[sb_])
            S.op("act", lambda e: e.activation(out=s_[:, 6:7], in_=s_[:, 5:6], func=AF.Sigmoid), reads=[sb_], writes=[sb_])
            S.op("act", lambda e: e.activation(out=s_[:, 7:8], in_=s_[:, 5:6], func=AF.Sigmoid, scale=-1.0), reads=[sb_], writes=[sb_])
            S.op("dve", lambda e, tt=tt: e.tensor_scalar(out=gatew[:, tt, :], in0=s_[:, 6:8], scalar1=s_[:, 2:3], scalar2=None,
                                                         op0=ALU.mult), reads=[sb_], writes=[cb["gatew"]])
            for kk, (oh, ohb) in enumerate(ohs):
                pp, ppb = P(2, 6)
                S.op("pe", lambda e, pp=pp, oh=oh: e.matmul(pp[:, 0:NE], lhsT=tril[:], rhs=oh, start=True, stop=True),
                     reads=[ohb, cb["tril"]], writes=[ppb])
                S.op("dve", lambda e, pp=pp: e.tensor_tensor(out=pos, in0=pp[:, 0:NE], in1=ebase[:], op=ALU.add),
                     reads=[ppb, cb["ebase"]], writes=[posb])
                S.op("dve", lambda e, oh=oh: e.tensor_tensor(out=tmpe, in0=oh, in1=pos, op=ALU.mult), reads=[ohb, posb], writes=[tmpeb])
                S.op("dve", lambda e: e.tensor_reduce(out=s_[:, 8:9], in_=tmpe, axis=AX.X, op=ALU.add), reads=[tmpeb], writes=[sb_])
                S.op("dve", lambda e, oh=oh: e.tensor_tensor(out=tmpe, in0=oh, in1=eoff[:], op=ALU.mult), reads=[ohb, cb["eoff"]],
                     writes=[tmpeb])
                S.op("dve", lambda e: e.tensor_reduce(out=s_[:, 9:10], in_=tmpe, axis=AX.X, op=ALU.add), reads=[tmpeb], writes=[sb_])
                S.op("dve", lambda e: e.tensor_scalar(out=s_[:, 10:11], in0=s_[:, 8:9], scalar1=float(CAP) - 0.5, scalar2=1.0e9,
                                                      op0=ALU.is_ge, op1=ALU.mult), reads=[sb_], writes=[sb_])
                S.op("dve", lambda e: e.tensor_tensor(out=s_[:, 8:9], in0=s_[:, 8:9], in1=s_[:, 9:10], op=ALU.add), reads=[sb_], writes=[sb_])
                S.op("dve", lambda e: e.tensor_tensor(out=s_[:, 8:9], in0=s_[:, 8:9], in1=s_[:, 10:11], op=ALU.add), reads=[sb_], writes=[sb_])
                S.op("dve", lambda e, tt=tt, kk=kk: e.tensor_copy(out=dest_i[:, tt, kk:kk + 1], in_=s_[:, 8:9]), reads=[sb_],
                     writes=[cb["dest"]])
                pt, ptb = P(2, 6)
                S.op("pe", lambda e, pt=pt, oh=oh: e.matmul(pt[:, 0:NE], lhsT=ones_f[:], rhs=oh, start=True, stop=True),
                     reads=[ohb, cb["ones"]], writes=[ptb])
                S.op("dve", lambda e, pt=pt: e.tensor_tensor(out=ebase[:], in0=ebase[:], in1=pt[:, 0:NE], op=ALU.add),
                     reads=[ptb, cb["ebase"]], writes=[cb["ebase"]])
                S.dma("pool", lambda e, tt=tt, kk=kk, vb_=vb_: e.indirect_dma_start(
                    out=XSORT, out_offset=bass.IndirectOffsetOnAxis(ap=dest_i[:, tt, kk:kk + 1], axis=0), in_=vb_, in_offset=None,
                    bounds_check=regs["bc"], oob_is_err=False), reads=[vbb, cb["dest"]], writes=[dbufs["XSORT"]])

    def phase_experts(l):
        new_phase()
        wgus = [ABF.alloc((16, 1024), f"wgu{i}") for i in range(2)]
        wdns = [ABF.alloc((4, D), f"wdn{i}") for i in range(2)]
        xes = [ABF.alloc((D,), f"xe{i}") for i in range(2)]
        xeTs = [ABF.alloc((16, 128), f"xeT{i}") for i in range(2)]
        a_s = [ABF.alloc((512,), f"aact{i}") for i in range(2)]
        aTs = [ABF.alloc((4, 128), f"aT{i}") for i in range(2)]
        sgs = [AF32.alloc((512,), f"sgl{i}") for i in range(2)]
        ys = [AF32.alloc((D,), f"ye{i}") for i in range(2)]
        NST = CAP // 128
        tiles = [(ex, s) for ex in range(NE) for s in range(NST)]
        n = len(tiles)
        dctr = [0]

        def load_gu(ex):
            if ex < NE:
                load_w_chunk(wgus[ex % 2][0], wgus[ex % 2][1], I["w_gu"][l, ex])

        def load_dn(ex):
            if ex < NE:
                load_w_chunk(wdns[ex % 2][0], wdns[ex % 2][1], I["w_dn"][l, ex])

        def stA(i):
            ex, s = tiles[i]
            r0 = ex * CAP + s * 128
            xe, xeb = xes[i % 2]
            xeT, xeTb = xeTs[i % 2]
            S.dma("sp", lambda e: e.dma_start(out=xe, in_=XSORT[r0:r0 + 128, :]), reads=[dbufs["XSORT"]], writes=[xeb])
            for kb in range(4):
                ps, pb = psum[kb % 2], pbuf[kb % 2]
                for kk in range(4):
                    k = kb * 4 + kk
                    S.op("pe", lambda e, ps=ps, k=k, kk=kk: e.matmul(ps[:, kk * 128:(kk + 1) * 128], lhsT=xe[:, k * 128:(k + 1) * 128],
                                                                    rhs=identb[:], start=True, stop=True),
                         reads=[xeb, cb["identb"]], writes=[pb], sig=(kk == 3))
                dstv = xeT[:, kb * 4:(kb + 1) * 4, :].rearrange("p a b -> p (a b)")
                if kb % 2 == 0:
                    S.op("act", lambda e, ps=ps, dstv=dstv: e.activation(out=dstv, in_=ps[:, 0:512], func=AF.Identity),
                         reads=[pb], writes=[xeTb])
                else:
                    S.op("dve", lambda e, ps=ps, dstv=dstv: e.tensor_copy(out=dstv, in_=ps[:, 0:512]), reads=[pb], writes=[xeTb])

        def stB(i):
            ex, s = tiles[i]
            wgu, wgub = wgus[ex % 2]
            xeT, xeTb = xeTs[i % 2]
            sg, sgb = sgs[i % 2]
            a_, ab_ = a_s[i % 2]
            for (bk, c0) in ((2, 0), (3, 512)):
                for k in range(16):
                    S.op("pe", lambda e, bk=bk, k=k, c0=c0: e.matmul(psum[bk][:, 0:512], lhsT=xeT[:, k, :], rhs=wgu[:, k, c0:c0 + 512],
                                                                    start=(k == 0), stop=(k == 15)),
                         reads=[xeTb, wgub], writes=[pbuf[bk]], sig=(k == 15))
            S.op("act", lambda e: e.activation(out=sg, in_=psum[2][:, 0:512], func=AF.Silu), reads=[pbuf[2]], writes=[sgb])
            S.op("dve", lambda e: e.tensor_tensor(out=a_, in0=psum[3][:, 0:512], in1=sg, op=ALU.mult), reads=[pbuf[3], sgb], writes=[ab_])
            if s == NST - 1:
                load_gu(ex + 2)

        def stC(i):
            a_, ab_ = a_s[i % 2]
            aT, aTb = aTs[i % 2]
            for k in range(4):
                S.op("pe", lambda e, k=k: e.matmul(psum[4][:, k * 128:(k + 1) * 128], lhsT=a_[:, k * 128:(k + 1) * 128], rhs=identb[:],
                                                  start=True, stop=True), reads=[ab_, cb["identb"]], writes=[pbuf[4]], sig=(k == 3))
            S.op("dve", lambda e: e.tensor_copy(out=aT.rearrange("p a b -> p (a b)"), in_=psum[4][:, 0:512]), reads=[pbuf[4]], writes=[aTb])

        def stD(i):
            ex, s = tiles[i]
            r0 = ex * CAP + s * 128
            wdn, wdnb = wdns[ex % 2]
            aT, aTb = aTs[i % 2]
            y, yb = ys[i % 2]
            for nb in range(4):
                bk = 5 + dctr[0] % 3
                dctr[0] += 1
                for k in range(4):
                    S.op("pe", lambda e, bk=bk, k=k, nb=nb: e.matmul(psum[bk][:, 0:512], lhsT=aT[:, k, :],
                                                                    rhs=wdn[:, k, nb * 512:(nb + 1) * 512], start=(k == 0), stop=(k == 3)),
                         reads=[aTb, wdnb], writes=[pbuf[bk]], sig=(k == 3))
                if nb % 2 == 0:
                    S.op("act", lambda e, bk=bk, nb=nb: e.activation(out=y[:, nb * 512:(nb + 1) * 512], in_=psum[bk][:, 0:512],
                                                                    func=AF.Identity), reads=[pbuf[bk]], writes=[yb])
                else:
                    S.op("dve", lambda e, bk=bk, nb=nb: e.tensor_copy(out=y[:, nb * 512:(nb + 1) * 512], in_=psum[bk][:, 0:512]),
                         reads=[pbuf[bk]], writes=[yb])
            S.dma("sp", lambda e: e.dma_start(out=YSORT[r0:r0 + 128, :], in_=y), reads=[yb], writes=[dbufs["YSORT"]])
            if s == NST - 1:
                load_dn(ex + 2)

        for ex in range(2):
            load_gu(ex)
            load_dn(ex)
        for it in range(n + 3):
            if it < n:
                stA(it)
            if 0 <= it - 1 < n:
                stB(it - 1)
            if 0 <= it - 2 < n:
                stC(it - 2)
            if 0 <= it - 3 < n:
                stD(it - 3)

    def phase_combine(l, last):
        new_phase()
        gl, glb = AF32.alloc((D,), "g2lat")
        gc, gcb = AF32.alloc((D,), "g2ctx")
        lng, lngb = AF32.alloc((D,), "lng2")
        lnb, lnbb = AF32.alloc((D,), "lnb2")
        S.dma("sp", lambda e: e.dma_start(out=gl, in_=GROW[l, 1, 0:1, :].partition_broadcast(128)), reads=[dbufs["GROW"]], writes=[glb])
        S.dma("sp", lambda e: e.dma_start(out=gc, in_=GROW[l, 1, 1:2, :].partition_broadcast(128)), reads=[dbufs["GROW"]], writes=[gcb])
        S.dma("sp", lambda e: e.dma_start(out=lng, in_=I["ln2_g"][l:l + 1, :].partition_broadcast(128)), writes=[lngb])
        S.dma("sp", lambda e: e.dma_start(out=lnb, in_=I["ln2_b"][l:l + 1, :].partition_broadcast(128)), writes=[lnbb])
        A, Ab = AF32.alloc((D,), "yA")
        B, Bb = AF32.alloc((D,), "yB")
        xt, xb = AF32.alloc((D,), "xtc")
        st, stb = AF32.alloc((4,), "stc")
        for tt in range(NT):
            S.op("pool", lambda e: e.memset(A, 0.0), writes=[Ab])
            S.op("pool", lambda e: e.memset(B, 0.0), writes=[Bb])
            for kk, (dst, dstb) in enumerate(((A, Ab), (B, Bb))):
                S.dma("pool", lambda e, tt=tt, kk=kk, dst=dst: e.indirect_dma_start(
                    out=dst, out_offset=None, in_=YSORT, in_offset=bass.IndirectOffsetOnAxis(ap=dest_i[:, tt, kk:kk + 1], axis=0),
                    bounds_check=regs["bc"], oob_is_err=False), reads=[dbufs["YSORT"], cb["dest"]], writes=[dstb])
            S.dma("sp", lambda e, tt=tt: e.dma_start(out=xt, in_=XS[tt * 128:(tt + 1) * 128, :]), reads=[dbufs["XS"]], writes=[xb])
            S.op("dve", lambda e, tt=tt: e.tensor_scalar(out=A, in0=A, scalar1=gatew[:, tt, 0:1], scalar2=None, op0=ALU.mult),
                 reads=[Ab, cb["gatew"]], writes=[Ab])
            S.op("dve", lambda e, tt=tt: e.scalar_tensor_tensor(out=A, in0=B, scalar=gatew[:, tt, 1:2], in1=A, op0=ALU.mult, op1=ALU.add),
                 reads=[Ab, Bb, cb["gatew"]], writes=[Ab])
            g, gb_ = (gc, gcb) if is_ctx_tile(tt) else (gl, glb)
            src4 = [(A[:, nb * 512:(nb + 1) * 512], Ab) for nb in range(4)]
            deepnorm_tile(src4, xt, xb, B, Bb, A, Ab, st, stb, g, gb_, lng, lngb, lnb, lnbb)
            S.dma("sp", lambda e, tt=tt: e.dma_start(out=XS[tt * 128:(tt + 1) * 128, :], in_=xt), reads=[xb], writes=[dbufs["XS"]])
            if last and not is_ctx_tile(tt):
                o0 = (tt - NTC) * 128
                S.dma("sp", lambda e, o0=o0: e.dma_start(out=OUT[o0:o0 + 128, :], in_=xt), reads=[xb], writes=[dbufs["OUT"]])

    def dump(dst_ap, src_ap, srcname):
        new_phase()
        n = src_ap.shape[1]
        t_, tb_ = ABF.alloc((n,), "dumpst")
        nr = src_ap.shape[0]
        S.dma("sp", lambda e: e.dma_start(out=t_[0:nr], in_=src_ap), reads=[dbufs[srcname]], writes=[tb_])
        S.dma("pool", lambda e: e.dma_start(out=dst_ap, in_=t_[0:nr]), reads=[tb_], writes=[dbufs["DBG"]])

    S.dma("sp", lambda e: e.dma_start(out=XS[0:T_CTX, :], in_=I["ctx"]), writes=[dbufs["XS"]])
    S.dma("sp", lambda e: e.dma_start(out=XS[T_CTX:T, :], in_=I["x"]), writes=[dbufs["XS"]])
    done = False
    if stop == "const":
        S.dma("sp", lambda e: e.dma_start(out=DBG[0:128, 0:128], in_=tril[:]), reads=[cb["tril"]], writes=[dbufs["DBG"]])
        done = True
    else:
        phase_adaln()
    if stop == "adaln":
        S.dma("sp", lambda e: e.dma_start(out=DBG[0:128, 0:192], in_=mod[:, 0].rearrange("p j r -> p (j r)")),
              reads=[cb["mod"]], writes=[dbufs["DBG"]])
        S.dma("sp", lambda e: e.dma_start(out=DBG[128:256, 0:64], in_=smallv[:, 0]), reads=[cb["smallv"]], writes=[dbufs["DBG"]])
        S.dma("sp", lambda e: e.dma_start(out=DBG[256:384, 0:8], in_=lamt[:, 0]), reads=[cb["lamt"]], writes=[dbufs["DBG"]])
        S.dma("sp", lambda e: e.dma_start(out=DBG[384:388, 0:D], in_=GROW[0].rearrange("a b d -> (a b) d")),
              reads=[dbufs["GROW"]], writes=[dbufs["DBG"]])
        done = True
    for l in range(0 if done else NL):
        r = phase_inproj(l)
        if stop == "uT":
            uT_, uTb_ = r
            for k in range(16):
                S.dma("pool", lambda e, k=k: e.dma_start(out=DBG[k * 128:(k + 1) * 128, 0:T], in_=uT_[:, k, :]),
                      reads=[uTb_], writes=[dbufs["DBG"]])
            done = True
            break
        if stop == "inproj":
            o = 0
            for nm, ap, rows in (("KD", KD[0], 128), ("KR", KR2, 128), ("CKG", CKG[1], 128), ("QD", QD[3], 128),
                                 ("GT", GT[5], 128), ("OP", OP[2], 128)):
                if nm in cfg.get("dumps", ("KD", "KR", "CKG", "QD", "GT", "OP")):
                    dump(DBG[o:o + rows, 0:T], ap, nm)
                o += rows
            if "VD" in cfg.get("dumps", ("VD",)):
                dump(DBG[o:o + 128, 0:516], VD[1], "VD")
            done = True
            break
        phase_upproj(l, *r)
        phase_attn(l)
        if stop == "attn":
            o = 0
            for nm, ap in (("OM", OM[0]), ("OM", OM[7]), ("OD", OD[0]), ("OD", OD[3]), ("KN", KN[2]), ("QN", QN[5]), ("QR", QR[3])):
                rows = ap.shape[0]
                dump(DBG[o:o + rows, 0:T], ap, nm)
                o += 128
            dump(DBG[o:o + 128, 0:1032], VM[1], "VM")
            done = True
            break
        phase_merge(l)
        phase_wo(l)
        if stop == "mixer":
            S.dma("sp", lambda e: e.dma_start(out=DBG[0:T, 0:D], in_=XS), reads=[dbufs["XS"]], writes=[dbufs["DBG"]])
            done = True
            break
        phase_route(l)
        if stop == "route":
            S.dma("sp", lambda e: e.dma_start(out=DBG[0:128, 0:NT * 2], in_=gatew[:].rearrange("p a b -> p (a b)")),
                  reads=[cb["gatew"]], writes=[dbufs["DBG"]])
            df, dfb = AF32.alloc((NT * 2,), "destf")
            S.op("dve", lambda e: e.tensor_copy(out=df, in_=dest_i[:].rearrange("p a b -> p (a b)")), reads=[cb["dest"]], writes=[dfb])
            S.dma("sp", lambda e: e.dma_start(out=DBG[128:256, 0:NT * 2], in_=df), reads=[dfb], writes=[dbufs["DBG"]])
            done = True
            break
        phase_experts(l)
        phase_combine(l, l == NL - 1)
        if stop == "layer" and l == 0:
            S.dma("sp", lambda e: e.dma_start(out=DBG[0:T, 0:D], in_=XS), reads=[dbufs["XS"]], writes=[dbufs["DBG"]])
            done = True
            break
    S.finish(list(dbufs.values()))
    barrier()
    S.emit()
    return nc, es, S


def host_consts(T_CTX, T_LAT, NE, CAP):
    T = T_CTX + T_LAT
    quarter = ROPE // 4
    inv_freq = (10000.0 ** (-np.arange(quarter, dtype=np.float32) / quarter)).astype(np.float32)
    rows = T_LAT // GRID_W
    row = np.repeat(np.arange(rows, dtype=np.float32), GRID_W)
    col = (np.arange(rows * GRID_W) % GRID_W).astype(np.float32)
    ang = np.stack([row[:, None] * inv_freq, col[:, None] * inv_freq], axis=1)
    cos, sin = np.cos(ang).astype(np.float32), np.sin(ang).astype(np.float32)
    c64 = np.ones((T, 2, 2, quarter), np.float32)
    s64 = np.zeros((T, 2, 2, quarter), np.float32)
    c64[T_CTX:] = cos[:, :, None, :]
    s64[T_CTX:] = sin[:, :, None, :]
    c64 = c64.reshape(T, 64).T
    s64 = s64.reshape(T, 64).T
    ropc = np.ascontiguousarray(np.concatenate([c64, c64], axis=0))
    rops = np.ascontiguousarray(np.concatenate([s64, s64], axis=0))
    rcnt = np.zeros((4, T), np.float32)
    for gi, w in enumerate(POOL_WINDOWS):
        half = w // 2
        for (s0, n) in ((0, T_CTX), (T_CTX, T_LAT)):
            t = np.arange(n)
            cnt = np.minimum(t + half, n) - np.maximum(t - half, 0)
            rcnt[gi, s0:s0 + n] = 1.0 / cnt.astype(np.float32)
    tril = (np.arange(128)[:, None] < np.arange(128)[None, :]).astype(np.float32)
    eoff = np.broadcast_to((np.arange(NE, dtype=np.float32) * CAP)[None, :], (128, NE)).copy()
    return dict(ident=np.eye(128, dtype=np.float32), ropc=ropc, rops=rops, rcnt=rcnt, tril=tril, eoff=eoff)


N_ACTIVE = 4
MOE_CAP = 384


def _per_core_inputs(b, x, c, ctx, c_ctx, weights, consts):
    m = dict(x=np.ascontiguousarray(x[b]), ctx=np.ascontiguousarray(ctx[b]),
             cvec=np.ascontiguousarray(np.stack([c[b], c_ctx])))
    m.update(weights)
    m.update(consts)
    return m


def kernel(x, c, ctx, c_ctx, w_ada, b_ada, w_in, b_gate, g_q, g_kv, w_uq, w_ukv, lam, g_sub, w_pool, pool_scale,
           w_br_mla, w_br_diff, w_br_pool, w_o, ln1_g, ln1_b, w_grp, b_grp, w_exp, b_exp, w_gu, w_dn, ln2_g, ln2_b):
    B, T_LAT, _ = x.shape
    T_CTX = ctx.shape[1]
    weights = dict(w_ada=w_ada, b_ada=b_ada, w_in=w_in, b_gate=np.reshape(b_gate, (DEPTH, 3 * D)), g_q=g_q, g_kv=g_kv,
                   w_uq=w_uq, w_ukv=w_ukv, lam=np.reshape(lam, (DEPTH, 4 * DIFF_HD)), g_sub=g_sub, w_pool=w_pool,
                   pool_scale=pool_scale, w_br_mla=w_br_mla, w_br_diff=w_br_diff, w_br_pool=w_br_pool, w_o=w_o,
                   ln1_g=ln1_g, ln1_b=ln1_b, w_grp=w_grp, b_grp=b_grp, w_exp=w_exp, b_exp=b_exp, w_gu=w_gu, w_dn=w_dn,
                   ln2_g=ln2_g, ln2_b=ln2_b)
    weights = {k: np.ascontiguousarray(np.asarray(v, np.float32)) for k, v in weights.items()}
    cfg = dict(T_CTX=T_CTX, T_LAT=T_LAT, NL=DEPTH, NG=N_GROUPS, CAP=MOE_CAP, stop="end")
    nc, es, S = build_program(cfg)
    consts = host_consts(T_CTX, T_LAT, N_EXPERTS, MOE_CAP)
    in_maps = [_per_core_inputs(b, x, c, ctx, c_ctx, weights, consts) for b in range(B)]
    res = run_bass_kernel_spmd(nc, in_maps, core_ids=list(range(N_ACTIVE)))
    return np.stack([res.results[b]["out"] for b in range(B)], axis=0).astype(np.float32)
```

```python
import math
import numpy as np
import ml_dtypes
import concourse.bass as bass
import concourse.mybir as mybir
from concourse.bass_utils import run_bass_kernel_spmd

F32 = mybir.dt.float32
BF16 = mybir.dt.bfloat16
I32 = mybir.dt.int32
ALU = mybir.AluOpType
AF = mybir.ActivationFunctionType
AX = mybir.AxisListType

D = 2048
DC = D // 128
DEPTH = 2
GRID_W = 64
MLA_HEADS = 8
Q_RANK = 768
KV_RANK = 512
NOPE = 128
ROPE = 64
MLA_V = 128
DIFF_HEADS = 4
DIFF_HD = 64
DIFF_V = 128
POOL_WINDOWS = (2, 4, 8, 16)
IN_COLS = 9536
N_GROUPS = 8
EPG = 8
N_EXPERTS = 64
D_EXPERT = 512
EPS = 1e-6
DN_ALPHA = (2 * DEPTH) ** 0.25
C_CKV, C_KROT, C_KDIFF, C_VDIFF, C_CQ, C_QDIFF, C_POOL, C_GATE = 0, 512, 576, 1088, 1600, 2368, 2880, 3392
BIG = 1.0e30


class Buf:
    __slots__ = ("w", "r", "name")

    def __init__(self, name=""):
        self.w = None
        self.r = {}
        self.name = name


class Sched:
    SEM_LIMIT = 30000
    ENGS = ("pe", "act", "dve", "pool", "sp")

    def __init__(self, nc, es):
        self.nc = nc
        self.es = es
        self.prog = {k: [] for k in self.ENGS}
        self.sem = {}
        self.cnt = {}
        self.waited = {k: {} for k in self.ENGS}
        self.last = {}
        self.init = {}
        self.pe_sems = set()
        self.nsem = 0
        for k in self.ENGS:
            self._new_sem(k)
        self.dq = {}
        for q in ("sp", "pool", "act"):
            sems = [self._alloc(f"dq_{q}_{i}") for i in range(6)]
            self.dq[q] = {"sems": sems, "cnt": [0] * len(sems), "i": 0}
        self.ninst = 0

    def _alloc(self, name):
        self.nsem += 1
        return self.es.enter_context(self.nc.semaphore(f"{name}_{self.nsem}"))

    def _new_sem(self, k):
        self.sem[k] = self._alloc(f"s_{k}")
        self.cnt[k] = 0
        if k == "pe":
            self.pe_sems.add(id(self.sem[k]))

    def _wait(self, k, tok):
        sem, val = tok
        sid = id(sem)
        if k == "pe" and sid in self.pe_sems:
            return
        w = self.waited[k]
        if w.get(sid, 0) >= val:
            return
        self.prog[k].append(("w", sem, val))
        w[sid] = val

    def _deps(self, k, reads, writes):
        for b in reads:
            if b.w is not None:
                self._wait(k, b.w)
        for b in writes:
            if b.w is not None:
                self._wait(k, b.w)
            for tok in b.r.values():
                self._wait(k, tok)

    def _commit(self, tok, reads, writes):
        sid = id(tok[0])
        for b in reads:
            b.r[sid] = tok
        for b in writes:
            b.w = tok
            b.r = {}

    def op(self, k, fn, reads=(), writes=(), sig=True):
        self._deps(k, reads, writes)
        if not sig:
            assert k == "pe"
            self.prog[k].append(("n", fn))
            tok = (self.sem[k], self.cnt[k] + 1)
            self._commit(tok, reads, writes)
            self.ninst += 1
            self.unsig = True
            return tok
        pending = k == "pe" and getattr(self, "unsig", False)
        if k == "pe":
            self.unsig = False
        if self.cnt[k] >= self.SEM_LIMIT and not pending:
            self._new_sem(k)
        self.cnt[k] += 1
        self.prog[k].append(("i", fn, self.sem[k], 1))
        tok = (self.sem[k], self.cnt[k])
        self.last[k] = tok
        self._commit(tok, reads, writes)
        self.ninst += 1
        return tok

    def dma(self, q, fn, reads=(), writes=()):
        self._deps(q, reads, writes)
        dq = self.dq[q]
        i = dq["i"]
        dq["i"] = (i + 1) % len(dq["sems"])
        sem = dq["sems"][i]
        if dq["cnt"][i] > 0:
            self._wait(q, (sem, dq["cnt"][i]))
        if dq["cnt"][i] >= self.SEM_LIMIT:
            sem = self._alloc(f"dq_{q}_{i}")
            dq["sems"][i] = sem
            dq["cnt"][i] = 0
        dq["cnt"][i] += 16
        self.prog[q].append(("i", fn, sem, 16))
        tok = (sem, dq["cnt"][i])
        self._commit(tok, reads, writes)
        self.ninst += 1
        return tok

    def finish(self, bufs):
        for b in bufs:
            if b.w is not None:
                self._wait("sp", b.w)

    def emit(self):
        def replay(k):
            def body(eng):
                for f in self.init.get(k, ()):
                    f(eng)
                for ent in self.prog[k]:
                    if ent[0] == "w":
                        eng.wait_ge(ent[1], ent[2])
                    elif ent[0] == "n":
                        ent[1](eng)
                    else:
                        ent[1](eng).then_inc(ent[2], ent[3])
            return body

        with self.nc.Block() as block:
            block.tensor(replay("pe"))
            block.scalar(replay("act"))
            block.vector(replay("dve"))
            block.gpsimd(replay("pool"))
            block.sync(replay("sp"))


class Arena:
    def __init__(self, tile, n):
        self.t = tile
        self.n = n
        self.off = 0

    def reset(self):
        self.off = 0

    def alloc(self, shape_free, name=""):
        n = int(np.prod(shape_free))
        n = (n + 15) // 16 * 16
        assert self.off + n <= self.n, f"arena overflow {name}: {self.off}+{n}>{self.n}"
        ap = self.t[:, self.off:self.off + int(np.prod(shape_free))]
        self.off += n
        if len(shape_free) == 2:
            ap = ap.rearrange("p (a b) -> p a b", b=shape_free[1])
        elif len(shape_free) == 3:
            ap = ap.rearrange("p (a b c) -> p a b c", b=shape_free[1], c=shape_free[2])
        return ap, Buf(name)


def token_blocks(t_ctx, t_lat, bw=512):
    blks = []
    for s0, n in ((0, t_ctx), (t_ctx, t_lat)):
        o = 0
        while o < n:
            w = min(bw, n - o)
            blks.append((s0 + o, w))
            o += w
    return blks


def build_program(cfg):
    T_CTX, T_LAT, NL = cfg["T_CTX"], cfg["T_LAT"], cfg["NL"]
    NG = cfg.get("NG", N_GROUPS)
    NE = NG * EPG
    CAP = cfg.get("CAP", 128)
    stop = cfg.get("stop", "end")
    T = T_CTX + T_LAT
    NT = T // 128
    NTC = T_CTX // 128
    BLKS = token_blocks(T_CTX, T_LAT)
    import contextlib
    es = contextlib.ExitStack()
    nc = bass.Bass("TRN2", target_bir_lowering=False)

    def din(name, shape, dt=F32):
        return nc.dram_tensor(name, list(shape), dt, kind="ExternalInput").ap()

    I = {}
    I["x"] = din("x", [T_LAT, D])
    I["ctx"] = din("ctx", [T_CTX, D])
    I["cvec"] = din("cvec", [2, D])
    L = DEPTH
    for nm, shp in (("w_ada", [L, D, 6 * D]), ("b_ada", [L, 6 * D]), ("w_in", [L, D, IN_COLS]), ("b_gate", [L, 3 * D]),
                    ("g_q", [L, Q_RANK]), ("g_kv", [L, KV_RANK]), ("w_uq", [L, Q_RANK, 1536]), ("w_ukv", [L, KV_RANK, 2048]),
                    ("lam", [L, 4 * DIFF_HD]), ("g_sub", [L, 128]), ("w_pool", [L, 4, 128, 128]), ("pool_scale", [L, 512]),
                    ("w_br_mla", [L, 1024, D]), ("w_br_diff", [L, 512, D]), ("w_br_pool", [L, 512, D]), ("w_o", [L, D, D]),
                    ("ln1_g", [L, D]), ("ln1_b", [L, D]), ("w_grp", [L, D, N_GROUPS]), ("b_grp", [L, N_GROUPS]),
                    ("w_exp", [L, D, N_EXPERTS]), ("b_exp", [L, N_EXPERTS]), ("w_gu", [L, NE, D, 1024]),
                    ("w_dn", [L, NE, 512, D]), ("ln2_g", [L, D]), ("ln2_b", [L, D])):
        I[nm] = din(nm, shp)
    I["ident"] = din("ident", [128, 128])
    I["ropc"] = din("ropc", [128, T])
    I["rops"] = din("rops", [128, T])
    I["rcnt"] = din("rcnt", [4, T])
    I["tril"] = din("tril", [128, 128])
    I["eoff"] = din("eoff", [128, NE])
    OUT = nc.dram_tensor("out", [T_LAT, D], F32, kind="ExternalOutput").ap()
    DBG = None
    if cfg.get("dbg"):
        DBG = nc.dram_tensor("dbg", list(cfg["dbg"]), F32, kind="ExternalOutput").ap()

    def dscr(name, shape, dt=BF16):
        return nc.dram_tensor(name, list(shape), dt).ap()

    XS = dscr("XS", [T, D], F32)
    GROW = dscr("GROW", [DEPTH, 2, 2, D], F32)
    KN = dscr("KN", [8, 128, T])
    KR2 = dscr("KR2", [128, T])
    CKG = dscr("CKG", [4, 128, T])
    CQG = dscr("CQG", [6, 128, T])
    VM = dscr("VM", [NT, 128, 8 * 129])
    QN = dscr("QN", [8, 128, T])
    QR = dscr("QR", [8, 64, T])
    KD = dscr("KD", [4, 128, T])
    QD = dscr("QD", [4, 128, T])
    VD = dscr("VD", [NT, 128, 4 * 129])
    OP = dscr("OP", [4, 128, T])
    GT = dscr("GT", [48, 128, T])
    OM = dscr("OM", [8, 128, T])
    OD = dscr("OD", [4, 128, T])
    MT = dscr("MT", [16, 128, T])
    XSORT = dscr("XSORT", [NE * CAP, D])
    YSORT = dscr("YSORT", [NE * CAP, D], F32)
    dbufs = {k: Buf(k) for k in ("XS", "GROW", "KN", "KR", "CKG", "CQG", "VM", "QN", "QR", "KD", "QD", "VD", "OP", "GT", "OM", "OD",
                                 "MT", "XSORT", "YSORT", "OUT", "DBG")}

    NBF = 58 * 1024
    NF32 = 14 * 1024 + 512
    abf_t = es.enter_context(nc.sbuf_tensor("abf", [128, NBF], BF16))
    af_t = es.enter_context(nc.sbuf_tensor("af32", [128, NF32], F32))
    ABF = Arena(abf_t, NBF)
    AF32 = Arena(af_t, NF32)
    ident = es.enter_context(nc.sbuf_tensor("identf", [128, 128], F32))
    identb = es.enter_context(nc.sbuf_tensor("identb", [128, 128], BF16))
    ones_f = es.enter_context(nc.sbuf_tensor("onesf", [128, 128], F32))
    ones_b = es.enter_context(nc.sbuf_tensor("onesb", [128, 128], BF16))
    tril = es.enter_context(nc.sbuf_tensor("trilf", [128, 128], F32))
    eoff = es.enter_context(nc.sbuf_tensor("eoff_sb", [128, NE], F32))
    zero_b = es.enter_context(nc.sbuf_tensor("zerob", [128, D], BF16))
    mod = es.enter_context(nc.sbuf_tensor("mod_sb", [128, DEPTH, 96, 2], F32))
    mod1 = es.enter_context(nc.sbuf_tensor("mod1", [128, DEPTH, 96, 2], F32))
    smallv = es.enter_context(nc.sbuf_tensor("smallv", [128, DEPTH, 64], F32))
    lamt = es.enter_context(nc.sbuf_tensor("lamt", [128, DEPTH, 8], F32))
    dest_i = es.enter_context(nc.sbuf_tensor("dest_i", [128, NT, 2], I32))
    gatew = es.enter_context(nc.sbuf_tensor("gatew", [128, NT, 2], F32))
    ebase = es.enter_context(nc.sbuf_tensor("ebase", [128, NE], F32))
    cb = {k: Buf(k) for k in ("ident", "identb", "ones", "tril", "eoff", "zero", "mod", "smallv", "lamt", "dest", "gatew", "ebase")}
    psum = [es.enter_context(nc.psum_tensor(f"ps{i}", [128, 512], F32)) for i in range(8)]
    pbuf = [Buf(f"ps{i}") for i in range(8)]
    S = Sched(nc, es)
    pctr = [0]

    def P(n=8, base=0):
        i = base + pctr[0] % n
        pctr[0] += 1
        return psum[i], pbuf[i]

    def barrier():
        toks = list(S.last.values())
        for q in S.dq.values():
            for sem, c in zip(q["sems"], q["cnt"]):
                if c > 0:
                    toks.append((sem, c))
        for k in S.ENGS:
            for t in toks:
                S._wait(k, t)

    def new_phase():
        barrier()
        ABF.reset()
        AF32.reset()

    S.dma("sp", lambda e: e.dma_start(out=ident[:], in_=I["ident"]), writes=[cb["ident"]])
    S.dma("sp", lambda e: e.dma_start(out=tril[:], in_=I["tril"]), writes=[cb["tril"]])
    S.dma("sp", lambda e: e.dma_start(out=eoff[:], in_=I["eoff"]), writes=[cb["eoff"]])
    S.op("dve", lambda e: e.tensor_copy(out=identb[:], in_=ident[:]), reads=[cb["ident"]], writes=[cb["identb"]])
    S.op("pool", lambda e: e.memset(ones_f[:], 1.0), writes=[cb["ones"]])
    S.op("pool", lambda e: e.memset(ones_b[:], 1.0), writes=[cb["ones"]])
    S.op("pool", lambda e: e.memset(zero_b[:], 0.0), writes=[cb["zero"]])

    def load_vec_fm(dst_ap, dst_buf, src2d, n):
        st, sb = AF32.alloc((128,), "vecst")
        S.dma("sp", lambda e: e.dma_start(out=st[0:n, :], in_=src2d), writes=[sb])
        ps, pb = P()
        S.op("pe", lambda e: e.transpose(ps[:, 0:n], st[0:n, :], ident[0:n, 0:n]), reads=[sb, cb["ident"]], writes=[pb])
        S.op("dve", lambda e: e.tensor_copy(out=dst_ap, in_=ps[:, 0:n]), reads=[pb], writes=[dst_buf])

    def phase_adaln():
        new_phase()
        cst, cstb = AF32.alloc((128,), "cst")
        sc, scb = AF32.alloc((32,), "sc")
        S.dma("sp", lambda e: e.dma_start(out=cst[0:32, :], in_=I["cvec"].rearrange("r (k p) -> (r k) p", p=128)), writes=[cstb])
        ps, pb = P()
        S.op("pe", lambda e: e.transpose(ps[:, 0:32], cst[0:32, :], ident[0:32, 0:32]), reads=[cstb, cb["ident"]], writes=[pb])
        S.op("act", lambda e: e.activation(out=sc, in_=ps[:, 0:32], func=AF.Silu), reads=[pb], writes=[scb])
        scv = sc.rearrange("p (r k) -> p k r", r=2)
        mark_ada = AF32.off
        for l in range(NL):
            barrier()
            AF32.off = mark_ada
            gsts = [AF32.alloc((256,), f"gst{i}") for i in range(2)]
            brs = [AF32.alloc((256,), f"br{i}") for i in range(2)]
            slabs = [AF32.alloc((16, 256), f"wada{i}") for i in range(2)]
            mps, mpb = psum[7], pbuf[7]
            for sl in range(6 * D // 256):
                w, wb = slabs[sl % 2]
                gst, gstb = gsts[sl % 2]
                br, brb = brs[sl % 2]
                S.dma("sp", lambda e, w=w, sl=sl, l=l: e.dma_start(
                    out=w, in_=I["w_ada"][l][:, sl * 256:(sl + 1) * 256].rearrange("(k p) n -> p k n", p=128)), writes=[wb])
                S.dma("sp", lambda e, br=br, sl=sl, l=l: e.dma_start(out=br[0:1, :], in_=I["b_ada"][l:l + 1, sl * 256:(sl + 1) * 256]),
                      writes=[brb])
                gps, gpb = P(4)
                for k in range(16):
                    S.op("pe", lambda e, w=w, k=k, gps=gps: e.matmul(gps[0:2, 0:256], lhsT=scv[:, k, :], rhs=w[:, k, :], start=(k == 0),
                                                                    stop=False), reads=[wb, scb], writes=[gpb], sig=False)
                S.op("pe", lambda e, gps=gps, br=br: e.matmul(gps[0:2, 0:256], lhsT=ones_f[0:1, 0:2], rhs=br[0:1, :], start=False, stop=True),
                     reads=[brb, cb["ones"]], writes=[gpb])
                S.op("dve", lambda e, gps=gps, gst=gst: e.tensor_copy(out=gst[0:2, :], in_=gps[0:2, 0:256]), reads=[gpb], writes=[gstb])
                which = (sl * 256) // D
                if which in (2, 5):
                    wi = 0 if which == 2 else 1
                    c0 = sl * 256 - which * D
                    S.dma("sp", lambda e, gst=gst, wi=wi, c0=c0, l=l: e.dma_start(
                        out=GROW[l, wi, :, c0:c0 + 256], in_=gst[0:2, :]), reads=[gstb], writes=[dbufs["GROW"]])
                for jj in range(2):
                    j = sl * 2 + jj
                    S.op("pe", lambda e, gst=gst, jj=jj, j=j: e.transpose(mps[:, 2 * j:2 * j + 2], gst[0:2, jj * 128:(jj + 1) * 128],
                                                                        ident[0:2, 0:2]), reads=[gstb, cb["ident"]], writes=[mpb])
            for r in range(2):
                S.op("dve", lambda e, l=l, r=r: e.tensor_copy(
                    out=mod[:, l, :, r], in_=mps[:, 0:192].rearrange("p (j r) -> p j r", r=2)[:, :, r]), reads=[mpb], writes=[cb["mod"]])
            S.op("dve", lambda e, l=l: e.tensor_scalar_add(out=mod1[:, l], in0=mod[:, l], scalar1=1.0),
                 reads=[cb["mod"]], writes=[cb["mod"]])
            load_vec_fm(smallv[:, l, 0:48], cb["smallv"], I["b_gate"][l].rearrange("(n p) -> n p", p=128), 48)
            load_vec_fm(smallv[:, l, 48:54], cb["smallv"], I["g_q"][l].rearrange("(n p) -> n p", p=128), 6)
            load_vec_fm(smallv[:, l, 54:58], cb["smallv"], I["g_kv"][l].rearrange("(n p) -> n p", p=128), 4)
            load_vec_fm(smallv[:, l, 58:62], cb["smallv"], I["pool_scale"][l].rearrange("(n p) -> n p", p=128), 4)
            load_vec_fm(smallv[:, l, 62:63], cb["smallv"], I["g_sub"][l].rearrange("(n p) -> n p", p=128), 1)
            lt, ltb = AF32.alloc((4 * DIFF_HD,), "lamin")
            S.dma("sp", lambda e, l=l: e.dma_start(out=lt, in_=I["lam"][l].partition_broadcast(128)), writes=[ltb])
            lp, lpb = AF32.alloc((2, DIFF_HD), "lamp")
            ltv = lt.rearrange("p (a b d) -> p a b d", a=2, b=2)
            S.op("dve", lambda e: e.tensor_tensor(out=lp, in0=ltv[:, :, 0, :], in1=ltv[:, :, 1, :], op=ALU.mult),
                 reads=[ltb], writes=[lpb])
            S.op("dve", lambda e, l=l: e.tensor_reduce(out=lamt[:, l, 0:2], in_=lp, axis=AX.X, op=ALU.add),
                 reads=[lpb], writes=[cb["lamt"]])
            S.op("act", lambda e, l=l: e.activation(out=lamt[:, l, 2:4], in_=lamt[:, l, 0:2], func=AF.Exp),
                 reads=[cb["lamt"]], writes=[cb["lamt"]])
            lam_init = 0.8 - 0.6 * math.exp(-0.3 * l)
            S.op("dve", lambda e, l=l: e.tensor_tensor(out=lamt[:, l, 4:5], in0=lamt[:, l, 3:4], in1=lamt[:, l, 2:3],
                                                       op=ALU.subtract), reads=[cb["lamt"]], writes=[cb["lamt"]])
            S.op("dve", lambda e, l=l, li=lam_init: e.tensor_scalar_add(out=lamt[:, l, 5:6], in0=lamt[:, l, 4:5], scalar1=-li),
                 reads=[cb["lamt"]], writes=[cb["lamt"]])
            AF32.off = 0 if False else AF32.off

    def ln_tile(xt, xb, st, stb):
        S.op("dve", lambda e: e.tensor_reduce(out=st[:, 0:1], in_=xt, axis=AX.X, op=ALU.add), reads=[xb], writes=[stb])
        S.op("dve", lambda e: e.tensor_scalar(out=st[:, 1:2], in0=st[:, 0:1], scalar1=-1.0 / D, scalar2=None, op0=ALU.mult),
             reads=[stb], writes=[stb])
        S.op("dve", lambda e: e.tensor_scalar(out=st[:, 0:1], in0=st[:, 0:1], scalar1=1.0 / D, scalar2=None, op0=ALU.mult),
             reads=[stb], writes=[stb])

    def ln_finish(sq, sqb, st, stb):
        S.op("dve", lambda e: e.tensor_reduce(out=st[:, 2:3], in_=sq, axis=AX.X, op=ALU.add), reads=[sqb], writes=[stb])
        S.op("dve", lambda e: e.tensor_scalar(out=st[:, 2:3], in0=st[:, 2:3], scalar1=1.0 / D, scalar2=EPS, op0=ALU.mult,
                                              op1=ALU.add), reads=[stb], writes=[stb])
        S.op("act", lambda e: e.activation(out=st[:, 3:4], in_=st[:, 2:3], func=AF.Sqrt), reads=[stb], writes=[stb])
        S.op("dve", lambda e: e.reciprocal(out=st[:, 2:3], in_=st[:, 3:4]), reads=[stb], writes=[stb])

    def layernorm(xt, xb, xn, xnb, sq, sqb, st, stb):
        ln_tile(xt, xb, st, stb)
        S.op("act", lambda e: e.activation(out=sq, in_=xt, func=AF.Square, bias=st[:, 1:2], scale=1.0),
             reads=[xb, stb], writes=[sqb])
        ln_finish(sq, sqb, st, stb)
        S.op("dve", lambda e: e.tensor_scalar(out=xn, in0=xt, scalar1=st[:, 0:1], scalar2=st[:, 2:3], op0=ALU.subtract,
                                              op1=ALU.mult), reads=[xb, stb], writes=[xnb])

    def is_ctx_tile(tt):
        return tt < NTC

    def load_w_chunk(dst, dstb, src_cols_ap, q="pool"):
        S.dma(q, lambda e: e.dma_start(out=dst, in_=src_cols_ap.rearrange("(k p) n -> p k n", p=128)), writes=[dstb])

    def make_rot_w(w, wb, wp, wpb):
        wv = w.rearrange("p k (g h j) -> p (k g) h j", h=2, j=16)
        wpv = wp.rearrange("p k (g h j) -> p (k g) h j", h=2, j=16)
        S.op("pool", lambda e: e.tensor_scalar(out=wpv[:, :, 0, :], in0=wv[:, :, 1, :], scalar1=-1.0, scalar2=None,
                                               op0=ALU.mult), reads=[wb], writes=[wpb])
        S.op("pool", lambda e: e.tensor_copy(out=wpv[:, :, 1, :], in_=wv[:, :, 0, :]), reads=[wb], writes=[wpb])

    def phase_inproj(l):
        new_phase()
        uT, uTb = ABF.alloc((16, T), "uT")
        ssq_kv, ssq_kvb = AF32.alloc((T,), "ssq_kv")
        ssq_q, ssq_qb = AF32.alloc((T,), "ssq_q")
        mark = AF32.off
        xts = [AF32.alloc((D,), f"xt{i}") for i in range(2)]
        xn, xnb = AF32.alloc((D,), "xn")
        sq, sqb = AF32.alloc((D,), "sq")
        st, stb = AF32.alloc((4,), "st")
        for tt in range(NT):
            xt, xb = xts[tt % 2]
            S.dma("sp", lambda e, xt=xt, tt=tt: e.dma_start(out=xt, in_=XS[tt * 128:(tt + 1) * 128, :]),
                  reads=[dbufs["XS"]], writes=[xb])
            layernorm(xt, xb, xn, xnb, sq, sqb, st, stb)
            r = 1 if is_ctx_tile(tt) else 0
            for k in range(16):
                if k % 4 == 0:
                    ps, pb = P()
                S.op("pe", lambda e, ps=ps, k=k: e.transpose(ps[:, (k % 4) * 128:(k % 4 + 1) * 128],
                                                             xn[:, k * 128:(k + 1) * 128], ident[:]),
                     reads=[xnb, cb["ident"]], writes=[pb])
                S.op("act", lambda e, ps=ps, k=k, tt=tt, r=r: e.activation(
                    out=uT[:, k, tt * 128:(tt + 1) * 128], in_=ps[:, (k % 4) * 128:(k % 4 + 1) * 128], func=AF.Identity,
                    scale=mod1[:, l, 16 + k, r:r + 1], bias=mod[:, l, k, r:r + 1]), reads=[pb, cb["mod"]], writes=[uTb])
        AF32.off = mark
        if stop == "uT":
            return uT, uTb
        barrier()
        ropc, ropcb = AF32.alloc((T,), "ropc")
        rops, ropsb = AF32.alloc((T,), "rops")
        S.dma("sp", lambda e: e.dma_start(out=ropc, in_=I["ropc"]), writes=[ropcb])
        S.dma("sp", lambda e: e.dma_start(out=rops, in_=I["rops"]), writes=[ropsb])
        wch = [ABF.alloc((16, 128), f"wch{i}") for i in range(2)]
        wchp = [ABF.alloc((16, 128), f"wchp{i}") for i in range(2)]
        stg = [ABF.alloc((512,), f"stg{i}") for i in range(3)]
        sqs = [AF32.alloc((512,), f"sqs{i}") for i in range(2)]
        tmp = [AF32.alloc((512,), f"tmpa{i}") for i in range(2)]
        ctr = [0, 0, 0]

        def proj_chunk(c0, ncols, evac, rot=False, dup=False):
            if ctr[0] >= cfg.get("m2cut", 10 ** 9):
                return
            i = ctr[0] % 2
            ctr[0] += 1
            w, wb = wch[i]
            nw = ncols * (2 if dup else 1)
            load_w_chunk(w[:, :, 0:ncols], wb, I["w_in"][l][:, c0:c0 + ncols])
            if dup:
                load_w_chunk(w[:, :, ncols:2 * ncols], wb, I["w_in"][l][:, c0:c0 + ncols])
            if rot:
                wp, wpb = wchp[i]
                make_rot_w(w[:, :, 0:nw], wb, wp[:, :, 0:nw], wpb)
            for (b0, bw) in BLKS:
                ps, pb = P(6)
                for k in range(16):
                    S.op("pe", lambda e, ps=ps, k=k, b0=b0, bw=bw: e.matmul(
                        ps[0:nw, 0:bw], lhsT=w[:, k, 0:nw], rhs=uT[:, k, b0:b0 + bw], start=(k == 0), stop=(k == 15)),
                        reads=[wb, uTb], writes=[pb], sig=(k == 15))
                ps2, pb2 = None, None
                if rot:
                    ps2, pb2 = P(6)
                    for k in range(16):
                        S.op("pe", lambda e, ps2=ps2, k=k, b0=b0, bw=bw: e.matmul(
                            ps2[0:nw, 0:bw], lhsT=wp[:, k, 0:nw], rhs=uT[:, k, b0:b0 + bw], start=(k == 0), stop=(k == 15)),
                            reads=[wpb, uTb], writes=[pb2], sig=(k == 15))
                evac(ps, pb, ps2, pb2, b0, bw, nw)

        def next_stg():
            s = stg[ctr[1] % 3]
            ctr[1] += 1
            return s

        def evac_rope(dram_rows, dname):
            def f(ps, pb, ps2, pb2, b0, bw, nw):
                t1, t1b = tmp[0]
                t2, t2b = tmp[1]
                sg, sgb = next_stg()
                S.op("dve", lambda e: e.tensor_tensor(out=t1[0:nw, 0:bw], in0=ps[0:nw, 0:bw], in1=ropc[0:nw, b0:b0 + bw],
                                                      op=ALU.mult), reads=[pb, ropcb], writes=[t1b])
                S.op("dve", lambda e: e.tensor_tensor(out=t2[0:nw, 0:bw], in0=ps2[0:nw, 0:bw], in1=rops[0:nw, b0:b0 + bw],
                                                      op=ALU.mult), reads=[pb2, ropsb], writes=[t2b])
                S.op("pool", lambda e: e.tensor_tensor(out=sg[0:nw, 0:bw], in0=t1[0:nw, 0:bw], in1=t2[0:nw, 0:bw],
                                                       op=ALU.add), reads=[t1b, t2b], writes=[sgb])
                S.dma("sp", lambda e: e.dma_start(out=dram_rows[0:nw, b0:b0 + bw], in_=sg[0:nw, 0:bw]), reads=[sgb],
                      writes=[dbufs[dname]])
            return f

        def evac_rms(dram_rows, dname, gcol, ssq, ssqb, first):
            def f(ps, pb, ps2, pb2, b0, bw, nw):
                em = cfg.get("evacmode", 9)
                if em < 1:
                    return
                sg, sgb = next_stg()
                s2, s2b = sqs[ctr[2] % 2]
                ctr[2] += 1
                S.op("act", lambda e: e.activation(out=s2[:, 0:bw], in_=ps[:, 0:bw], func=AF.Square), reads=[pb], writes=[s2b])
                if em < 2:
                    return
                S.op("act", lambda e: e.activation(out=sg[:, 0:bw], in_=ps[:, 0:bw], func=AF.Identity,
                                                   scale=smallv[:, l, gcol:gcol + 1]), reads=[pb, cb["smallv"]], writes=[sgb])
                S.dma("sp", lambda e: e.dma_start(out=dram_rows[:, b0:b0 + bw], in_=sg[:, 0:bw]), reads=[sgb],
                      writes=[dbufs[dname]])
                if em < 3:
                    return
                pq, pqb = P(2, 6)
                S.op("pe", lambda e: e.matmul(pq[:, 0:bw], lhsT=ones_f[:], rhs=s2[:, 0:bw], start=True, stop=True),
                     reads=[s2b, cb["ones"]], writes=[pqb])
                if first:
                    S.op("dve", lambda e: e.tensor_copy(out=ssq[:, b0:b0 + bw], in_=pq[:, 0:bw]), reads=[pqb], writes=[ssqb])
                else:
                    S.op("dve", lambda e: e.tensor_tensor(out=ssq[:, b0:b0 + bw], in0=ssq[:, b0:b0 + bw], in1=pq[:, 0:bw],
                                                          op=ALU.add), reads=[pqb, ssqb], writes=[ssqb])
            return f

        for kc in range(4):
            proj_chunk(C_CKV + kc * 128, 128, evac_rms(CKG[kc], "CKG", 54 + kc, ssq_kv, ssq_kvb, kc == 0))
        proj_chunk(C_KROT, 64, evac_rope(KR2, "KR"), rot=True, dup=True)
        for h in range(4):
            proj_chunk(C_KDIFF + h * 128, 128, evac_rope(KD[h], "KD"), rot=True)
        for kc in range(6):
            proj_chunk(C_CQ + kc * 128, 128, evac_rms(CQG[kc], "CQG", 48 + kc, ssq_q, ssq_qb, kc == 0))
        for h in range(4):
            proj_chunk(C_QDIFF + h * 128, 128, evac_rope(QD[h], "QD"), rot=True)

        def evac_gate(j):
            def f(ps, pb, ps2, pb2, b0, bw, nw):
                sg, sgb = next_stg()
                S.op("act", lambda e: e.activation(out=sg[:, 0:bw], in_=ps[:, 0:bw], func=AF.Sigmoid,
                                                   bias=smallv[:, l, j:j + 1], scale=1.0), reads=[pb, cb["smallv"]], writes=[sgb])
                S.dma("sp", lambda e: e.dma_start(out=GT[j][:, b0:b0 + bw], in_=sg[:, 0:bw]), reads=[sgb], writes=[dbufs["GT"]])
            return f
        for j in range(48):
            proj_chunk(C_GATE + j * 128, 128, evac_gate(j))

        if "m2cut" in cfg:
            return ssq_kv, ssq_kvb, ssq_q, ssq_qb, mark
        wv, wvb = ABF.alloc((16, 512), "wvd")
        load_w_chunk(wv, wvb, I["w_in"][l][:, C_VDIFF:C_VDIFF + 512])
        vst = [ABF.alloc((4, 129), f"vst{i}") for i in range(2)]
        for i in range(2):
            S.op("pool", lambda e, i=i: e.memset(vst[i][0], 1.0), writes=[vst[i][1]])
        for tt in range(NT):
            ps, pb = P(6)
            for k in range(16):
                S.op("pe", lambda e, ps=ps, k=k, tt=tt: e.matmul(ps[:, 0:512], lhsT=uT[:, k, tt * 128:(tt + 1) * 128],
                                                                 rhs=wv[:, k, :], start=(k == 0), stop=(k == 15)),
                     reads=[uTb, wvb], writes=[pb])
            v, vb = vst[tt % 2]
            S.op("act", lambda e, ps=ps, v=v: e.activation(out=v[:, :, 0:128], in_=ps[:, 0:512].rearrange("p (h d) -> p h d", d=128),
                                                           func=AF.Copy), reads=[pb], writes=[vb])
            S.dma("sp", lambda e, v=v, tt=tt: e.dma_start(out=VD[tt], in_=v.rearrange("p h d -> p (h d)")), reads=[vb],
                  writes=[dbufs["VD"]])

        barrier()
        AF32.off = mark
        PADW = 16
        TP = T + 4 * PADW
        xp, xpb = AF32.alloc((TP,), "xp")
        b1, b1b = AF32.alloc((TP,), "b1")
        rc, rcb = AF32.alloc((T,), "rc")
        pooled, pooledb = ABF.alloc((T,), "pooled")
        wpl, wplb = ABF.alloc((128,), "wpool")

        def segs():
            return ((PADW, 0, T_CTX), (3 * PADW + T_CTX, T_CTX, T_LAT))
        for gi, wwin in enumerate(POOL_WINDOWS):
            half = wwin // 2
            S.op("pool", lambda e: e.memset(xp, 0.0), writes=[xpb])
            S.dma("sp", lambda e, gi=gi: e.dma_start(out=rc, in_=I["rcnt"][gi:gi + 1, :].partition_broadcast(128)), writes=[rcb])
            S.dma("pool", lambda e, gi=gi: e.dma_start(out=wpl, in_=I["w_pool"][l, gi]), writes=[wplb])

            def evac_pool(ps, pb, ps2, pb2, b0, bw, nw):
                po = PADW + b0 if b0 < T_CTX else 3 * PADW + b0
                S.op("act", lambda e: e.activation(out=xp[:, po:po + bw], in_=ps[:, 0:bw], func=AF.Copy), reads=[pb], writes=[xpb])
            proj_chunk(C_POOL + gi * 128, 128, evac_pool)
            src, srcb, dst, dstb = xp, xpb, b1, b1b
            m = 1
            first = True
            while m < wwin:
                if first:
                    S.op("dve", lambda e, m=m: e.tensor_tensor(out=b1[:, 0:TP - m], in0=xp[:, 0:TP - m], in1=xp[:, m:TP],
                                                               op=ALU.add), reads=[xpb], writes=[b1b])
                    first = False
                else:
                    S.op("dve", lambda e, m=m: e.tensor_tensor(out=b1[:, 0:TP - m], in0=b1[:, 0:TP - m], in1=b1[:, m:TP],
                                                               op=ALU.add), reads=[b1b], writes=[b1b])
                m *= 2
            for (po, t0, n) in segs():
                S.op("dve", lambda e, po=po, t0=t0, n=n, half=half: e.tensor_tensor(
                    out=b1[:, po - half:po - half + n], in0=b1[:, po - half:po - half + n], in1=rc[:, t0:t0 + n], op=ALU.mult),
                    reads=[b1b, rcb], writes=[b1b])
                S.op("dve", lambda e, po=po, t0=t0, n=n, half=half: e.tensor_tensor(
                    out=pooled[:, t0:t0 + n], in0=b1[:, po - half:po - half + n], in1=xp[:, po:po + n], op=ALU.subtract),
                    reads=[b1b, xpb], writes=[pooledb])
            for (b0, bw) in BLKS:
                ps, pb = P(6)
                S.op("pe", lambda e, ps=ps, b0=b0, bw=bw: e.matmul(ps[:, 0:bw], lhsT=wpl, rhs=pooled[:, b0:b0 + bw], start=True,
                                                                   stop=True), reads=[wplb, pooledb], writes=[pb])
                sg, sgb = next_stg()
                S.op("act", lambda e, ps=ps, sg=sg, bw=bw, gi=gi: e.activation(
                    out=sg[:, 0:bw], in_=ps[:, 0:bw], func=AF.Identity, scale=smallv[:, l, 58 + gi:59 + gi]),
                    reads=[pb, cb["smallv"]], writes=[sgb])
                S.dma("sp", lambda e, sg=sg, b0=b0, bw=bw, gi=gi: e.dma_start(out=OP[gi][:, b0:b0 + bw], in_=sg[:, 0:bw]),
                      reads=[sgb], writes=[dbufs["OP"]])
        return ssq_kv, ssq_kvb, ssq_q, ssq_qb, mark

    def rstd_inplace(ssq, ssqb, n, tmp, tmpb):
        S.op("dve", lambda e: e.tensor_scalar(out=ssq, in0=ssq, scalar1=1.0 / n, scalar2=EPS, op0=ALU.mult, op1=ALU.add),
             reads=[ssqb], writes=[ssqb])
        S.op("act", lambda e: e.activation(out=tmp, in_=ssq, func=AF.Sqrt), reads=[ssqb], writes=[tmpb])
        S.op("dve", lambda e: e.reciprocal(out=ssq, in_=tmp), reads=[tmpb], writes=[ssqb])

    def phase_upproj(l, ssq_kv, ssq_kvb, ssq_q, ssq_qb, mark):
        barrier()
        ABF.reset()
        AF32.off = mark
        tmpT, tmpTb = AF32.alloc((T,), "tmpT")
        rstd_inplace(ssq_kv, ssq_kvb, KV_RANK, tmpT, tmpTb)
        rstd_inplace(ssq_q, ssq_qb, Q_RANK, tmpT, tmpTb)
        rtm, rtmb = AF32.alloc((NT,), "rtm")
        ps, pb = P()
        for tt in range(NT):
            S.op("pe", lambda e, tt=tt: e.transpose(ps[:, tt:tt + 1], ssq_kv[0:1, tt * 128:(tt + 1) * 128], ident[0:1, 0:1]),
                 reads=[ssq_kvb, cb["ident"]], writes=[pb])
        S.op("dve", lambda e: e.tensor_copy(out=rtm, in_=ps[:, 0:NT]), reads=[pb], writes=[rtmb])
        ropc, ropcb = AF32.alloc((T,), "ropc")
        rops, ropsb = AF32.alloc((T,), "rops")
        S.dma("sp", lambda e: e.dma_start(out=ropc, in_=I["ropc"]), writes=[ropcb])
        S.dma("sp", lambda e: e.dma_start(out=rops, in_=I["rops"]), writes=[ropsb])
        t1, t1b = AF32.alloc((512,), "t1")
        t2, t2b = AF32.alloc((512,), "t2")
        ckg, ckgb = ABF.alloc((4, T), "ckg")
        cqg, cqgb = ABF.alloc((6, T), "cqg")
        S.dma("sp", lambda e: e.dma_start(out=ckg, in_=CKG.rearrange("k p t -> p k t")), reads=[dbufs["CKG"]], writes=[ckgb])
        S.dma("sp", lambda e: e.dma_start(out=cqg, in_=CQG.rearrange("k p t -> p k t")), reads=[dbufs["CQG"]], writes=[cqgb])
        wcs = [ABF.alloc((6, 128), f"wc{i}") for i in range(2)]
        wcp, wcpb = ABF.alloc((6, 128), "wcp")
        stg = [ABF.alloc((512,), f"stgu{i}") for i in range(3)]
        wv, wvb = ABF.alloc((4, 1024), "wv")
        vst = [ABF.alloc((8, 129), f"vstm{i}") for i in range(2)]
        ctr = [0, 0]

        def nstg():
            s = stg[ctr[1] % 3]
            ctr[1] += 1
            return s

        def nw():
            s = wcs[ctr[0] % 2]
            ctr[0] += 1
            return s
        for h in range(8):
            w, wb = nw()
            load_w_chunk(w[:, 0:4, :], wb, I["w_ukv"][l][:, h * 256:h * 256 + 128])
            for (b0, bw) in BLKS:
                ps, pb = P(6)
                for k in range(4):
                    S.op("pe", lambda e, ps=ps, k=k, b0=b0, bw=bw, w=w: e.matmul(
                        ps[:, 0:bw], lhsT=w[:, k, :], rhs=ckg[:, k, b0:b0 + bw], start=(k == 0), stop=(k == 3)),
                        reads=[wb, ckgb], writes=[pb])
                sg, sgb = nstg()
                S.op("dve", lambda e, ps=ps, sg=sg, b0=b0, bw=bw: e.tensor_tensor(
                    out=sg[:, 0:bw], in0=ps[:, 0:bw], in1=ssq_kv[:, b0:b0 + bw], op=ALU.mult), reads=[pb, ssq_kvb], writes=[sgb])
                S.dma("sp", lambda e, sg=sg, b0=b0, bw=bw, h=h: e.dma_start(out=KN[h][:, b0:b0 + bw], in_=sg[:, 0:bw]),
                      reads=[sgb], writes=[dbufs["KN"]])
            w, wb = nw()
            load_w_chunk(w, wb, I["w_uq"][l][:, h * 192:h * 192 + 128])
            for (b0, bw) in BLKS:
                ps, pb = P(6)
                for k in range(6):
                    S.op("pe", lambda e, ps=ps, k=k, b0=b0, bw=bw, w=w: e.matmul(
                        ps[:, 0:bw], lhsT=w[:, k, :], rhs=cqg[:, k, b0:b0 + bw], start=(k == 0), stop=(k == 5)),
                        reads=[wb, cqgb], writes=[pb])
                sg, sgb = nstg()
                S.op("dve", lambda e, ps=ps, sg=sg, b0=b0, bw=bw: e.tensor_tensor(
                    out=sg[:, 0:bw], in0=ps[:, 0:bw], in1=ssq_q[:, b0:b0 + bw], op=ALU.mult), reads=[pb, ssq_qb], writes=[sgb])
                S.dma("sp", lambda e, sg=sg, b0=b0, bw=bw, h=h: e.dma_start(out=QN[h][:, b0:b0 + bw], in_=sg[:, 0:bw]),
                      reads=[sgb], writes=[dbufs["QN"]])
            load_w_chunk(wv[:, :, h * 128:(h + 1) * 128], wvb, I["w_ukv"][l][:, h * 256 + 128:h * 256 + 256])
        for hp in range(4):
            w, wb = nw()
            for i in range(2):
                hh = 2 * hp + i
                load_w_chunk(w[:, :, i * 64:(i + 1) * 64], wb, I["w_uq"][l][:, hh * 192 + 128:hh * 192 + 192])
            make_rot_w(w, wb, wcp, wcpb)
            for (b0, bw) in BLKS:
                ps, pb = P(6)
                ps2, pb2 = P(6)
                for k in range(6):
                    S.op("pe", lambda e, ps=ps, k=k, b0=b0, bw=bw, w=w: e.matmul(
                        ps[:, 0:bw], lhsT=w[:, k, :], rhs=cqg[:, k, b0:b0 + bw], start=(k == 0), stop=(k == 5)),
                        reads=[wb, cqgb], writes=[pb])
                for k in range(6):
                    S.op("pe", lambda e, ps2=ps2, k=k, b0=b0, bw=bw: e.matmul(
                        ps2[:, 0:bw], lhsT=wcp[:, k, :], rhs=cqg[:, k, b0:b0 + bw], start=(k == 0), stop=(k == 5)),
                        reads=[wcpb, cqgb], writes=[pb2])
                sg, sgb = nstg()
                S.op("dve", lambda e, ps=ps, b0=b0, bw=bw: e.tensor_tensor(out=t1[:, 0:bw], in0=ps[:, 0:bw], in1=ropc[:, b0:b0 + bw],
                                                                        op=ALU.mult), reads=[pb, ropcb], writes=[t1b])
                S.op("dve", lambda e, ps2=ps2, b0=b0, bw=bw: e.tensor_tensor(out=t2[:, 0:bw], in0=ps2[:, 0:bw], in1=rops[:, b0:b0 + bw],
                                                                         op=ALU.mult), reads=[pb2, ropsb], writes=[t2b])
                S.op("pool", lambda e, bw=bw: e.tensor_tensor(out=t1[:, 0:bw], in0=t1[:, 0:bw], in1=t2[:, 0:bw], op=ALU.add),
                     reads=[t1b, t2b], writes=[t1b])
                S.op("dve", lambda e, sg=sg, b0=b0, bw=bw: e.tensor_tensor(out=sg[:, 0:bw], in0=t1[:, 0:bw], in1=ssq_q[:, b0:b0 + bw],
                                                                        op=ALU.mult), reads=[t1b, ssq_qb], writes=[sgb])
                for i in range(2):
                    S.dma("sp", lambda e, sg=sg, b0=b0, bw=bw, i=i, hp=hp: e.dma_start(
                        out=QR[2 * hp + i][:, b0:b0 + bw], in_=sg[i * 64:(i + 1) * 64, 0:bw]), reads=[sgb], writes=[dbufs["QR"]])
        for i in range(2):
            S.op("pool", lambda e, i=i: e.memset(vst[i][0], 1.0), writes=[vst[i][1]])
        for tt in range(NT):
            v, vb = vst[tt % 2]
            for half in range(2):
                ps, pb = P(6)
                for k in range(4):
                    S.op("pe", lambda e, ps=ps, k=k, tt=tt, half=half: e.matmul(
                        ps[:, 0:512], lhsT=ckg[:, k, tt * 128:(tt + 1) * 128], rhs=wv[:, k, half * 512:(half + 1) * 512],
                        start=(k == 0), stop=(k == 3)), reads=[ckgb, wvb], writes=[pb])
                S.op("act", lambda e, ps=ps, v=v, half=half, tt=tt: e.activation(
                    out=v[:, half * 4:(half + 1) * 4, 0:128], in_=ps[:, 0:512].rearrange("p (h d) -> p h d", d=128),
                    func=AF.Identity, scale=rtm[:, tt:tt + 1]), reads=[pb, rtmb], writes=[vb])
            S.dma("sp", lambda e, v=v, tt=tt: e.dma_start(out=VM[tt], in_=v.rearrange("p h d -> p (h d)")), reads=[vb],
                  writes=[dbufs["VM"]])

    def phase_attn(l):
        new_phase()
        kr, krb = ABF.alloc((T,), "kr")
        S.dma("sp", lambda e: e.dma_start(out=kr[0:64, :], in_=KR2[0:64, :]), reads=[dbufs["KR"]], writes=[krb])
        ops = [ABF.alloc((T,), f"kq{i}") for i in range(8)]
        vhs = [ABF.alloc((NT, 129), f"vh{i}") for i in range(2)]
        est = [ABF.alloc((512,), f"est{i}") for i in range(4)]
        ostg = [ABF.alloc((512,), f"ostg{i}") for i in range(2)]
        on = [AF32.alloc((128,), f"on{i}") for i in range(4)]
        ona = [AF32.alloc((4, 128), f"ona{i}") for i in range(2)]
        rc, rcb = AF32.alloc((8,), "rc")
        sqd, sqdb = AF32.alloc((128,), "sqd")
        gs, gsb = AF32.alloc((2,), "gs")
        lam_init = 0.8 - 0.6 * math.exp(-0.3 * l)
        S.op("dve", lambda e: e.tensor_scalar(out=gs[:, 0:1], in0=smallv[:, l, 62:63], scalar1=1.0 - lam_init, scalar2=None,
                                              op0=ALU.mult), reads=[cb["smallv"]], writes=[gsb])
        qblocks = []
        for (b0, bw) in BLKS:
            qblocks.append((b0, bw, NTC if b0 < T_CTX else NT))
        ectr = [0]

        rcp, rcpb = AF32.alloc((512,), "rcp")
        o0, o0b = AF32.alloc((512,), "o0f")
        o1, o1b = AF32.alloc((512,), "o1f")
        sq5, sq5b = AF32.alloc((512,), "sq5")
        t5, t5b = AF32.alloc((512,), "t5")
        OB, SB, QB = 4, 5, 6

        accs = [AF32.alloc((512,), "accA") + ("dve",), AF32.alloc((512,), "accB") + ("pool",)]

        def attend(kpart, qpart, vh, vhb, scale, q0, qw, nkt):
            def emit_pv(kti, es_, esb):
                S.op("pe", lambda e: e.matmul(psum[OB][:, 0:qw], lhsT=vh[:, kti, 0:128], rhs=es_[:, 0:qw],
                                              start=(kti == 0), stop=(kti == nkt - 1)),
                     reads=[esb, vhb], writes=[pbuf[OB]], sig=True)
            prev = None
            n = len(kpart)
            for kti in range(nkt):
                ps, pb = P(3)
                for i, ((ka, kb_), (qa, qb_)) in enumerate(zip(kpart, qpart)):
                    S.op("pe", lambda e, ps=ps, ka=ka, qa=qa, i=i, kti=kti: e.matmul(
                        ps[:, 0:qw], lhsT=ka[:, kti * 128:(kti + 1) * 128], rhs=qa[:, q0:q0 + qw], start=(i == 0), stop=(i == n - 1)),
                        reads=[kb_, qb_], writes=[pb], sig=(i == n - 1))
                if prev is not None:
                    emit_pv(*prev)
                es_, esb = est[ectr[0] % 4]
                ectr[0] += 1
                S.op("act", lambda e, ps=ps, es_=es_: e.activation(out=es_[:, 0:qw], in_=ps[:, 0:qw], func=AF.Exp, scale=scale),
                     reads=[pb], writes=[esb])
                acc, accb, eng = accs[kti % 2]
                if kti < 2:
                    S.op(eng, lambda e, acc=acc, es_=es_: e.tensor_copy(out=acc[:, 0:qw], in_=es_[:, 0:qw]), reads=[esb], writes=[accb])
                else:
                    S.op(eng, lambda e, acc=acc, es_=es_: e.tensor_tensor(out=acc[:, 0:qw], in0=acc[:, 0:qw], in1=es_[:, 0:qw], op=ALU.add),
                         reads=[esb, accb], writes=[accb])
                prev = (kti, es_, esb)
            emit_pv(*prev)
            parts = accs[0:min(2, nkt)]
            for i, (acc, accb, _) in enumerate(parts):
                S.op("pe", lambda e, acc=acc, i=i: e.matmul(psum[SB][:, 0:qw], lhsT=ones_f[:], rhs=acc[:, 0:qw], start=(i == 0),
                                                          stop=(i == len(parts) - 1)),
                     reads=[accb, cb["ones"]], writes=[pbuf[SB]], sig=(i == len(parts) - 1))

        octr = [0]
        for h in range(MLA_HEADS):
            (kt, ktb), (qt, qtb), (qr, qrb) = ops[(h % 2) * 3:(h % 2) * 3 + 3]
            vh, vhb = vhs[h % 2]
            S.dma("sp", lambda e, kt=kt, h=h: e.dma_start(out=kt, in_=KN[h]), reads=[dbufs["KN"]], writes=[ktb])
            S.dma("sp", lambda e, qt=qt, h=h: e.dma_start(out=qt, in_=QN[h]), reads=[dbufs["QN"]], writes=[qtb])
            S.dma("sp", lambda e, qr=qr, h=h: e.dma_start(out=qr[0:64, :], in_=QR[h]), reads=[dbufs["QR"]], writes=[qrb])
            S.dma("sp", lambda e, vh=vh, h=h: e.dma_start(out=vh, in_=VM.rearrange("n p d -> p n d")[:, :, h * 129:(h + 1) * 129]),
                  reads=[dbufs["VM"]], writes=[vhb])
            for (q0, qw, nkt) in qblocks:
                attend([(kt, ktb), (kr[0:64, :], krb)], [(qt, qtb), (qr[0:64, :], qrb)], vh, vhb, 192.0 ** -0.5, q0, qw, nkt)
                og, ogb = ostg[octr[0] % 2]
                octr[0] += 1
                S.op("dve", lambda e, qw=qw: e.reciprocal(out=rcp[:, 0:qw], in_=psum[SB][:, 0:qw]), reads=[pbuf[SB]], writes=[rcpb])
                S.op("dve", lambda e, qw=qw, og=og: e.tensor_tensor(out=og[:, 0:qw], in0=psum[OB][:, 0:qw], in1=rcp[:, 0:qw], op=ALU.mult),
                     reads=[pbuf[OB], rcpb], writes=[ogb])
                S.dma("sp", lambda e, og=og, q0=q0, qw=qw, h=h: e.dma_start(out=OM[h][:, q0:q0 + qw], in_=og[:, 0:qw]),
                      reads=[ogb], writes=[dbufs["OM"]])
        for h in range(DIFF_HEADS):
            (kd, kdb), (qd, qdb) = ops[6:8] if h % 2 else ops[0:2]
            vh, vhb = vhs[h % 2]
            S.dma("sp", lambda e, kd=kd, h=h: e.dma_start(out=kd, in_=KD[h]), reads=[dbufs["KD"]], writes=[kdb])
            S.dma("sp", lambda e, qd=qd, h=h: e.dma_start(out=qd, in_=QD[h]), reads=[dbufs["QD"]], writes=[qdb])
            S.dma("sp", lambda e, vh=vh, h=h: e.dma_start(out=vh[:, :, :], in_=VD.rearrange("n p d -> p n d")[:, :, h * 129:(h + 1) * 129]),
                  reads=[dbufs["VD"]], writes=[vhb])
            for (q0, qw, nkt) in qblocks:
                for m, (om_, omb_) in enumerate(((o0, o0b), (o1, o1b))):
                    attend([(kd[m * 64:(m + 1) * 64, :], kdb)], [(qd[m * 64:(m + 1) * 64, :], qdb)], vh, vhb, 64.0 ** -0.5, q0, qw, nkt)
                    S.op("dve", lambda e, qw=qw: e.reciprocal(out=rcp[:, 0:qw], in_=psum[SB][:, 0:qw]), reads=[pbuf[SB]], writes=[rcpb])
                    S.op("dve", lambda e, qw=qw, om_=om_: e.tensor_tensor(out=om_[:, 0:qw], in0=psum[OB][:, 0:qw], in1=rcp[:, 0:qw],
                                                                        op=ALU.mult), reads=[pbuf[OB], rcpb], writes=[omb_])
                og, ogb = ostg[octr[0] % 2]
                octr[0] += 1
                S.op("dve", lambda e, qw=qw: e.scalar_tensor_tensor(out=o0[:, 0:qw], in0=o1[:, 0:qw], scalar=lamt[:, l, 5:6],
                                                                    in1=o0[:, 0:qw], op0=ALU.mult, op1=ALU.add),
                     reads=[o0b, o1b, cb["lamt"]], writes=[o0b])
                S.op("act", lambda e, qw=qw: e.activation(out=sq5[:, 0:qw], in_=o0[:, 0:qw], func=AF.Square), reads=[o0b], writes=[sq5b])
                S.op("pe", lambda e, qw=qw: e.matmul(psum[QB][:, 0:qw], lhsT=ones_f[:], rhs=sq5[:, 0:qw], start=True, stop=True),
                     reads=[sq5b, cb["ones"]], writes=[pbuf[QB]])
                S.op("dve", lambda e, qw=qw: e.tensor_copy(out=t5[:, 0:qw], in_=psum[QB][:, 0:qw]), reads=[pbuf[QB]], writes=[t5b])
                S.op("dve", lambda e, qw=qw: e.tensor_scalar(out=t5[:, 0:qw], in0=t5[:, 0:qw], scalar1=1.0 / 128, scalar2=EPS,
                                                             op0=ALU.mult, op1=ALU.add), reads=[t5b], writes=[t5b])
                S.op("act", lambda e, qw=qw: e.activation(out=t5[:, 0:qw], in_=t5[:, 0:qw], func=AF.Sqrt), reads=[t5b], writes=[t5b])
                S.op("dve", lambda e, qw=qw: e.reciprocal(out=t5[:, 0:qw], in_=t5[:, 0:qw]), reads=[t5b], writes=[t5b])
                S.op("dve", lambda e, qw=qw: e.tensor_tensor(out=o0[:, 0:qw], in0=o0[:, 0:qw], in1=t5[:, 0:qw], op=ALU.mult),
                     reads=[o0b, t5b], writes=[o0b])
                S.op("act", lambda e, qw=qw, og=og: e.activation(out=og[:, 0:qw], in_=o0[:, 0:qw], func=AF.Identity, scale=gs[:, 0:1]),
                     reads=[o0b, gsb], writes=[ogb])
                S.dma("sp", lambda e, og=og, q0=q0, qw=qw, h=h: e.dma_start(out=OD[h][:, q0:q0 + qw], in_=og[:, 0:qw]),
                      reads=[ogb], writes=[dbufs["OD"]])

    def phase_merge(l):
        new_phase()
        om, omb = ABF.alloc((8, T), "om")
        od, odb = ABF.alloc((4, T), "od")
        opp, oppb = ABF.alloc((4, T), "opp")
        S.dma("sp", lambda e: e.dma_start(out=om, in_=OM.rearrange("k p t -> p k t")), reads=[dbufs["OM"]], writes=[omb])
        S.dma("sp", lambda e: e.dma_start(out=od, in_=OD.rearrange("k p t -> p k t")), reads=[dbufs["OD"]], writes=[odb])
        S.dma("sp", lambda e: e.dma_start(out=opp, in_=OP.rearrange("k p t -> p k t")), reads=[dbufs["OP"]], writes=[oppb])
        wbs = [ABF.alloc((16, 128), f"wb{i}") for i in range(2)]
        gts = [ABF.alloc((3, 512), f"gt{i}") for i in range(2)]
        stg = [ABF.alloc((512,), f"stgm{i}") for i in range(2)]
        ta, tab = AF32.alloc((512,), "ta")
        tb, tbb = AF32.alloc((512,), "tb")
        ctr = 0
        for j in range(16):
            w, wb = wbs[j % 2]
            load_w_chunk(w[:, 0:8, :], wb, I["w_br_mla"][l][:, j * 128:(j + 1) * 128])
            load_w_chunk(w[:, 8:12, :], wb, I["w_br_diff"][l][:, j * 128:(j + 1) * 128])
            load_w_chunk(w[:, 12:16, :], wb, I["w_br_pool"][l][:, j * 128:(j + 1) * 128])
            for (b0, bw) in BLKS:
                g, gb = gts[ctr % 2]
                sg, sgb = stg[ctr % 2]
                ctr += 1
                for i in range(3):
                    S.dma("sp", lambda e, g=g, i=i, b0=b0, bw=bw, j=j: e.dma_start(out=g[:, i, 0:bw], in_=GT[i * 16 + j][:, b0:b0 + bw]),
                          reads=[dbufs["GT"]], writes=[gb])
                pss = []
                for (src_, srcb_, k0, nk) in ((om, omb, 0, 8), (od, odb, 8, 4), (opp, oppb, 12, 4)):
                    ps, pb = P(6)
                    for k in range(nk):
                        S.op("pe", lambda e, ps=ps, k=k, k0=k0, nk=nk, src_=src_, b0=b0, bw=bw, w=w: e.matmul(
                            ps[:, 0:bw], lhsT=w[:, k0 + k, :], rhs=src_[:, k, b0:b0 + bw], start=(k == 0), stop=(k == nk - 1)),
                            reads=[wb, srcb_], writes=[pb], sig=(k == nk - 1))
                    pss.append((ps, pb))
                S.op("dve", lambda e, p=pss[0][0], g=g, bw=bw: e.tensor_tensor(out=ta[:, 0:bw], in0=p[:, 0:bw], in1=g[:, 0, 0:bw],
                                                                            op=ALU.mult), reads=[pss[0][1], gb], writes=[tab])
                S.op("dve", lambda e, p=pss[1][0], g=g, bw=bw: e.tensor_tensor(out=tb[:, 0:bw], in0=p[:, 0:bw], in1=g[:, 1, 0:bw],
                                                                            op=ALU.mult), reads=[pss[1][1], gb], writes=[tbb])
                S.op("pool", lambda e, bw=bw: e.tensor_tensor(out=ta[:, 0:bw], in0=ta[:, 0:bw], in1=tb[:, 0:bw], op=ALU.add),
                     reads=[tab, tbb], writes=[tab])
                S.op("dve", lambda e, p=pss[2][0], g=g, bw=bw: e.tensor_tensor(out=tb[:, 0:bw], in0=p[:, 0:bw], in1=g[:, 2, 0:bw],
                                                                            op=ALU.mult), reads=[pss[2][1], gb], writes=[tbb])
                S.op("pool", lambda e, sg=sg, bw=bw: e.tensor_tensor(out=sg[:, 0:bw], in0=ta[:, 0:bw], in1=tb[:, 0:bw], op=ALU.add),
                     reads=[tab, tbb], writes=[sgb])
                S.dma("sp", lambda e, sg=sg, b0=b0, bw=bw, j=j: e.dma_start(out=MT[j][:, b0:b0 + bw], in_=sg[:, 0:bw]),
                      reads=[sgb], writes=[dbufs["MT"]])

    def deepnorm_tile(src4, xt, xb, t, tb_, sq, sqb, st, stb, gbc, gbcb, lng, lngb, lnb, lnbb):
        for nb, (ya, yb) in enumerate(src4):
            S.op("dve", lambda e, ya=ya, nb=nb: e.tensor_tensor(out=t[:, nb * 512:(nb + 1) * 512], in0=ya,
                                                               in1=gbc[:, nb * 512:(nb + 1) * 512], op=ALU.mult),
                 reads=[yb, gbcb], writes=[tb_])
        S.op("dve", lambda e: e.scalar_tensor_tensor(out=t, in0=xt, scalar=DN_ALPHA, in1=t, op0=ALU.mult, op1=ALU.add),
             reads=[xb, tb_], writes=[tb_])
        layernorm(t, tb_, xt, xb, sq, sqb, st, stb)
        S.op("dve", lambda e: e.tensor_tensor(out=xt, in0=xt, in1=lng, op=ALU.mult), reads=[xb, lngb], writes=[xb])
        S.op("pool", lambda e: e.tensor_tensor(out=xt, in0=xt, in1=lnb, op=ALU.add), reads=[xb, lnbb], writes=[xb])

    def phase_wo(l):
        new_phase()
        wo, wob = ABF.alloc((16, D), "wo")
        for nb in range(4):
            load_w_chunk(wo[:, :, nb * 512:(nb + 1) * 512], wob, I["w_o"][l][:, nb * 512:(nb + 1) * 512])
        mts = [ABF.alloc((16, 128), f"mt{i}") for i in range(2)]
        gl, glb = AF32.alloc((D,), "g1lat")
        gc, gcb = AF32.alloc((D,), "g1ctx")
        lng, lngb = AF32.alloc((D,), "lng")
        lnb, lnbb = AF32.alloc((D,), "lnb")
        S.dma("sp", lambda e: e.dma_start(out=gl, in_=GROW[l, 0, 0:1, :].partition_broadcast(128)), reads=[dbufs["GROW"]], writes=[glb])
        S.dma("sp", lambda e: e.dma_start(out=gc, in_=GROW[l, 0, 1:2, :].partition_broadcast(128)), reads=[dbufs["GROW"]], writes=[gcb])
        S.dma("sp", lambda e: e.dma_start(out=lng, in_=I["ln1_g"][l:l + 1, :].partition_broadcast(128)), writes=[lngb])
        S.dma("sp", lambda e: e.dma_start(out=lnb, in_=I["ln1_b"][l:l + 1, :].partition_broadcast(128)), writes=[lnbb])
        xt, xb = AF32.alloc((D,), "xtw")
        t, tb_ = AF32.alloc((D,), "tw")
        sq, sqb = AF32.alloc((D,), "sqw")
        st, stb = AF32.alloc((4,), "stw")
        for tt in range(NT):
            mt, mtb = mts[tt % 2]
            S.dma("sp", lambda e, mt=mt, tt=tt: e.dma_start(out=mt, in_=MT.rearrange("k p t -> p k t")[:, :, tt * 128:(tt + 1) * 128]),
                  reads=[dbufs["MT"]], writes=[mtb])
            S.dma("sp", lambda e, tt=tt: e.dma_start(out=xt, in_=XS[tt * 128:(tt + 1) * 128, :]), reads=[dbufs["XS"]], writes=[xb])
            src4 = []
            for nb in range(4):
                b = 4 + nb
                for k in range(16):
                    S.op("pe", lambda e, b=b, k=k, nb=nb, mt=mt: e.matmul(psum[b][:, 0:512], lhsT=mt[:, k, :],
                                                                        rhs=wo[:, k, nb * 512:(nb + 1) * 512], start=(k == 0),
                                                                        stop=(k == 15)), reads=[mtb, wob], writes=[pbuf[b]], sig=(k == 15))
                src4.append((psum[b][:, 0:512], pbuf[b]))
            g, gb_ = (gc, gcb) if is_ctx_tile(tt) else (gl, glb)
            deepnorm_tile(src4, xt, xb, t, tb_, sq, sqb, st, stb, g, gb_, lng, lngb, lnb, lnbb)
            S.dma("sp", lambda e, tt=tt: e.dma_start(out=XS[tt * 128:(tt + 1) * 128, :], in_=xt), reads=[xb], writes=[dbufs["XS"]])

    NR = NG + NE
    NSLOT = NE * CAP
    regs = {}

    def _init_pool(eng):
        regs["bc"] = eng.alloc_register("bc")
        eng.reg_mov(regs["bc"], NSLOT - 1)
    S.init.setdefault("pool", []).append(_init_pool)

    def phase_route(l):
        new_phase()
        for i in range(NSLOT // 128):
            S.dma("sp", lambda e, i=i: e.dma_start(out=XSORT[i * 128:(i + 1) * 128, :], in_=zero_b[:]), reads=[cb["zero"]],
                  writes=[dbufs["XSORT"]])
        S.op("pool", lambda e: e.memset(ebase[:], 0.0), writes=[cb["ebase"]])
        wr, wrb = AF32.alloc((16, NR), "wr")
        rb, rbb = AF32.alloc((NR,), "rb")
        S.dma("sp", lambda e: e.dma_start(out=wr[:, :, 0:NG], in_=I["w_grp"][l][:, 0:NG].rearrange("(k p) n -> p k n", p=128), allow_slow_non_contiguous=True), writes=[wrb])
        S.dma("sp", lambda e: e.dma_start(out=wr[:, :, NG:NR], in_=I["w_exp"][l][:, 0:NE].rearrange("(k p) n -> p k n", p=128), allow_slow_non_contiguous=True), writes=[wrb])
        S.dma("sp", lambda e: e.dma_start(out=rb[:, 0:NG], in_=I["b_grp"][l:l + 1, 0:NG].partition_broadcast(128)), writes=[rbb])
        S.dma("sp", lambda e: e.dma_start(out=rb[:, NG:NR], in_=I["b_exp"][l:l + 1, 0:NE].partition_broadcast(128)), writes=[rbb])
        xt, xb = AF32.alloc((D,), "xtr")
        xn, xnb = AF32.alloc((D,), "xnr")
        sq, sqb = AF32.alloc((D,), "sqr")
        vT, vTb = AF32.alloc((16, 128), "vT")
        st, stb = AF32.alloc((4,), "str")
        lg, lgb = AF32.alloc((NR,), "lg")
        ge, geb = AF32.alloc((NG,), "ge")
        pen, penb = AF32.alloc((NG,), "pen")
        msk, mskb = AF32.alloc((NE,), "msk")
        msk2, msk2b = AF32.alloc((NE,), "msk2")
        ohs = [AF32.alloc((NE,), f"oh{i}") for i in range(2)]
        pos, posb = AF32.alloc((NE,), "pos")
        tmpe, tmpeb = AF32.alloc((NE,), "tmpe")
        s_, sb_ = AF32.alloc((16,), "rs")
        vbs = [ABF.alloc((D,), f"vb{i}") for i in range(2)]
        for tt in range(NT):
            r = 1 if is_ctx_tile(tt) else 0
            S.dma("sp", lambda e, tt=tt: e.dma_start(out=xt, in_=XS[tt * 128:(tt + 1) * 128, :]), reads=[dbufs["XS"]], writes=[xb])
            layernorm(xt, xb, xn, xnb, sq, sqb, st, stb)
            for k in range(16):
                if k % 4 == 0:
                    ps, pb = P(4)
                S.op("pe", lambda e, ps=ps, k=k: e.transpose(ps[:, (k % 4) * 128:(k % 4 + 1) * 128], xn[:, k * 128:(k + 1) * 128],
                                                             ident[:]), reads=[xnb, cb["ident"]], writes=[pb])
                S.op("act", lambda e, ps=ps, k=k, r=r: e.activation(
                    out=vT[:, k, :], in_=ps[:, (k % 4) * 128:(k % 4 + 1) * 128], func=AF.Identity,
                    scale=mod1[:, l, 64 + k, r:r + 1], bias=mod[:, l, 48 + k, r:r + 1]), reads=[pb, cb["mod"]], writes=[vTb])
            pr, prb = P(2, 4)
            for k in range(16):
                S.op("pe", lambda e, k=k, pr=pr: e.matmul(pr[:, 0:NR], lhsT=vT[:, k, :], rhs=wr[:, k, :], start=(k == 0), stop=(k == 15)),
                     reads=[vTb, wrb], writes=[prb])
            S.op("dve", lambda e, pr=pr: e.tensor_tensor(out=lg, in0=pr[:, 0:NR], in1=rb, op=ALU.add), reads=[prb, rbb], writes=[lgb])
            vb_, vbb = vbs[tt % 2]
            for k in range(16):
                if k % 4 == 0:
                    ps, pb = P(4)
                S.op("pe", lambda e, ps=ps, k=k: e.transpose(ps[:, (k % 4) * 128:(k % 4 + 1) * 128], vT[:, k, :], ident[:]),
                     reads=[vTb, cb["ident"]], writes=[pb])
                if k % 4 == 3:
                    kb = k // 4
                    if kb % 2 == 0:
                        S.op("act", lambda e, ps=ps, kb=kb, vb_=vb_: e.activation(out=vb_[:, kb * 512:(kb + 1) * 512], in_=ps[:, 0:512],
                                                                                 func=AF.Identity), reads=[pb], writes=[vbb])
                    else:
                        S.op("dve", lambda e, ps=ps, kb=kb, vb_=vb_: e.tensor_copy(out=vb_[:, kb * 512:(kb + 1) * 512], in_=ps[:, 0:512]),
                             reads=[pb], writes=[vbb])
            S.op("dve", lambda e: e.tensor_reduce(out=s_[:, 0:1], in_=lg[:, 0:NG], axis=AX.X, op=ALU.max), reads=[lgb], writes=[sb_])
            S.op("dve", lambda e: e.tensor_scalar(out=ge, in0=lg[:, 0:NG], scalar1=s_[:, 0:1], scalar2=None, op0=ALU.subtract),
                 reads=[lgb, sb_], writes=[geb])
            S.op("act", lambda e: e.activation(out=ge, in_=ge, func=AF.Exp), reads=[geb], writes=[geb])
            S.op("dve", lambda e: e.tensor_reduce(out=s_[:, 1:2], in_=ge, axis=AX.X, op=ALU.add), reads=[geb], writes=[sb_])
            S.op("dve", lambda e: e.reciprocal(out=s_[:, 2:3], in_=s_[:, 1:2]), reads=[sb_], writes=[sb_])
            S.op("dve", lambda e: e.tensor_scalar(out=pen, in0=lg[:, 0:NG], scalar1=s_[:, 0:1], scalar2=None, op0=ALU.is_equal),
                 reads=[lgb, sb_], writes=[penb])
            S.op("dve", lambda e: e.tensor_scalar(out=pen, in0=pen, scalar1=-1.0, scalar2=BIG, op0=ALU.add, op1=ALU.mult),
                 reads=[penb], writes=[penb])
            for g in range(NG):
                S.op("dve", lambda e, g=g: e.tensor_scalar(out=msk[:, g * EPG:(g + 1) * EPG], in0=lg[:, NG + g * EPG:NG + (g + 1) * EPG],
                                                           scalar1=pen[:, g:g + 1], scalar2=None, op0=ALU.add),
                     reads=[lgb, penb], writes=[mskb])
            oh1, oh1b = ohs[0]
            oh2, oh2b = ohs[1]
            S.op("dve", lambda e: e.tensor_reduce(out=s_[:, 3:4], in_=msk, axis=AX.X, op=ALU.max), reads=[mskb], writes=[sb_])
            S.op("dve", lambda e: e.tensor_scalar(out=oh1, in0=msk, scalar1=s_[:, 3:4], scalar2=None, op0=ALU.is_equal),
                 reads=[mskb, sb_], writes=[oh1b])
            S.op("dve", lambda e: e.scalar_tensor_tensor(out=msk2, in0=oh1, scalar=-BIG, in1=msk, op0=ALU.mult, op1=ALU.add),
                 reads=[oh1b, mskb], writes=[msk2b])
            S.op("dve", lambda e: e.tensor_reduce(out=s_[:, 4:5], in_=msk2, axis=AX.X, op=ALU.max), reads=[msk2b], writes=[sb_])
            S.op("dve", lambda e: e.tensor_scalar(out=oh2, in0=msk2, scalar1=s_[:, 4:5], scalar2=None, op0=ALU.is_equal),
                 reads=[msk2b, sb_], writes=[oh2b])
            S.op("dve", lambda e: e.tensor_tensor(out=s_[:, 5:6], in0=s_[:, 3:4], in1=s_[:, 4:5], op=ALU.subtract), reads=[sb_], writes=[sb_])
            S.op("act", lambda e: e.activation(out=s_[:, 6:7], in_=s_[:, 5:6], func=AF.Sigmoid), reads=[sb_], writes=[sb_])
            S.op("act", lambda e: e.activation(out=s_[:, 7:8], in_=s_[:, 5:6], func=AF.Sigmoid, scale=-1.0), reads=[sb_], writes=[sb_])
            S.op("dve", lambda e, tt=tt: e.tensor_scalar(out=gatew[:, tt, :], in0=s_[:, 6:8], scalar1=s_[:, 2:3], scalar2=None,
                                                         op0=ALU.mult), reads=[sb_], writes=[cb["gatew"]])
            for kk, (oh, ohb) in enumerate(ohs):
                pp, ppb = P(2, 6)
                S.op("pe", lambda e, pp=pp, oh=oh: e.matmul(pp[:, 0:NE], lhsT=tril[:], rhs=oh, start=True, stop=True),
                     reads=[ohb, cb["tril"]], writes=[ppb])
                S.op("dve", lambda e, pp=pp: e.tensor_tensor(out=pos, in0=pp[:, 0:NE], in1=ebase[:], op=ALU.add),
                     reads=[ppb, cb["ebase"]], writes=[posb])
                S.op("dve", lambda e, oh=oh: e.tensor_tensor(out=tmpe, in0=oh, in1=pos, op=ALU.mult), reads=[ohb, posb], writes=[tmpeb])
                S.op("dve", lambda e: e.tensor_reduce(out=s_[:, 8:9], in_=tmpe, axis=AX.X, op=ALU.add), reads=[tmpeb], writes=[sb_])
                S.op("dve", lambda e, oh=oh: e.tensor_tensor(out=tmpe, in0=oh, in1=eoff[:], op=ALU.mult), reads=[ohb, cb["eoff"]],
                     writes=[tmpeb])
                S.op("dve", lambda e: e.tensor_reduce(out=s_[:, 9:10], in_=tmpe, axis=AX.X, op=ALU.add), reads=[tmpeb], writes=[sb_])
                S.op("dve", lambda e: e.tensor_scalar(out=s_[:, 10:11], in0=s_[:, 8:9], scalar1=float(CAP) - 0.5, scalar2=1.0e9,
                                                      op0=ALU.is_ge, op1=ALU.mult), reads=[sb_], writes=[sb_])
                S.op("dve", lambda e: e.tensor_tensor(out=s_[:, 8:9], in0=s_[:, 8:9], in1=s_[:, 9:10], op=ALU.add), reads=[sb_], writes=[sb_])
                S.op("dve", lambda e: e.tensor_tensor(out=s_[:, 8:9], in0=s_[:, 8:9], in1=s_[:, 10:11], op=ALU.add), reads=[sb_], writes=[sb_])
                S.op("dve", lambda e, tt=tt, kk=kk: e.tensor_copy(out=dest_i[:, tt, kk:kk + 1], in_=s_[:, 8:9]), reads=[sb_],
                     writes=[cb["dest"]])
                pt, ptb = P(2, 6)
                S.op("pe", lambda e, pt=pt, oh=oh: e.matmul(pt[:, 0:NE], lhsT=ones_f[:], rhs=oh, start=True, stop=True),
                     reads=[ohb, cb["ones"]], writes=[ptb])
                S.op("dve", lambda e, pt=pt: e.tensor_tensor(out=ebase[:], in0=ebase[:], in1=pt[:, 0:NE], op=ALU.add),
                     reads=[ptb, cb["ebase"]], writes=[cb["ebase"]])
                S.dma("pool", lambda e, tt=tt, kk=kk, vb_=vb_: e.indirect_dma_start(
                    out=XSORT, out_offset=bass.IndirectOffsetOnAxis(ap=dest_i[:, tt, kk:kk + 1], axis=0), in_=vb_, in_offset=None,
                    bounds_check=regs["bc"], oob_is_err=False), reads=[vbb, cb["dest"]], writes=[dbufs["XSORT"]])

    def phase_experts(l):
        new_phase()
        wgus = [ABF.alloc((16, 1024), f"wgu{i}") for i in range(2)]
        wdns = [ABF.alloc((4, D), f"wdn{i}") for i in range(2)]
        xes = [ABF.alloc((D,), f"xe{i}") for i in range(2)]
        xeTs = [ABF.alloc((16, 128), f"xeT{i}") for i in range(2)]
        a_s = [ABF.alloc((512,), f"aact{i}") for i in range(2)]
        aTs = [ABF.alloc((4, 128), f"aT{i}") for i in range(2)]
        sgs = [AF32.alloc((512,), f"sgl{i}") for i in range(2)]
        ys = [AF32.alloc((D,), f"ye{i}") for i in range(2)]
        NST = CAP // 128
        tiles = [(ex, s) for ex in range(NE) for s in range(NST)]
        n = len(tiles)
        dctr = [0]

        def load_gu(ex):
            if ex < NE:
                load_w_chunk(wgus[ex % 2][0], wgus[ex % 2][1], I["w_gu"][l, ex])

        def load_dn(ex):
            if ex < NE:
                load_w_chunk(wdns[ex % 2][0], wdns[ex % 2][1], I["w_dn"][l, ex])

        def stA(i):
            ex, s = tiles[i]
            r0 = ex * CAP + s * 128
            xe, xeb = xes[i % 2]
            xeT, xeTb = xeTs[i % 2]
            S.dma("sp", lambda e: e.dma_start(out=xe, in_=XSORT[r0:r0 + 128, :]), reads=[dbufs["XSORT"]], writes=[xeb])
            for kb in range(4):
                ps, pb = psum[kb % 2], pbuf[kb % 2]
                for kk in range(4):
                    k = kb * 4 + kk
                    S.op("pe", lambda e, ps=ps, k=k, kk=kk: e.matmul(ps[:, kk * 128:(kk + 1) * 128], lhsT=xe[:, k * 128:(k + 1) * 128],
                                                                    rhs=identb[:], start=True, stop=True),
                         reads=[xeb, cb["identb"]], writes=[pb], sig=(kk == 3))
                dstv = xeT[:, kb * 4:(kb + 1) * 4, :].rearrange("p a b -> p (a b)")
                if kb % 2 == 0:
                    S.op("act", lambda e, ps=ps, dstv=dstv: e.activation(out=dstv, in_=ps[:, 0:512], func=AF.Identity),
                         reads=[pb], writes=[xeTb])
                else:
                    S.op("dve", lambda e, ps=ps, dstv=dstv: e.tensor_copy(out=dstv, in_=ps[:, 0:512]), reads=[pb], writes=[xeTb])

        def stB(i):
            ex, s = tiles[i]
            wgu, wgub = wgus[ex % 2]
            xeT, xeTb = xeTs[i % 2]
            sg, sgb = sgs[i % 2]
            a_, ab_ = a_s[i % 2]
            for (bk, c0) in ((2, 0), (3, 512)):
                for k in range(16):
                    S.op("pe", lambda e, bk=bk, k=k, c0=c0: e.matmul(psum[bk][:, 0:512], lhsT=xeT[:, k, :], rhs=wgu[:, k, c0:c0 + 512],
                                                                    start=(k == 0), stop=(k == 15)),
                         reads=[xeTb, wgub], writes=[pbuf[bk]], sig=(k == 15))
            S.op("act", lambda e: e.activation(out=sg, in_=psum[2][:, 0:512], func=AF.Silu), reads=[pbuf[2]], writes=[sgb])
            S.op("dve", lambda e: e.tensor_tensor(out=a_, in0=psum[3][:, 0:512], in1=sg, op=ALU.mult), reads=[pbuf[3], sgb], writes=[ab_])
            if s == NST - 1:
                load_gu(ex + 2)

        def stC(i):
            a_, ab_ = a_s[i % 2]
            aT, aTb = aTs[i % 2]
            for k in range(4):
                S.op("pe", lambda e, k=k: e.matmul(psum[4][:, k * 128:(k + 1) * 128], lhsT=a_[:, k * 128:(k + 1) * 128], rhs=identb[:],
                                                  start=True, stop=True), reads=[ab_, cb["identb"]], writes=[pbuf[4]], sig=(k == 3))
            S.op("dve", lambda e: e.tensor_copy(out=aT.rearrange("p a b -> p (a b)"), in_=psum[4][:, 0:512]), reads=[pbuf[4]], writes=[aTb])

        def stD(i):
            ex, s = tiles[i]
            r0 = ex * CAP + s * 128
            wdn, wdnb = wdns[ex % 2]
            aT, aTb = aTs[i % 2]
            y, yb = ys[i % 2]
            for nb in range(4):
                bk = 5 + dctr[0] % 3
                dctr[0] += 1
                for k in range(4):
                    S.op("pe", lambda e, bk=bk, k=k, nb=nb: e.matmul(psum[bk][:, 0:512], lhsT=aT[:, k, :],
                                                                    rhs=wdn[:, k, nb * 512:(nb + 1) * 512], start=(k == 0), stop=(k == 3)),
                         reads=[aTb, wdnb], writes=[pbuf[bk]], sig=(k == 3))
                if nb % 2 == 0:
                    S.op("act", lambda e, bk=bk, nb=nb: e.activation(out=y[:, nb * 512:(nb + 1) * 512], in_=psum[bk][:, 0:512],
                                                                    func=AF.Identity), reads=[pbuf[bk]], writes=[yb])
                else:
                    S.op("dve", lambda e, bk=bk, nb=nb: e.tensor_copy(out=y[:, nb * 512:(nb + 1) * 512], in_=psum[bk][:, 0:512]),
                         reads=[pbuf[bk]], writes=[yb])
            S.dma("sp", lambda e: e.dma_start(out=YSORT[r0:r0 + 128, :], in_=y), reads=[yb], writes=[dbufs["YSORT"]])
            if s == NST - 1:
                load_dn(ex + 2)

        for ex in range(2):
            load_gu(ex)
            load_dn(ex)
        for it in range(n + 3):
            if it < n:
                stA(it)
            if 0 <= it - 1 < n:
                stB(it - 1)
            if 0 <= it - 2 < n:
                stC(it - 2)
            if 0 <= it - 3 < n:
                stD(it - 3)

    def phase_combine(l, last):
        new_phase()
        gl, glb = AF32.alloc((D,), "g2lat")
        gc, gcb = AF32.alloc((D,), "g2ctx")
        lng, lngb = AF32.alloc((D,), "lng2")
        lnb, lnbb = AF32.alloc((D,), "lnb2")
        S.dma("sp", lambda e: e.dma_start(out=gl, in_=GROW[l, 1, 0:1, :].partition_broadcast(128)), reads=[dbufs["GROW"]], writes=[glb])
        S.dma("sp", lambda e: e.dma_start(out=gc, in_=GROW[l, 1, 1:2, :].partition_broadcast(128)), reads=[dbufs["GROW"]], writes=[gcb])
        S.dma("sp", lambda e: e.dma_start(out=lng, in_=I["ln2_g"][l:l + 1, :].partition_broadcast(128)), writes=[lngb])
        S.dma("sp", lambda e: e.dma_start(out=lnb, in_=I["ln2_b"][l:l + 1, :].partition_broadcast(128)), writes=[lnbb])
        A, Ab = AF32.alloc((D,), "yA")
        B, Bb = AF32.alloc((D,), "yB")
        xt, xb = AF32.alloc((D,), "xtc")
        st, stb = AF32.alloc((4,), "stc")
        for tt in range(NT):
            S.op("pool", lambda e: e.memset(A, 0.0), writes=[Ab])
            S.op("pool", lambda e: e.memset(B, 0.0), writes=[Bb])
            for kk, (dst, dstb) in enumerate(((A, Ab), (B, Bb))):
                S.dma("pool", lambda e, tt=tt, kk=kk, dst=dst: e.indirect_dma_start(
                    out=dst, out_offset=None, in_=YSORT, in_offset=bass.IndirectOffsetOnAxis(ap=dest_i[:, tt, kk:kk + 1], axis=0),
                    bounds_check=regs["bc"], oob_is_err=False), reads=[dbufs["YSORT"], cb["dest"]], writes=[dstb])
            S.dma("sp", lambda e, tt=tt: e.dma_start(out=xt, in_=XS[tt * 128:(tt + 1) * 128, :]), reads=[dbufs["XS"]], writes=[xb])
            S.op("dve", lambda e, tt=tt: e.tensor_scalar(out=A, in0=A, scalar1=gatew[:, tt, 0:1], scalar2=None, op0=ALU.mult),
                 reads=[Ab, cb["gatew"]], writes=[Ab])
            S.op("dve", lambda e, tt=tt: e.scalar_tensor_tensor(out=A, in0=B, scalar=gatew[:, tt, 1:2], in1=A, op0=ALU.mult, op1=ALU.add),
                 reads=[Ab, Bb, cb["gatew"]], writes=[Ab])
            g, gb_ = (gc, gcb) if is_ctx_tile(tt) else (gl, glb)
            src4 = [(A[:, nb * 512:(nb + 1) * 512], Ab) for nb in range(4)]
            deepnorm_tile(src4, xt, xb, B, Bb, A, Ab, st, stb, g, gb_, lng, lngb, lnb, lnbb)
            S.dma("sp", lambda e, tt=tt: e.dma_start(out=XS[tt * 128:(tt + 1) * 128, :], in_=xt), reads=[xb], writes=[dbufs["XS"]])
            if last and not is_ctx_tile(tt):
                o0 = (tt - NTC) * 128
                S.dma("sp", lambda e, o0=o0: e.dma_start(out=OUT[o0:o0 + 128, :], in_=xt), reads=[xb], writes=[dbufs["OUT"]])

    def dump(dst_ap, src_ap, srcname):
        new_phase()
        n = src_ap.shape[1]
        t_, tb_ = ABF.alloc((n,), "dumpst")
        nr = src_ap.shape[0]
        S.dma("sp", lambda e: e.dma_start(out=t_[0:nr], in_=src_ap), reads=[dbufs[srcname]], writes=[tb_])
        S.dma("pool", lambda e: e.dma_start(out=dst_ap, in_=t_[0:nr]), reads=[tb_], writes=[dbufs["DBG"]])

    S.dma("sp", lambda e: e.dma_start(out=XS[0:T_CTX, :], in_=I["ctx"]), writes=[dbufs["XS"]])
    S.dma("sp", lambda e: e.dma_start(out=XS[T_CTX:T, :], in_=I["x"]), writes=[dbufs["XS"]])
    done = False
    if stop == "const":
        S.dma("sp", lambda e: e.dma_start(out=DBG[0:128, 0:128], in_=tril[:]), reads=[cb["tril"]], writes=[dbufs["DBG"]])
        done = True
    else:
        phase_adaln()
    if stop == "adaln":
        S.dma("sp", lambda e: e.dma_start(out=DBG[0:128, 0:192], in_=mod[:, 0].rearrange("p j r -> p (j r)")),
              reads=[cb["mod"]], writes=[dbufs["DBG"]])
        S.dma("sp", lambda e: e.dma_start(out=DBG[128:256, 0:64], in_=smallv[:, 0]), reads=[cb["smallv"]], writes=[dbufs["DBG"]])
        S.dma("sp", lambda e: e.dma_start(out=DBG[256:384, 0:8], in_=lamt[:, 0]), reads=[cb["lamt"]], writes=[dbufs["DBG"]])
        S.dma("sp", lambda e: e.dma_start(out=DBG[384:388, 0:D], in_=GROW[0].rearrange("a b d -> (a b) d")),
              reads=[dbufs["GROW"]], writes=[dbufs["DBG"]])
        done = True
    for l in range(0 if done else NL):
        r = phase_inproj(l)
        if stop == "uT":
            uT_, uTb_ = r
            for k in range(16):
                S.dma("pool", lambda e, k=k: e.dma_start(out=DBG[k * 128:(k + 1) * 128, 0:T], in_=uT_[:, k, :]),
                      reads=[uTb_], writes=[dbufs["DBG"]])
            done = True
            break
        if stop == "inproj":
            o = 0
            for nm, ap, rows in (("KD", KD[0], 128), ("KR", KR2, 128), ("CKG", CKG[1], 128), ("QD", QD[3], 128),
                                 ("GT", GT[5], 128), ("OP", OP[2], 128)):
                if nm in cfg.get("dumps", ("KD", "KR", "CKG", "QD", "GT", "OP")):
                    dump(DBG[o:o + rows, 0:T], ap, nm)
                o += rows
            if "VD" in cfg.get("dumps", ("VD",)):
                dump(DBG[o:o + 128, 0:516], VD[1], "VD")
            done = True
            break
        phase_upproj(l, *r)
        phase_attn(l)
        if stop == "attn":
            o = 0
            for nm, ap in (("OM", OM[0]), ("OM", OM[7]), ("OD", OD[0]), ("OD", OD[3]), ("KN", KN[2]), ("QN", QN[5]), ("QR", QR[3])):
                rows = ap.shape[0]
                dump(DBG[o:o + rows, 0:T], ap, nm)
                o += 128
            dump(DBG[o:o + 128, 0:1032], VM[1], "VM")
            done = True
            break
        phase_merge(l)
        phase_wo(l)
        if stop == "mixer":
            S.dma("sp", lambda e: e.dma_start(out=DBG[0:T, 0:D], in_=XS), reads=[dbufs["XS"]], writes=[dbufs["DBG"]])
            done = True
            break
        phase_route(l)
        if stop == "route":
            S.dma("sp", lambda e: e.dma_start(out=DBG[0:128, 0:NT * 2], in_=gatew[:].rearrange("p a b -> p (a b)")),
                  reads=[cb["gatew"]], writes=[dbufs["DBG"]])
            df, dfb = AF32.alloc((NT * 2,), "destf")
            S.op("dve", lambda e: e.tensor_copy(out=df, in_=dest_i[:].rearrange("p a b -> p (a b)")), reads=[cb["dest"]], writes=[dfb])
            S.dma("sp", lambda e: e.dma_start(out=DBG[128:256, 0:NT * 2], in_=df), reads=[dfb], writes=[dbufs["DBG"]])
            done = True
            break
        phase_experts(l)
        phase_combine(l, l == NL - 1)
        if stop == "layer" and l == 0:
            S.dma("sp", lambda e: e.dma_start(out=DBG[0:T, 0:D], in_=XS), reads=[dbufs["XS"]], writes=[dbufs["DBG"]])
            done = True
            break
    S.finish(list(dbufs.values()))
    barrier()
    S.emit()
    return nc, es, S


def host_consts(T_CTX, T_LAT, NE, CAP):
    T = T_CTX + T_LAT
    quarter = ROPE // 4
    inv_freq = (10000.0 ** (-np.arange(quarter, dtype=np.float32) / quarter)).astype(np.float32)
    rows = T_LAT // GRID_W
    row = np.repeat(np.arange(rows, dtype=np.float32), GRID_W)
    col = (np.arange(rows * GRID_W) % GRID_W).astype(np.float32)
    ang = np.stack([row[:, None] * inv_freq, col[:, None] * inv_freq], axis=1)
    cos, sin = np.cos(ang).astype(np.float32), np.sin(ang).astype(np.float32)
    c64 = np.ones((T, 2, 2, quarter), np.float32)
    s64 = np.zeros((T, 2, 2, quarter), np.float32)
    c64[T_CTX:] = cos[:, :, None, :]
    s64[T_CTX:] = sin[:, :, None, :]
    c64 = c64.reshape(T, 64).T
    s64 = s64.reshape(T, 64).T
    ropc = np.ascontiguousarray(np.concatenate([c64, c64], axis=0))
    rops = np.ascontiguousarray(np.concatenate([s64, s64], axis=0))
    rcnt = np.zeros((4, T), np.float32)
    for gi, w in enumerate(POOL_WINDOWS):
        half = w // 2
        for (s0, n) in ((0, T_CTX), (T_CTX, T_LAT)):
            t = np.arange(n)
            cnt = np.minimum(t + half, n) - np.maximum(t - half, 0)
            rcnt[gi, s0:s0 + n] = 1.0 / cnt.astype(np.float32)
    tril = (np.arange(128)[:, None] < np.arange(128)[None, :]).astype(np.float32)
    eoff = np.broadcast_to((np.arange(NE, dtype=np.float32) * CAP)[None, :], (128, NE)).copy()
    return dict(ident=np.eye(128, dtype=np.float32), ropc=ropc, rops=rops, rcnt=rcnt, tril=tril, eoff=eoff)


N_ACTIVE = 4
MOE_CAP = 384


def _per_core_inputs(b, x, c, ctx, c_ctx, weights, consts):
    m = dict(x=np.ascontiguousarray(x[b]), ctx=np.ascontiguousarray(ctx[b]),
             cvec=np.ascontiguousarray(np.stack([c[b], c_ctx])))
    m.update(weights)
    m.update(consts)
    return m


def kernel(x, c, ctx, c_ctx, w_ada, b_ada, w_in, b_gate, g_q, g_kv, w_uq, w_ukv, lam, g_sub, w_pool, pool_scale,
           w_br_mla, w_br_diff, w_br_pool, w_o, ln1_g, ln1_b, w_grp, b_grp, w_exp, b_exp, w_gu, w_dn, ln2_g, ln2_b):
    B, T_LAT, _ = x.shape
    T_CTX = ctx.shape[1]
    weights = dict(w_ada=w_ada, b_ada=b_ada, w_in=w_in, b_gate=np.reshape(b_gate, (DEPTH, 3 * D)), g_q=g_q, g_kv=g_kv,
                   w_uq=w_uq, w_ukv=w_ukv, lam=np.reshape(lam, (DEPTH, 4 * DIFF_HD)), g_sub=g_sub, w_pool=w_pool,
                   pool_scale=pool_scale, w_br_mla=w_br_mla, w_br_diff=w_br_diff, w_br_pool=w_br_pool, w_o=w_o,
                   ln1_g=ln1_g, ln1_b=ln1_b, w_grp=w_grp, b_grp=b_grp, w_exp=w_exp, b_exp=b_exp, w_gu=w_gu, w_dn=w_dn,
                   ln2_g=ln2_g, ln2_b=ln2_b)
    weights = {k: np.ascontiguousarray(np.asarray(v, np.float32)) for k, v in weights.items()}
    cfg = dict(T_CTX=T_CTX, T_LAT=T_LAT, NL=DEPTH, NG=N_GROUPS, CAP=MOE_CAP, stop="end")
    nc, es, S = build_program(cfg)
    consts = host_consts(T_CTX, T_LAT, N_EXPERTS, MOE_CAP)
    in_maps = [_per_core_inputs(b, x, c, ctx, c_ctx, weights, consts) for b in range(B)]
    res = run_bass_kernel_spmd(nc, in_maps, core_ids=list(range(N_ACTIVE)))
    return np.stack([res.results[b]["out"] for b in range(B)], axis=0).astype(np.float32)
```

```python
import math
import numpy as np
import ml_dtypes
import concourse.bass as bass
import concourse.mybir as mybir
from concourse.bass_utils import run_bass_kernel_spmd

F32 = mybir.dt.float32
BF16 = mybir.dt.bfloat16
I32 = mybir.dt.int32
ALU = mybir.AluOpType
AF = mybir.ActivationFunctionType
AX = mybir.AxisListType

D = 2048
DC = D // 128
DEPTH = 2
GRID_W = 64
MLA_HEADS = 8
Q_RANK = 768
KV_RANK = 512
NOPE = 128
ROPE = 64
MLA_V = 128
DIFF_HEADS = 4
DIFF_HD = 64
DIFF_V = 128
POOL_WINDOWS = (2, 4, 8, 16)
IN_COLS = 9536
N_GROUPS = 8
EPG = 8
N_EXPERTS = 64
D_EXPERT = 512
EPS = 1e-6
DN_ALPHA = (2 * DEPTH) ** 0.25
C_CKV, C_KROT, C_KDIFF, C_VDIFF, C_CQ, C_QDIFF, C_POOL, C_GATE = 0, 512, 576, 1088, 1600, 2368, 2880, 3392
BIG = 1.0e30


class Buf:
    __slots__ = ("w", "r", "name")

    def __init__(self, name=""):
        self.w = None
        self.r = {}
        self.name = name


class Sched:
    SEM_LIMIT = 30000
    ENGS = ("pe", "act", "dve", "pool", "sp")

    def __init__(self, nc, es):
        self.nc = nc
        self.es = es
        self.prog = {k: [] for k in self.ENGS}
        self.sem = {}
        self.cnt = {}
        self.waited = {k: {} for k in self.ENGS}
        self.last = {}
        self.init = {}
        self.pe_sems = set()
        self.nsem = 0
        for k in self.ENGS:
            self._new_sem(k)
        self.dq = {}
        for q in ("sp", "pool", "act"):
            sems = [self._alloc(f"dq_{q}_{i}") for i in range(6)]
            self.dq[q] = {"sems": sems, "cnt": [0] * len(sems), "i": 0}
        self.ninst = 0

    def _alloc(self, name):
        self.nsem += 1
        return self.es.enter_context(self.nc.semaphore(f"{name}_{self.nsem}"))

    def _new_sem(self, k):
        self.sem[k] = self._alloc(f"s_{k}")
        self.cnt[k] = 0
        if k == "pe":
            self.pe_sems.add(id(self.sem[k]))

    def _wait(self, k, tok):
        sem, val = tok
        sid = id(sem)
        if k == "pe" and sid in self.pe_sems:
            return
        w = self.waited[k]
        if w.get(sid, 0) >= val:
            return
        self.prog[k].append(("w", sem, val))
        w[sid] = val

    def _deps(self, k, reads, writes):
        for b in reads:
            if b.w is not None:
                self._wait(k, b.w)
        for b in writes:
            if b.w is not None:
                self._wait(k, b.w)
            for tok in b.r.values():
                self._wait(k, tok)

    def _commit(self, tok, reads, writes):
        sid = id(tok[0])
        for b in reads:
            b.r[sid] = tok
        for b in writes:
            b.w = tok
            b.r = {}

    def op(self, k, fn, reads=(), writes=(), sig=True):
        self._deps(k, reads, writes)
        if not sig:
            assert k == "pe"
            self.prog[k].append(("n", fn))
            tok = (self.sem[k], self.cnt[k] + 1)
            self._commit(tok, reads, writes)
            self.ninst += 1
            self.unsig = True
            return tok
        pending = k == "pe" and getattr(self, "unsig", False)
        if k == "pe":
            self.unsig = False
        if self.cnt[k] >= self.SEM_LIMIT and not pending:
            self._new_sem(k)
        self.cnt[k] += 1
        self.prog[k].append(("i", fn, self.sem[k], 1))
        tok = (self.sem[k], self.cnt[k])
        self.last[k] = tok
        self._commit(tok, reads, writes)
        self.ninst += 1
        return tok

    def dma(self, q, fn, reads=(), writes=()):
        self._deps(q, reads, writes)
        dq = self.dq[q]
        i = dq["i"]
        dq["i"] = (i + 1) % len(dq["sems"])
        sem = dq["sems"][i]
        if dq["cnt"][i] > 0:
            self._wait(q, (sem, dq["cnt"][i]))
        if dq["cnt"][i] >= self.SEM_LIMIT:
            sem = self._alloc(f"dq_{q}_{i}")
            dq["sems"][i] = sem
            dq["cnt"][i] = 0
        dq["cnt"][i] += 16
        self.prog[q].append(("i", fn, sem, 16))
        tok = (sem, dq["cnt"][i])
        self._commit(tok, reads, writes)
        self.ninst += 1
        return tok

    def finish(self, bufs):
        for b in bufs:
            if b.w is not None:
                self._wait("sp", b.w)

    def emit(self):
        def replay(k):
            def body(eng):
                for f in self.init.get(k, ()):
                    f(eng)
                for ent in self.prog[k]:
                    if ent[0] == "w":
                        eng.wait_ge(ent[1], ent[2])
                    elif ent[0] == "n":
                        ent[1](eng)
                    else:
                        ent[1](eng).then_inc(ent[2], ent[3])
            return body

        with self.nc.Block() as block:
            block.tensor(replay("pe"))
            block.scalar(replay("act"))
            block.vector(replay("dve"))
            block.gpsimd(replay("pool"))
            block.sync(replay("sp"))


class Arena:
    def __init__(self, tile, n):
        self.t = tile
        self.n = n
        self.off = 0

    def reset(self):
        self.off = 0

    def alloc(self, shape_free, name=""):
        n = int(np.prod(shape_free))
        n = (n + 15) // 16 * 16
        assert self.off + n <= self.n, f"arena overflow {name}: {self.off}+{n}>{self.n}"
        ap = self.t[:, self.off:self.off + int(np.prod(shape_free))]
        self.off += n
        if len(shape_free) == 2:
            ap = ap.rearrange("p (a b) -> p a b", b=shape_free[1])
        elif len(shape_free) == 3:
            ap = ap.rearrange("p (a b c) -> p a b c", b=shape_free[1], c=shape_free[2])
        return ap, Buf(name)


def token_blocks(t_ctx, t_lat, bw=512):
    blks = []
    for s0, n in ((0, t_ctx), (t_ctx, t_lat)):
        o = 0
        while o < n:
            w = min(bw, n - o)
            blks.append((s0 + o, w))
            o += w
    return blks


def build_program(cfg):
    T_CTX, T_LAT, NL = cfg["T_CTX"], cfg["T_LAT"], cfg["NL"]
    NG = cfg.get("NG", N_GROUPS)
    NE = NG * EPG
    CAP = cfg.get("CAP", 128)
    stop = cfg.get("stop", "end")
    T = T_CTX + T_LAT
    NT = T // 128
    NTC = T_CTX // 128
    BLKS = token_blocks(T_CTX, T_LAT)
    import contextlib
    es = contextlib.ExitStack()
    nc = bass.Bass("TRN2", target_bir_lowering=False)

    def din(name, shape, dt=F32):
        return nc.dram_tensor(name, list(shape), dt, kind="ExternalInput").ap()

    I = {}
    I["x"] = din("x", [T_LAT, D])
    I["ctx"] = din("ctx", [T_CTX, D])
    I["cvec"] = din("cvec", [2, D])
    L = DEPTH
    for nm, shp in (("w_ada", [L, D, 6 * D]), ("b_ada", [L, 6 * D]), ("w_in", [L, D, IN_COLS]), ("b_gate", [L, 3 * D]),
                    ("g_q", [L, Q_RANK]), ("g_kv", [L, KV_RANK]), ("w_uq", [L, Q_RANK, 1536]), ("w_ukv", [L, KV_RANK, 2048]),
                    ("lam", [L, 4 * DIFF_HD]), ("g_sub", [L, 128]), ("w_pool", [L, 4, 128, 128]), ("pool_scale", [L, 512]),
                    ("w_br_mla", [L, 1024, D]), ("w_br_diff", [L, 512, D]), ("w_br_pool", [L, 512, D]), ("w_o", [L, D, D]),
                    ("ln1_g", [L, D]), ("ln1_b", [L, D]), ("w_grp", [L, D, N_GROUPS]), ("b_grp", [L, N_GROUPS]),
                    ("w_exp", [L, D, N_EXPERTS]), ("b_exp", [L, N_EXPERTS]), ("w_gu", [L, NE, D, 1024]),
                    ("w_dn", [L, NE, 512, D]), ("ln2_g", [L, D]), ("ln2_b", [L, D])):
        I[nm] = din(nm, shp)
    I["ident"] = din("ident", [128, 128])
    I["ropc"] = din("ropc", [128, T])
    I["rops"] = din("rops", [128, T])
    I["rcnt"] = din("rcnt", [4, T])
    I["tril"] = din("tril", [128, 128])
    I["eoff"] = din("eoff", [128, NE])
    OUT = nc.dram_tensor("out", [T_LAT, D], F32, kind="ExternalOutput").ap()
    DBG = None
    if cfg.get("dbg"):
        DBG = nc.dram_tensor("dbg", list(cfg["dbg"]), F32, kind="ExternalOutput").ap()

    def dscr(name, shape, dt=BF16):
        return nc.dram_tensor(name, list(shape), dt).ap()

    XS = dscr("XS", [T, D], F32)
    GROW = dscr("GROW", [DEPTH, 2, 2, D], F32)
    KN = dscr("KN", [8, 128, T])
    KR2 = dscr("KR2", [128, T])
    CKG = dscr("CKG", [4, 128, T])
    CQG = dscr("CQG", [6, 128, T])
    VM = dscr("VM", [NT, 128, 8 * 129])
    QN = dscr("QN", [8, 128, T])
    QR = dscr("QR", [8, 64, T])
    KD = dscr("KD", [4, 128, T])
    QD = dscr("QD", [4, 128, T])
    VD = dscr("VD", [NT, 128, 4 * 129])
    OP = dscr("OP", [4, 128, T])
    GT = dscr("GT", [48, 128, T])
    OM = dscr("OM", [8, 128, T])
    OD = dscr("OD", [4, 128, T])
    MT = dscr("MT", [16, 128, T])
    XSORT = dscr("XSORT", [NE * CAP, D])
    YSORT = dscr("YSORT", [NE * CAP, D], F32)
    dbufs = {k: Buf(k) for k in ("XS", "GROW", "KN", "KR", "CKG", "CQG", "VM", "QN", "QR", "KD", "QD", "VD", "OP", "GT", "OM", "OD",
                                 "MT", "XSORT", "YSORT", "OUT", "DBG")}

    NBF = 58 * 1024
    NF32 = 14 * 1024 + 512
    abf_t = es.enter_context(nc.sbuf_tensor("abf", [128, NBF], BF16))
    af_t = es.enter_context(nc.sbuf_tensor("af32", [128, NF32], F32))
    ABF = Arena(abf_t, NBF)
    AF32 = Arena(af_t, NF32)
    ident = es.enter_context(nc.sbuf_tensor("identf", [128, 128], F32))
    identb = es.enter_context(nc.sbuf_tensor("identb", [128, 128], BF16))
    ones_f = es.enter_context(nc.sbuf_tensor("onesf", [128, 128], F32))
    ones_b = es.enter_context(nc.sbuf_tensor("onesb", [128, 128], BF16))
    tril = es.enter_context(nc.sbuf_tensor("trilf", [128, 128], F32))
    eoff = es.enter_context(nc.sbuf_tensor("eoff_sb", [128, NE], F32))
    zero_b = es.enter_context(nc.sbuf_tensor("zerob", [128, D], BF16))
    mod = es.enter_context(nc.sbuf_tensor("mod_sb", [128, DEPTH, 96, 2], F32))
    mod1 = es.enter_context(nc.sbuf_tensor("mod1", [128, DEPTH, 96, 2], F32))
    smallv = es.enter_context(nc.sbuf_tensor("smallv", [128, DEPTH, 64], F32))
    lamt = es.enter_context(nc.sbuf_tensor("lamt", [128, DEPTH, 8], F32))
    dest_i = es.enter_context(nc.sbuf_tensor("dest_i", [128, NT, 2], I32))
    gatew = es.enter_context(nc.sbuf_tensor("gatew", [128, NT, 2], F32))
    ebase = es.enter_context(nc.sbuf_tensor("ebase", [128, NE], F32))
    cb = {k: Buf(k) for k in ("ident", "identb", "ones", "tril", "eoff", "zero", "mod", "smallv", "lamt", "dest", "gatew", "ebase")}
    psum = [es.enter_context(nc.psum_tensor(f"ps{i}", [128, 512], F32)) for i in range(8)]
    pbuf = [Buf(f"ps{i}") for i in range(8)]
    S = Sched(nc, es)
    pctr = [0]

    def P(n=8, base=0):
        i = base + pctr[0] % n
        pctr[0] += 1
        return psum[i], pbuf[i]

    def barrier():
        toks = list(S.last.values())
        for q in S.dq.values():
            for sem, c in zip(q["sems"], q["cnt"]):
                if c > 0:
                    toks.append((sem, c))
        for k in S.ENGS:
            for t in toks:
                S._wait(k, t)

    def new_phase():
        barrier()
        ABF.reset()
        AF32.reset()

    S.dma("sp", lambda e: e.dma_start(out=ident[:], in_=I["ident"]), writes=[cb["ident"]])
    S.dma("sp", lambda e: e.dma_start(out=tril[:], in_=I["tril"]), writes=[cb["tril"]])
    S.dma("sp", lambda e: e.dma_start(out=eoff[:], in_=I["eoff"]), writes=[cb["eoff"]])
    S.op("dve", lambda e: e.tensor_copy(out=identb[:], in_=ident[:]), reads=[cb["ident"]], writes=[cb["identb"]])
    S.op("pool", lambda e: e.memset(ones_f[:], 1.0), writes=[cb["ones"]])
    S.op("pool", lambda e: e.memset(ones_b[:], 1.0), writes=[cb["ones"]])
    S.op("pool", lambda e: e.memset(zero_b[:], 0.0), writes=[cb["zero"]])

    def load_vec_fm(dst_ap, dst_buf, src2d, n):
        st, sb = AF32.alloc((128,), "vecst")
        S.dma("sp", lambda e: e.dma_start(out=st[0:n, :], in_=src2d), writes=[sb])
        ps, pb = P()
        S.op("pe", lambda e: e.transpose(ps[:, 0:n], st[0:n, :], ident[0:n, 0:n]), reads=[sb, cb["ident"]], writes=[pb])
        S.op("dve", lambda e: e.tensor_copy(out=dst_ap, in_=ps[:, 0:n]), reads=[pb], writes=[dst_buf])

    def phase_adaln():
        new_phase()
        cst, cstb = AF32.alloc((128,), "cst")
        sc, scb = AF32.alloc((32,), "sc")
        S.dma("sp", lambda e: e.dma_start(out=cst[0:32, :], in_=I["cvec"].rearrange("r (k p) -> (r k) p", p=128)), writes=[cstb])
        ps, pb = P()
        S.op("pe", lambda e: e.transpose(ps[:, 0:32], cst[0:32, :], ident[0:32, 0:32]), reads=[cstb, cb["ident"]], writes=[pb])
        S.op("act", lambda e: e.activation(out=sc, in_=ps[:, 0:32], func=AF.Silu), reads=[pb], writes=[scb])
        scv = sc.rearrange("p (r k) -> p k r", r=2)
        mark_ada = AF32.off
        for l in range(NL):
            barrier()
            AF32.off = mark_ada
            if l == 0:
                for i in range(NE * CAP // 128):
                    S.dma("pool", lambda e, i=i: e.dma_start(out=XSORT[i * 128:(i + 1) * 128, :], in_=zero_b[:]),
                          reads=[cb["zero"]], writes=[dbufs["XSORT"]])
            gsts = [AF32.alloc((256,), f"gst{i}") for i in range(2)]
            brs = [AF32.alloc((256,), f"br{i}") for i in range(2)]
            slabs = [AF32.alloc((16, 256), f"wada{i}") for i in range(2)]
            mps, mpb = psum[7], pbuf[7]
            for sl in range(6 * D // 256):
                w, wb = slabs[sl % 2]
                gst, gstb = gsts[sl % 2]
                br, brb = brs[sl % 2]
                S.dma("sp", lambda e, w=w, sl=sl, l=l: e.dma_start(
                    out=w, in_=I["w_ada"][l][:, sl * 256:(sl + 1) * 256].rearrange("(k p) n -> p k n", p=128)), writes=[wb])
                S.dma("sp", lambda e, br=br, sl=sl, l=l: e.dma_start(out=br[0:1, :], in_=I["b_ada"][l:l + 1, sl * 256:(sl + 1) * 256]),
                      writes=[brb])
                gps, gpb = P(4)
                for k in range(16):
                    S.op("pe", lambda e, w=w, k=k, gps=gps: e.matmul(gps[0:2, 0:256], lhsT=scv[:, k, :], rhs=w[:, k, :], start=(k == 0),
                                                                    stop=False), reads=[wb, scb], writes=[gpb], sig=False)
                S.op("pe", lambda e, gps=gps, br=br: e.matmul(gps[0:2, 0:256], lhsT=ones_f[0:1, 0:2], rhs=br[0:1, :], start=False, stop=True),
                     reads=[brb, cb["ones"]], writes=[gpb])
                S.op("dve", lambda e, gps=gps, gst=gst: e.tensor_copy(out=gst[0:2, :], in_=gps[0:2, 0:256]), reads=[gpb], writes=[gstb])
                which = (sl * 256) // D
                if which in (2, 5):
                    wi = 0 if which == 2 else 1
                    c0 = sl * 256 - which * D
                    S.dma("sp", lambda e, gst=gst, wi=wi, c0=c0, l=l: e.dma_start(
                        out=GROW[l, wi, :, c0:c0 + 256], in_=gst[0:2, :]), reads=[gstb], writes=[dbufs["GROW"]])
                for jj in range(2):
                    j = sl * 2 + jj
                    S.op("pe", lambda e, gst=gst, jj=jj, j=j: e.transpose(mps[:, 2 * j:2 * j + 2], gst[0:2, jj * 128:(jj + 1) * 128],
                                                                        ident[0:2, 0:2]), reads=[gstb, cb["ident"]], writes=[mpb])
            for r in range(2):
                S.op("dve", lambda e, l=l, r=r: e.tensor_copy(
                    out=mod[:, l, :, r], in_=mps[:, 0:192].rearrange("p (j r) -> p j r", r=2)[:, :, r]), reads=[mpb], writes=[cb["mod"]])
            S.op("dve", lambda e, l=l: e.tensor_scalar_add(out=mod1[:, l], in0=mod[:, l], scalar1=1.0),
                 reads=[cb["mod"]], writes=[cb["mod"]])
            load_vec_fm(smallv[:, l, 0:48], cb["smallv"], I["b_gate"][l].rearrange("(n p) -> n p", p=128), 48)
            load_vec_fm(smallv[:, l, 48:54], cb["smallv"], I["g_q"][l].rearrange("(n p) -> n p", p=128), 6)
            load_vec_fm(smallv[:, l, 54:58], cb["smallv"], I["g_kv"][l].rearrange("(n p) -> n p", p=128), 4)
            load_vec_fm(smallv[:, l, 58:62], cb["smallv"], I["pool_scale"][l].rearrange("(n p) -> n p", p=128), 4)
            load_vec_fm(smallv[:, l, 62:63], cb["smallv"], I["g_sub"][l].rearrange("(n p) -> n p", p=128), 1)
            lt, ltb = AF32.alloc((4 * DIFF_HD,), "lamin")
            S.dma("sp", lambda e, l=l: e.dma_start(out=lt, in_=I["lam"][l].partition_broadcast(128)), writes=[ltb])
            lp, lpb = AF32.alloc((2, DIFF_HD), "lamp")
            ltv = lt.rearrange("p (a b d) -> p a b d", a=2, b=2)
            S.op("dve", lambda e: e.tensor_tensor(out=lp, in0=ltv[:, :, 0, :], in1=ltv[:, :, 1, :], op=ALU.mult),
                 reads=[ltb], writes=[lpb])
            S.op("dve", lambda e, l=l: e.tensor_reduce(out=lamt[:, l, 0:2], in_=lp, axis=AX.X, op=ALU.add),
                 reads=[lpb], writes=[cb["lamt"]])
            S.op("act", lambda e, l=l: e.activation(out=lamt[:, l, 2:4], in_=lamt[:, l, 0:2], func=AF.Exp),
                 reads=[cb["lamt"]], writes=[cb["lamt"]])
            lam_init = 0.8 - 0.6 * math.exp(-0.3 * l)
            S.op("dve", lambda e, l=l: e.tensor_tensor(out=lamt[:, l, 4:5], in0=lamt[:, l, 3:4], in1=lamt[:, l, 2:3],
                                                       op=ALU.subtract), reads=[cb["lamt"]], writes=[cb["lamt"]])
            S.op("dve", lambda e, l=l, li=lam_init: e.tensor_scalar_add(out=lamt[:, l, 5:6], in0=lamt[:, l, 4:5], scalar1=-li),
                 reads=[cb["lamt"]], writes=[cb["lamt"]])
            AF32.off = 0 if False else AF32.off

    def ln_tile(xt, xb, st, stb):
        S.op("dve", lambda e: e.tensor_reduce(out=st[:, 0:1], in_=xt, axis=AX.X, op=ALU.add), reads=[xb], writes=[stb])
        S.op("dve", lambda e: e.tensor_scalar(out=st[:, 1:2], in0=st[:, 0:1], scalar1=-1.0 / D, scalar2=None, op0=ALU.mult),
             reads=[stb], writes=[stb])
        S.op("dve", lambda e: e.tensor_scalar(out=st[:, 0:1], in0=st[:, 0:1], scalar1=1.0 / D, scalar2=None, op0=ALU.mult),
             reads=[stb], writes=[stb])

    def ln_finish(sq, sqb, st, stb):
        S.op("dve", lambda e: e.tensor_reduce(out=st[:, 2:3], in_=sq, axis=AX.X, op=ALU.add), reads=[sqb], writes=[stb])
        S.op("dve", lambda e: e.tensor_scalar(out=st[:, 2:3], in0=st[:, 2:3], scalar1=1.0 / D, scalar2=EPS, op0=ALU.mult,
                                              op1=ALU.add), reads=[stb], writes=[stb])
        S.op("act", lambda e: e.activation(out=st[:, 3:4], in_=st[:, 2:3], func=AF.Sqrt), reads=[stb], writes=[stb])
        S.op("dve", lambda e: e.reciprocal(out=st[:, 2:3], in_=st[:, 3:4]), reads=[stb], writes=[stb])

    def layernorm(xt, xb, xn, xnb, sq, sqb, st, stb):
        ln_tile(xt, xb, st, stb)
        S.op("act", lambda e: e.activation(out=sq, in_=xt, func=AF.Square, bias=st[:, 1:2], scale=1.0),
             reads=[xb, stb], writes=[sqb])
        ln_finish(sq, sqb, st, stb)
        S.op("dve", lambda e: e.tensor_scalar(out=xn, in0=xt, scalar1=st[:, 0:1], scalar2=st[:, 2:3], op0=ALU.subtract,
                                              op1=ALU.mult), reads=[xb, stb], writes=[xnb])

    def is_ctx_tile(tt):
        return tt < NTC

    def load_w_chunk(dst, dstb, src_cols_ap, q="pool"):
        S.dma(q, lambda e: e.dma_start(out=dst, in_=src_cols_ap.rearrange("(k p) n -> p k n", p=128)), writes=[dstb])

    def make_rot_w(w, wb, wp, wpb):
        wv = w.rearrange("p k (g h j) -> p (k g) h j", h=2, j=16)
        wpv = wp.rearrange("p k (g h j) -> p (k g) h j", h=2, j=16)
        S.op("pool", lambda e: e.tensor_scalar(out=wpv[:, :, 0, :], in0=wv[:, :, 1, :], scalar1=-1.0, scalar2=None,
                                               op0=ALU.mult), reads=[wb], writes=[wpb])
        S.op("pool", lambda e: e.tensor_copy(out=wpv[:, :, 1, :], in_=wv[:, :, 0, :]), reads=[wb], writes=[wpb])

    def phase_inproj(l):
        new_phase()
        uT, uTb = ABF.alloc((16, T), "uT")
        ssq_kv, ssq_kvb = AF32.alloc((T,), "ssq_kv")
        ssq_q, ssq_qb = AF32.alloc((T,), "ssq_q")
        mark = AF32.off
        xts = [AF32.alloc((D,), f"xt{i}") for i in range(2)]
        xn, xnb = AF32.alloc((D,), "xn")
        sq, sqb = AF32.alloc((D,), "sq")
        st, stb = AF32.alloc((4,), "st")
        for tt in range(NT):
            xt, xb = xts[tt % 2]
            S.dma("sp", lambda e, xt=xt, tt=tt: e.dma_start(out=xt, in_=XS[tt * 128:(tt + 1) * 128, :]),
                  reads=[dbufs["XS"]], writes=[xb])
            layernorm(xt, xb, xn, xnb, sq, sqb, st, stb)
            r = 1 if is_ctx_tile(tt) else 0
            for k in range(16):
                if k % 4 == 0:
                    ps, pb = P()
                S.op("pe", lambda e, ps=ps, k=k: e.transpose(ps[:, (k % 4) * 128:(k % 4 + 1) * 128],
                                                             xn[:, k * 128:(k + 1) * 128], ident[:]),
                     reads=[xnb, cb["ident"]], writes=[pb])
                S.op("act", lambda e, ps=ps, k=k, tt=tt, r=r: e.activation(
                    out=uT[:, k, tt * 128:(tt + 1) * 128], in_=ps[:, (k % 4) * 128:(k % 4 + 1) * 128], func=AF.Identity,
                    scale=mod1[:, l, 16 + k, r:r + 1], bias=mod[:, l, k, r:r + 1]), reads=[pb, cb["mod"]], writes=[uTb])
        AF32.off = mark
        if stop == "uT":
            return uT, uTb
        barrier()
        ropc, ropcb = AF32.alloc((T,), "ropc")
        rops, ropsb = AF32.alloc((T,), "rops")
        S.dma("sp", lambda e: e.dma_start(out=ropc, in_=I["ropc"]), writes=[ropcb])
        S.dma("sp", lambda e: e.dma_start(out=rops, in_=I["rops"]), writes=[ropsb])
        wch = [ABF.alloc((16, 128), f"wch{i}") for i in range(2)]
        wchp = [ABF.alloc((16, 128), f"wchp{i}") for i in range(2)]
        stg = [ABF.alloc((512,), f"stg{i}") for i in range(3)]
        sqs = [AF32.alloc((512,), f"sqs{i}") for i in range(2)]
        tmp = [AF32.alloc((512,), f"tmpa{i}") for i in range(2)]
        ctr = [0, 0, 0]

        def proj_chunk(c0, ncols, evac, rot=False, dup=False):
            if ctr[0] >= cfg.get("m2cut", 10 ** 9):
                return
            i = ctr[0] % 2
            ctr[0] += 1
            w, wb = wch[i]
            nw = ncols * (2 if dup else 1)
            load_w_chunk(w[:, :, 0:ncols], wb, I["w_in"][l][:, c0:c0 + ncols])
            if dup:
                load_w_chunk(w[:, :, ncols:2 * ncols], wb, I["w_in"][l][:, c0:c0 + ncols])
            if rot:
                wp, wpb = wchp[i]
                make_rot_w(w[:, :, 0:nw], wb, wp[:, :, 0:nw], wpb)
            for (b0, bw) in BLKS:
                ps, pb = P(6)
                for k in range(16):
                    S.op("pe", lambda e, ps=ps, k=k, b0=b0, bw=bw: e.matmul(
                        ps[0:nw, 0:bw], lhsT=w[:, k, 0:nw], rhs=uT[:, k, b0:b0 + bw], start=(k == 0), stop=(k == 15)),
                        reads=[wb, uTb], writes=[pb], sig=(k == 15))
                ps2, pb2 = None, None
                if rot:
                    ps2, pb2 = P(6)
                    for k in range(16):
                        S.op("pe", lambda e, ps2=ps2, k=k, b0=b0, bw=bw: e.matmul(
                            ps2[0:nw, 0:bw], lhsT=wp[:, k, 0:nw], rhs=uT[:, k, b0:b0 + bw], start=(k == 0), stop=(k == 15)),
                            reads=[wpb, uTb], writes=[pb2], sig=(k == 15))
                evac(ps, pb, ps2, pb2, b0, bw, nw)

        def next_stg():
            s = stg[ctr[1] % 3]
            ctr[1] += 1
            return s

        def evac_rope(dram_rows, dname):
            def f(ps, pb, ps2, pb2, b0, bw, nw):
                t1, t1b = tmp[0]
                t2, t2b = tmp[1]
                sg, sgb = next_stg()
                S.op("dve", lambda e: e.tensor_tensor(out=t1[0:nw, 0:bw], in0=ps[0:nw, 0:bw], in1=ropc[0:nw, b0:b0 + bw],
                                                      op=ALU.mult), reads=[pb, ropcb], writes=[t1b])
                S.op("dve", lambda e: e.tensor_tensor(out=t2[0:nw, 0:bw], in0=ps2[0:nw, 0:bw], in1=rops[0:nw, b0:b0 + bw],
                                                      op=ALU.mult), reads=[pb2, ropsb], writes=[t2b])
                S.op("pool", lambda e: e.tensor_tensor(out=sg[0:nw, 0:bw], in0=t1[0:nw, 0:bw], in1=t2[0:nw, 0:bw],
                                                       op=ALU.add), reads=[t1b, t2b], writes=[sgb])
                S.dma("sp", lambda e: e.dma_start(out=dram_rows[0:nw, b0:b0 + bw], in_=sg[0:nw, 0:bw]), reads=[sgb],
                      writes=[dbufs[dname]])
            return f

        def evac_rms(dram_rows, dname, gcol, ssq, ssqb, first):
            def f(ps, pb, ps2, pb2, b0, bw, nw):
                em = cfg.get("evacmode", 9)
                if em < 1:
                    return
                sg, sgb = next_stg()
                s2, s2b = sqs[ctr[2] % 2]
                ctr[2] += 1
                S.op("act", lambda e: e.activation(out=s2[:, 0:bw], in_=ps[:, 0:bw], func=AF.Square), reads=[pb], writes=[s2b])
                if em < 2:
                    return
                S.op("act", lambda e: e.activation(out=sg[:, 0:bw], in_=ps[:, 0:bw], func=AF.Identity,
                                                   scale=smallv[:, l, gcol:gcol + 1]), reads=[pb, cb["smallv"]], writes=[sgb])
                S.dma("sp", lambda e: e.dma_start(out=dram_rows[:, b0:b0 + bw], in_=sg[:, 0:bw]), reads=[sgb],
                      writes=[dbufs[dname]])
                if em < 3:
                    return
                pq, pqb = P(2, 6)
                S.op("pe", lambda e: e.matmul(pq[:, 0:bw], lhsT=ones_f[:], rhs=s2[:, 0:bw], start=True, stop=True),
                     reads=[s2b, cb["ones"]], writes=[pqb])
                if first:
                    S.op("dve", lambda e: e.tensor_copy(out=ssq[:, b0:b0 + bw], in_=pq[:, 0:bw]), reads=[pqb], writes=[ssqb])
                else:
                    S.op("dve", lambda e: e.tensor_tensor(out=ssq[:, b0:b0 + bw], in0=ssq[:, b0:b0 + bw], in1=pq[:, 0:bw],
                                                          op=ALU.add), reads=[pqb, ssqb], writes=[ssqb])
            return f

        for kc in range(4):
            proj_chunk(C_CKV + kc * 128, 128, evac_rms(CKG[kc], "CKG", 54 + kc, ssq_kv, ssq_kvb, kc == 0))
        proj_chunk(C_KROT, 64, evac_rope(KR2, "KR"), rot=True, dup=True)
        for h in range(4):
            proj_chunk(C_KDIFF + h * 128, 128, evac_rope(KD[h], "KD"), rot=True)
        for kc in range(6):
            proj_chunk(C_CQ + kc * 128, 128, evac_rms(CQG[kc], "CQG", 48 + kc, ssq_q, ssq_qb, kc == 0))
        for h in range(4):
            proj_chunk(C_QDIFF + h * 128, 128, evac_rope(QD[h], "QD"), rot=True)

        def evac_gate(j):
            def f(ps, pb, ps2, pb2, b0, bw, nw):
                sg, sgb = next_stg()
                S.op("act", lambda e: e.activation(out=sg[:, 0:bw], in_=ps[:, 0:bw], func=AF.Sigmoid,
                                                   bias=smallv[:, l, j:j + 1], scale=1.0), reads=[pb, cb["smallv"]], writes=[sgb])
                S.dma("sp", lambda e: e.dma_start(out=GT[j][:, b0:b0 + bw], in_=sg[:, 0:bw]), reads=[sgb], writes=[dbufs["GT"]])
            return f
        for j in range(48):
            proj_chunk(C_GATE + j * 128, 128, evac_gate(j))

        if "m2cut" in cfg:
            return ssq_kv, ssq_kvb, ssq_q, ssq_qb, mark
        wv, wvb = ABF.alloc((16, 512), "wvd")
        load_w_chunk(wv, wvb, I["w_in"][l][:, C_VDIFF:C_VDIFF + 512])
        vst = [ABF.alloc((4, 129), f"vst{i}") for i in range(2)]
        for i in range(2):
            S.op("pool", lambda e, i=i: e.memset(vst[i][0], 1.0), writes=[vst[i][1]])
        for tt in range(NT):
            ps, pb = P(6)
            for k in range(16):
                S.op("pe", lambda e, ps=ps, k=k, tt=tt: e.matmul(ps[:, 0:512], lhsT=uT[:, k, tt * 128:(tt + 1) * 128],
                                                                 rhs=wv[:, k, :], start=(k == 0), stop=(k == 15)),
                     reads=[uTb, wvb], writes=[pb])
            v, vb = vst[tt % 2]
            S.op("act", lambda e, ps=ps, v=v: e.activation(out=v[:, :, 0:128], in_=ps[:, 0:512].rearrange("p (h d) -> p h d", d=128),
                                                           func=AF.Copy), reads=[pb], writes=[vb])
            S.dma("sp", lambda e, v=v, tt=tt: e.dma_start(out=VD[tt], in_=v.rearrange("p h d -> p (h d)")), reads=[vb],
                  writes=[dbufs["VD"]])

        barrier()
        AF32.off = mark
        PADW = 16
        TP = T + 4 * PADW
        xp, xpb = AF32.alloc((TP,), "xp")
        b1, b1b = AF32.alloc((TP,), "b1")
        rc, rcb = AF32.alloc((T,), "rc")
        pooled, pooledb = ABF.alloc((T,), "pooled")
        wpl, wplb = ABF.alloc((128,), "wpool")

        def segs():
            return ((PADW, 0, T_CTX), (3 * PADW + T_CTX, T_CTX, T_LAT))
        for gi, wwin in enumerate(POOL_WINDOWS):
            half = wwin // 2
            S.op("pool", lambda e: e.memset(xp, 0.0), writes=[xpb])
            S.dma("sp", lambda e, gi=gi: e.dma_start(out=rc, in_=I["rcnt"][gi:gi + 1, :].partition_broadcast(128)), writes=[rcb])
            S.dma("pool", lambda e, gi=gi: e.dma_start(out=wpl, in_=I["w_pool"][l, gi]), writes=[wplb])

            def evac_pool(ps, pb, ps2, pb2, b0, bw, nw):
                po = PADW + b0 if b0 < T_CTX else 3 * PADW + b0
                S.op("act", lambda e: e.activation(out=xp[:, po:po + bw], in_=ps[:, 0:bw], func=AF.Copy), reads=[pb], writes=[xpb])
            proj_chunk(C_POOL + gi * 128, 128, evac_pool)
            src, srcb, dst, dstb = xp, xpb, b1, b1b
            m = 1
            first = True
            while m < wwin:
                if first:
                    S.op("dve", lambda e, m=m: e.tensor_tensor(out=b1[:, 0:TP - m], in0=xp[:, 0:TP - m], in1=xp[:, m:TP],
                                                               op=ALU.add), reads=[xpb], writes=[b1b])
                    first = False
                else:
                    S.op("dve", lambda e, m=m: e.tensor_tensor(out=b1[:, 0:TP - m], in0=b1[:, 0:TP - m], in1=b1[:, m:TP],
                                                               op=ALU.add), reads=[b1b], writes=[b1b])
                m *= 2
            for (po, t0, n) in segs():
                S.op("dve", lambda e, po=po, t0=t0, n=n, half=half: e.tensor_tensor(
                    out=b1[:, po - half:po - half + n], in0=b1[:, po - half:po - half + n], in1=rc[:, t0:t0 + n], op=ALU.mult),
                    reads=[b1b, rcb], writes=[b1b])
                S.op("dve", lambda e, po=po, t0=t0, n=n, half=half: e.tensor_tensor(
                    out=pooled[:, t0:t0 + n], in0=b1[:, po - half:po - half + n], in1=xp[:, po:po + n], op=ALU.subtract),
                    reads=[b1b, xpb], writes=[pooledb])
            for (b0, bw) in BLKS:
                ps, pb = P(6)
                S.op("pe", lambda e, ps=ps, b0=b0, bw=bw: e.matmul(ps[:, 0:bw], lhsT=wpl, rhs=pooled[:, b0:b0 + bw], start=True,
                                                                   stop=True), reads=[wplb, pooledb], writes=[pb])
                sg, sgb = next_stg()
                S.op("act", lambda e, ps=ps, sg=sg, bw=bw, gi=gi: e.activation(
                    out=sg[:, 0:bw], in_=ps[:, 0:bw], func=AF.Identity, scale=smallv[:, l, 58 + gi:59 + gi]),
                    reads=[pb, cb["smallv"]], writes=[sgb])
                S.dma("sp", lambda e, sg=sg, b0=b0, bw=bw, gi=gi: e.dma_start(out=OP[gi][:, b0:b0 + bw], in_=sg[:, 0:bw]),
                      reads=[sgb], writes=[dbufs["OP"]])
        return ssq_kv, ssq_kvb, ssq_q, ssq_qb, mark

    def rstd_inplace(ssq, ssqb, n, tmp, tmpb):
        S.op("dve", lambda e: e.tensor_scalar(out=ssq, in0=ssq, scalar1=1.0 / n, scalar2=EPS, op0=ALU.mult, op1=ALU.add),
             reads=[ssqb], writes=[ssqb])
        S.op("act", lambda e: e.activation(out=tmp, in_=ssq, func=AF.Sqrt), reads=[ssqb], writes=[tmpb])
        S.op("dve", lambda e: e.reciprocal(out=ssq, in_=tmp), reads=[tmpb], writes=[ssqb])

    def phase_upproj(l, ssq_kv, ssq_kvb, ssq_q, ssq_qb, mark):
        barrier()
        ABF.reset()
        AF32.off = mark
        tmpT, tmpTb = AF32.alloc((T,), "tmpT")
        rstd_inplace(ssq_kv, ssq_kvb, KV_RANK, tmpT, tmpTb)
        rstd_inplace(ssq_q, ssq_qb, Q_RANK, tmpT, tmpTb)
        rtm, rtmb = AF32.alloc((NT,), "rtm")
        ps, pb = P()
        for tt in range(NT):
            S.op("pe", lambda e, tt=tt: e.transpose(ps[:, tt:tt + 1], ssq_kv[0:1, tt * 128:(tt + 1) * 128], ident[0:1, 0:1]),
                 reads=[ssq_kvb, cb["ident"]], writes=[pb])
        S.op("dve", lambda e: e.tensor_copy(out=rtm, in_=ps[:, 0:NT]), reads=[pb], writes=[rtmb])
        ropc, ropcb = AF32.alloc((T,), "ropc")
        rops, ropsb = AF32.alloc((T,), "rops")
        S.dma("sp", lambda e: e.dma_start(out=ropc, in_=I["ropc"]), writes=[ropcb])
        S.dma("sp", lambda e: e.dma_start(out=rops, in_=I["rops"]), writes=[ropsb])
        t1, t1b = AF32.alloc((512,), "t1")
        t2, t2b = AF32.alloc((512,), "t2")
        ckg, ckgb = ABF.alloc((4, T), "ckg")
        cqg, cqgb = ABF.alloc((6, T), "cqg")
        S.dma("sp", lambda e: e.dma_start(out=ckg, in_=CKG.rearrange("k p t -> p k t")), reads=[dbufs["CKG"]], writes=[ckgb])
        S.dma("sp", lambda e: e.dma_start(out=cqg, in_=CQG.rearrange("k p t -> p k t")), reads=[dbufs["CQG"]], writes=[cqgb])
        wcs = [ABF.alloc((6, 128), f"wc{i}") for i in range(2)]
        wcp, wcpb = ABF.alloc((6, 128), "wcp")
        stg = [ABF.alloc((512,), f"stgu{i}") for i in range(3)]
        wv, wvb = ABF.alloc((4, 1024), "wv")
        vst = [ABF.alloc((8, 129), f"vstm{i}") for i in range(2)]
        ctr = [0, 0]

        def nstg():
            s = stg[ctr[1] % 3]
            ctr[1] += 1
            return s

        def nw():
            s = wcs[ctr[0] % 2]
            ctr[0] += 1
            return s
        for h in range(8):
            w, wb = nw()
            load_w_chunk(w[:, 0:4, :], wb, I["w_ukv"][l][:, h * 256:h * 256 + 128])
            for (b0, bw) in BLKS:
                ps, pb = P(6)
                for k in range(4):
                    S.op("pe", lambda e, ps=ps, k=k, b0=b0, bw=bw, w=w: e.matmul(
                        ps[:, 0:bw], lhsT=w[:, k, :], rhs=ckg[:, k, b0:b0 + bw], start=(k == 0), stop=(k == 3)),
                        reads=[wb, ckgb], writes=[pb])
                sg, sgb = nstg()
                S.op("dve", lambda e, ps=ps, sg=sg, b0=b0, bw=bw: e.tensor_tensor(
                    out=sg[:, 0:bw], in0=ps[:, 0:bw], in1=ssq_kv[:, b0:b0 + bw], op=ALU.mult), reads=[pb, ssq_kvb], writes=[sgb])
                S.dma("sp", lambda e, sg=sg, b0=b0, bw=bw, h=h: e.dma_start(out=KN[h][:, b0:b0 + bw], in_=sg[:, 0:bw]),
                      reads=[sgb], writes=[dbufs["KN"]])
            w, wb = nw()
            load_w_chunk(w, wb, I["w_uq"][l][:, h * 192:h * 192 + 128])
            for (b0, bw) in BLKS:
                ps, pb = P(6)
                for k in range(6):
                    S.op("pe", lambda e, ps=ps, k=k, b0=b0, bw=bw, w=w: e.matmul(
                        ps[:, 0:bw], lhsT=w[:, k, :], rhs=cqg[:, k, b0:b0 + bw], start=(k == 0), stop=(k == 5)),
                        reads=[wb, cqgb], writes=[pb])
                sg, sgb = nstg()
                S.op("dve", lambda e, ps=ps, sg=sg, b0=b0, bw=bw: e.tensor_tensor(
                    out=sg[:, 0:bw], in0=ps[:, 0:bw], in1=ssq_q[:, b0:b0 + bw], op=ALU.mult), reads=[pb, ssq_qb], writes=[sgb])
                S.dma("sp", lambda e, sg=sg, b0=b0, bw=bw, h=h: e.dma_start(out=QN[h][:, b0:b0 + bw], in_=sg[:, 0:bw]),
                      reads=[sgb], writes=[dbufs["QN"]])
            load_w_chunk(wv[:, :, h * 128:(h + 1) * 128], wvb, I["w_ukv"][l][:, h * 256 + 128:h * 256 + 256])
        for hp in range(4):
            w, wb = nw()
            for i in range(2):
                hh = 2 * hp + i
                load_w_chunk(w[:, :, i * 64:(i + 1) * 64], wb, I["w_uq"][l][:, hh * 192 + 128:hh * 192 + 192])
            make_rot_w(w, wb, wcp, wcpb)
            for (b0, bw) in BLKS:
                ps, pb = P(6)
                ps2, pb2 = P(6)
                for k in range(6):
                    S.op("pe", lambda e, ps=ps, k=k, b0=b0, bw=bw, w=w: e.matmul(
                        ps[:, 0:bw], lhsT=w[:, k, :], rhs=cqg[:, k, b0:b0 + bw], start=(k == 0), stop=(k == 5)),
                        reads=[wb, cqgb], writes=[pb])
                for k in range(6):
                    S.op("pe", lambda e, ps2=ps2, k=k, b0=b0, bw=bw: e.matmul(
                        ps2[:, 0:bw], lhsT=wcp[:, k, :], rhs=cqg[:, k, b0:b0 + bw], start=(k == 0), stop=(k == 5)),
                        reads=[wcpb, cqgb], writes=[pb2])
                sg, sgb = nstg()
                S.op("dve", lambda e, ps=ps, b0=b0, bw=bw: e.tensor_tensor(out=t1[:, 0:bw], in0=ps[:, 0:bw], in1=ropc[:, b0:b0 + bw],
                                                                        op=ALU.mult), reads=[pb, ropcb], writes=[t1b])
                S.op("dve", lambda e, ps2=ps2, b0=b0, bw=bw: e.tensor_tensor(out=t2[:, 0:bw], in0=ps2[:, 0:bw], in1=rops[:, b0:b0 + bw],
                                                                         op=ALU.mult), reads=[pb2, ropsb], writes=[t2b])
                S.op("pool", lambda e, bw=bw: e.tensor_tensor(out=t1[:, 0:bw], in0=t1[:, 0:bw], in1=t2[:, 0:bw], op=ALU.add),
                     reads=[t1b, t2b], writes=[t1b])
                S.op("dve", lambda e, sg=sg, b0=b0, bw=bw: e.tensor_tensor(out=sg[:, 0:bw], in0=t1[:, 0:bw], in1=ssq_q[:, b0:b0 + bw],
                                                                        op=ALU.mult), reads=[t1b, ssq_qb], writes=[sgb])
                for i in range(2):
                    S.dma("sp", lambda e, sg=sg, b0=b0, bw=bw, i=i, hp=hp: e.dma_start(
                        out=QR[2 * hp + i][:, b0:b0 + bw], in_=sg[i * 64:(i + 1) * 64, 0:bw]), reads=[sgb], writes=[dbufs["QR"]])
        for i in range(2):
            S.op("pool", lambda e, i=i: e.memset(vst[i][0], 1.0), writes=[vst[i][1]])
        for tt in range(NT):
            v, vb = vst[tt % 2]
            for half in range(2):
                ps, pb = P(6)
                for k in range(4):
                    S.op("pe", lambda e, ps=ps, k=k, tt=tt, half=half: e.matmul(
                        ps[:, 0:512], lhsT=ckg[:, k, tt * 128:(tt + 1) * 128], rhs=wv[:, k, half * 512:(half + 1) * 512],
                        start=(k == 0), stop=(k == 3)), reads=[ckgb, wvb], writes=[pb])
                S.op("act", lambda e, ps=ps, v=v, half=half, tt=tt: e.activation(
                    out=v[:, half * 4:(half + 1) * 4, 0:128], in_=ps[:, 0:512].rearrange("p (h d) -> p h d", d=128),
                    func=AF.Identity, scale=rtm[:, tt:tt + 1]), reads=[pb, rtmb], writes=[vb])
            S.dma("sp", lambda e, v=v, tt=tt: e.dma_start(out=VM[tt], in_=v.rearrange("p h d -> p (h d)")), reads=[vb],
                  writes=[dbufs["VM"]])

    def phase_attn(l):
        new_phase()
        kr, krb = ABF.alloc((T,), "kr")
        S.dma("sp", lambda e: e.dma_start(out=kr[0:64, :], in_=KR2[0:64, :]), reads=[dbufs["KR"]], writes=[krb])
        ops = [ABF.alloc((T,), f"kq{i}") for i in range(8)]
        vhs = [ABF.alloc((NT, 129), f"vh{i}") for i in range(2)]
        est = [ABF.alloc((512,), f"est{i}") for i in range(4)]
        ostg = [ABF.alloc((512,), f"ostg{i}") for i in range(2)]
        on = [AF32.alloc((128,), f"on{i}") for i in range(4)]
        ona = [AF32.alloc((4, 128), f"ona{i}") for i in range(2)]
        rc, rcb = AF32.alloc((8,), "rc")
        sqd, sqdb = AF32.alloc((128,), "sqd")
        gs, gsb = AF32.alloc((2,), "gs")
        lam_init = 0.8 - 0.6 * math.exp(-0.3 * l)
        S.op("dve", lambda e: e.tensor_scalar(out=gs[:, 0:1], in0=smallv[:, l, 62:63], scalar1=1.0 - lam_init, scalar2=None,
                                              op0=ALU.mult), reads=[cb["smallv"]], writes=[gsb])
        qblocks = []
        for (b0, bw) in BLKS:
            qblocks.append((b0, bw, NTC if b0 < T_CTX else NT))
        ectr = [0]

        rcp, rcpb = AF32.alloc((512,), "rcp")
        o0, o0b = AF32.alloc((512,), "o0f")
        o1, o1b = AF32.alloc((512,), "o1f")
        sq5, sq5b = AF32.alloc((512,), "sq5")
        t5, t5b = AF32.alloc((512,), "t5")
        OB, SB, QB = 4, 5, 6

        accs = [AF32.alloc((512,), "accA") + ("dve",), AF32.alloc((512,), "accB") + ("pool",)]

        def attend(kpart, qpart, vh, vhb, scale, q0, qw, nkt):
            def emit_pv(kti, es_, esb):
                S.op("pe", lambda e: e.matmul(psum[OB][:, 0:qw], lhsT=vh[:, kti, 0:128], rhs=es_[:, 0:qw],
                                              start=(kti == 0), stop=(kti == nkt - 1)),
                     reads=[esb, vhb], writes=[pbuf[OB]], sig=True)
            prev = None
            n = len(kpart)
            for kti in range(nkt):
                ps, pb = P(3)
                for i, ((ka, kb_), (qa, qb_)) in enumerate(zip(kpart, qpart)):
                    S.op("pe", lambda e, ps=ps, ka=ka, qa=qa, i=i, kti=kti: e.matmul(
                        ps[:, 0:qw], lhsT=ka[:, kti * 128:(kti + 1) * 128], rhs=qa[:, q0:q0 + qw], start=(i == 0), stop=(i == n - 1)),
                        reads=[kb_, qb_], writes=[pb], sig=(i == n - 1))
                if prev is not None:
                    emit_pv(*prev)
                es_, esb = est[ectr[0] % 4]
                ectr[0] += 1
                S.op("act", lambda e, ps=ps, es_=es_: e.activation(out=es_[:, 0:qw], in_=ps[:, 0:qw], func=AF.Exp, scale=scale),
                     reads=[pb], writes=[esb])
                acc, accb, eng = accs[kti % 2]
                if kti < 2:
                    S.op(eng, lambda e, acc=acc, es_=es_: e.tensor_copy(out=acc[:, 0:qw], in_=es_[:, 0:qw]), reads=[esb], writes=[accb])
                else:
                    S.op(eng, lambda e, acc=acc, es_=es_: e.tensor_tensor(out=acc[:, 0:qw], in0=acc[:, 0:qw], in1=es_[:, 0:qw], op=ALU.add),
                         reads=[esb, accb], writes=[accb])
                prev = (kti, es_, esb)
            emit_pv(*prev)
            parts = accs[0:min(2, nkt)]
            for i, (acc, accb, _) in enumerate(parts):
                S.op("pe", lambda e, acc=acc, i=i: e.matmul(psum[SB][:, 0:qw], lhsT=ones_f[:], rhs=acc[:, 0:qw], start=(i == 0),
                                                          stop=(i == len(parts) - 1)),
                     reads=[accb, cb["ones"]], writes=[pbuf[SB]], sig=(i == len(parts) - 1))

        octr = [0]
        for h in range(MLA_HEADS):
            (kt, ktb), (qt, qtb), (qr, qrb) = ops[(h % 2) * 3:(h % 2) * 3 + 3]
            vh, vhb = vhs[h % 2]
            S.dma("sp", lambda e, kt=kt, h=h: e.dma_start(out=kt, in_=KN[h]), reads=[dbufs["KN"]], writes=[ktb])
            S.dma("sp", lambda e, qt=qt, h=h: e.dma_start(out=qt, in_=QN[h]), reads=[dbufs["QN"]], writes=[qtb])
            S.dma("sp", lambda e, qr=qr, h=h: e.dma_start(out=qr[0:64, :], in_=QR[h]), reads=[dbufs["QR"]], writes=[qrb])
            S.dma("sp", lambda e, vh=vh, h=h: e.dma_start(out=vh, in_=VM.rearrange("n p d -> p n d")[:, :, h * 129:(h + 1) * 129]),
                  reads=[dbufs["VM"]], writes=[vhb])
            for (q0, qw, nkt) in qblocks:
                attend([(kt, ktb), (kr[0:64, :], krb)], [(qt, qtb), (qr[0:64, :], qrb)], vh, vhb, 192.0 ** -0.5, q0, qw, nkt)
                og, ogb = ostg[octr[0] % 2]
                octr[0] += 1
                S.op("dve", lambda e, qw=qw: e.reciprocal(out=rcp[:, 0:qw], in_=psum[SB][:, 0:qw]), reads=[pbuf[SB]], writes=[rcpb])
                S.op("dve", lambda e, qw=qw, og=og: e.tensor_tensor(out=og[:, 0:qw], in0=psum[OB][:, 0:qw], in1=rcp[:, 0:qw], op=ALU.mult),
                     reads=[pbuf[OB], rcpb], writes=[ogb])
                S.dma("sp", lambda e, og=og, q0=q0, qw=qw, h=h: e.dma_start(out=OM[h][:, q0:q0 + qw], in_=og[:, 0:qw]),
                      reads=[ogb], writes=[dbufs["OM"]])
        for h in range(DIFF_HEADS):
            (kd, kdb), (qd, qdb) = ops[6:8] if h % 2 else ops[0:2]
            vh, vhb = vhs[h % 2]
            S.dma("sp", lambda e, kd=kd, h=h: e.dma_start(out=kd, in_=KD[h]), reads=[dbufs["KD"]], writes=[kdb])
            S.dma("sp", lambda e, qd=qd, h=h: e.dma_start(out=qd, in_=QD[h]), reads=[dbufs["QD"]], writes=[qdb])
            S.dma("sp", lambda e, vh=vh, h=h: e.dma_start(out=vh[:, :, :], in_=VD.rearrange("n p d -> p n d")[:, :, h * 129:(h + 1) * 129]),
                  reads=[dbufs["VD"]], writes=[vhb])
            for (q0, qw, nkt) in qblocks:
                for m, (om_, omb_) in enumerate(((o0, o0b), (o1, o1b))):
                    attend([(kd[m * 64:(m + 1) * 64, :], kdb)], [(qd[m * 64:(m + 1) * 64, :], qdb)], vh, vhb, 64.0 ** -0.5, q0, qw, nkt)
                    S.op("dve", lambda e, qw=qw: e.reciprocal(out=rcp[:, 0:qw], in_=psum[SB][:, 0:qw]), reads=[pbuf[SB]], writes=[rcpb])
                    S.op("dve", lambda e, qw=qw, om_=om_: e.tensor_tensor(out=om_[:, 0:qw], in0=psum[OB][:, 0:qw], in1=rcp[:, 0:qw],
                                                                        op=ALU.mult), reads=[pbuf[OB], rcpb], writes=[omb_])
                og, ogb = ostg[octr[0] % 2]
                octr[0] += 1
                S.op("dve", lambda e, qw=qw: e.scalar_tensor_tensor(out=o0[:, 0:qw], in0=o1[:, 0:qw], scalar=lamt[:, l, 5:6],
                                                                    in1=o0[:, 0:qw], op0=ALU.mult, op1=ALU.add),
                     reads=[o0b, o1b, cb["lamt"]], writes=[o0b])
                S.op("act", lambda e, qw=qw: e.activation(out=sq5[:, 0:qw], in_=o0[:, 0:qw], func=AF.Square), reads=[o0b], writes=[sq5b])
                S.op("pe", lambda e, qw=qw: e.matmul(psum[QB][:, 0:qw], lhsT=ones_f[:], rhs=sq5[:, 0:qw], start=True, stop=True),
                     reads=[sq5b, cb["ones"]], writes=[pbuf[QB]])
                S.op("dve", lambda e, qw=qw: e.tensor_copy(out=t5[:, 0:qw], in_=psum[QB][:, 0:qw]), reads=[pbuf[QB]], writes=[t5b])
                S.op("dve", lambda e, qw=qw: e.tensor_scalar(out=t5[:, 0:qw], in0=t5[:, 0:qw], scalar1=1.0 / 128, scalar2=EPS,
                                                             op0=ALU.mult, op1=ALU.add), reads=[t5b], writes=[t5b])
                S.op("act", lambda e, qw=qw: e.activation(out=t5[:, 0:qw], in_=t5[:, 0:qw], func=AF.Sqrt), reads=[t5b], writes=[t5b])
                S.op("dve", lambda e, qw=qw: e.reciprocal(out=t5[:, 0:qw], in_=t5[:, 0:qw]), reads=[t5b], writes=[t5b])
                S.op("dve", lambda e, qw=qw: e.tensor_tensor(out=o0[:, 0:qw], in0=o0[:, 0:qw], in1=t5[:, 0:qw], op=ALU.mult),
                     reads=[o0b, t5b], writes=[o0b])
                S.op("act", lambda e, qw=qw, og=og: e.activation(out=og[:, 0:qw], in_=o0[:, 0:qw], func=AF.Identity, scale=gs[:, 0:1]),
                     reads=[o0b, gsb], writes=[ogb])
                S.dma("sp", lambda e, og=og, q0=q0, qw=qw, h=h: e.dma_start(out=OD[h][:, q0:q0 + qw], in_=og[:, 0:qw]),
                      reads=[ogb], writes=[dbufs["OD"]])

    def phase_merge(l):
        new_phase()
        om, omb = ABF.alloc((8, T), "om")
        od, odb = ABF.alloc((4, T), "od")
        opp, oppb = ABF.alloc((4, T), "opp")
        S.dma("sp", lambda e: e.dma_start(out=om, in_=OM.rearrange("k p t -> p k t")), reads=[dbufs["OM"]], writes=[omb])
        S.dma("sp", lambda e: e.dma_start(out=od, in_=OD.rearrange("k p t -> p k t")), reads=[dbufs["OD"]], writes=[odb])
        S.dma("sp", lambda e: e.dma_start(out=opp, in_=OP.rearrange("k p t -> p k t")), reads=[dbufs["OP"]], writes=[oppb])
        wbs = [ABF.alloc((16, 128), f"wb{i}") for i in range(2)]
        gts = [ABF.alloc((3, 512), f"gt{i}") for i in range(2)]
        stg = [ABF.alloc((512,), f"stgm{i}") for i in range(2)]
        ta, tab = AF32.alloc((512,), "ta")
        tb, tbb = AF32.alloc((512,), "tb")
        ctr = 0
        for j in range(16):
            w, wb = wbs[j % 2]
            load_w_chunk(w[:, 0:8, :], wb, I["w_br_mla"][l][:, j * 128:(j + 1) * 128])
            load_w_chunk(w[:, 8:12, :], wb, I["w_br_diff"][l][:, j * 128:(j + 1) * 128])
            load_w_chunk(w[:, 12:16, :], wb, I["w_br_pool"][l][:, j * 128:(j + 1) * 128])
            for (b0, bw) in BLKS:
                g, gb = gts[ctr % 2]
                sg, sgb = stg[ctr % 2]
                ctr += 1
                for i in range(3):
                    S.dma("sp", lambda e, g=g, i=i, b0=b0, bw=bw, j=j: e.dma_start(out=g[:, i, 0:bw], in_=GT[i * 16 + j][:, b0:b0 + bw]),
                          reads=[dbufs["GT"]], writes=[gb])
                pss = []
                for (src_, srcb_, k0, nk) in ((om, omb, 0, 8), (od, odb, 8, 4), (opp, oppb, 12, 4)):
                    ps, pb = P(6)
                    for k in range(nk):
                        S.op("pe", lambda e, ps=ps, k=k, k0=k0, nk=nk, src_=src_, b0=b0, bw=bw, w=w: e.matmul(
                            ps[:, 0:bw], lhsT=w[:, k0 + k, :], rhs=src_[:, k, b0:b0 + bw], start=(k == 0), stop=(k == nk - 1)),
                            reads=[wb, srcb_], writes=[pb], sig=(k == nk - 1))
                    pss.append((ps, pb))
                S.op("dve", lambda e, p=pss[0][0], g=g, bw=bw: e.tensor_tensor(out=ta[:, 0:bw], in0=p[:, 0:bw], in1=g[:, 0, 0:bw],
                                                                            op=ALU.mult), reads=[pss[0][1], gb], writes=[tab])
                S.op("dve", lambda e, p=pss[1][0], g=g, bw=bw: e.tensor_tensor(out=tb[:, 0:bw], in0=p[:, 0:bw], in1=g[:, 1, 0:bw],
                                                                            op=ALU.mult), reads=[pss[1][1], gb], writes=[tbb])
                S.op("pool", lambda e, bw=bw: e.tensor_tensor(out=ta[:, 0:bw], in0=ta[:, 0:bw], in1=tb[:, 0:bw], op=ALU.add),
                     reads=[tab, tbb], writes=[tab])
                S.op("dve", lambda e, p=pss[2][0], g=g, bw=bw: e.tensor_tensor(out=tb[:, 0:bw], in0=p[:, 0:bw], in1=g[:, 2, 0:bw],
                                                                            op=ALU.mult), reads=[pss[2][1], gb], writes=[tbb])
                S.op("pool", lambda e, sg=sg, bw=bw: e.tensor_tensor(out=sg[:, 0:bw], in0=ta[:, 0:bw], in1=tb[:, 0:bw], op=ALU.add),
                     reads=[tab, tbb], writes=[sgb])
                S.dma("sp", lambda e, sg=sg, b0=b0, bw=bw, j=j: e.dma_start(out=MT[j][:, b0:b0 + bw], in_=sg[:, 0:bw]),
                      reads=[sgb], writes=[dbufs["MT"]])

    def deepnorm_tile(src4, xt, xb, t, tb_, sq, sqb, st, stb, gbc, gbcb, lng, lngb, lnb, lnbb):
        for nb, (ya, yb) in enumerate(src4):
            S.op("dve", lambda e, ya=ya, nb=nb: e.tensor_tensor(out=t[:, nb * 512:(nb + 1) * 512], in0=ya,
                                                               in1=gbc[:, nb * 512:(nb + 1) * 512], op=ALU.mult),
                 reads=[yb, gbcb], writes=[tb_])
        S.op("dve", lambda e: e.scalar_tensor_tensor(out=t, in0=xt, scalar=DN_ALPHA, in1=t, op0=ALU.mult, op1=ALU.add),
             reads=[xb, tb_], writes=[tb_])
        layernorm(t, tb_, xt, xb, sq, sqb, st, stb)
        S.op("dve", lambda e: e.tensor_tensor(out=xt, in0=xt, in1=lng, op=ALU.mult), reads=[xb, lngb], writes=[xb])
        S.op("pool", lambda e: e.tensor_tensor(out=xt, in0=xt, in1=lnb, op=ALU.add), reads=[xb, lnbb], writes=[xb])

    def phase_wo(l):
        new_phase()
        wo, wob = ABF.alloc((16, D), "wo")
        for nb in range(4):
            load_w_chunk(wo[:, :, nb * 512:(nb + 1) * 512], wob, I["w_o"][l][:, nb * 512:(nb + 1) * 512])
        mts = [ABF.alloc((16, 128), f"mt{i}") for i in range(2)]
        gl, glb = AF32.alloc((D,), "g1lat")
        gc, gcb = AF32.alloc((D,), "g1ctx")
        lng, lngb = AF32.alloc((D,), "lng")
        lnb, lnbb = AF32.alloc((D,), "lnb")
        S.dma("sp", lambda e: e.dma_start(out=gl, in_=GROW[l, 0, 0:1, :].partition_broadcast(128)), reads=[dbufs["GROW"]], writes=[glb])
        S.dma("sp", lambda e: e.dma_start(out=gc, in_=GROW[l, 0, 1:2, :].partition_broadcast(128)), reads=[dbufs["GROW"]], writes=[gcb])
        S.dma("sp", lambda e: e.dma_start(out=lng, in_=I["ln1_g"][l:l + 1, :].partition_broadcast(128)), writes=[lngb])
        S.dma("sp", lambda e: e.dma_start(out=lnb, in_=I["ln1_b"][l:l + 1, :].partition_broadcast(128)), writes=[lnbb])
        xt, xb = AF32.alloc((D,), "xtw")
        t, tb_ = AF32.alloc((D,), "tw")
        sq, sqb = AF32.alloc((D,), "sqw")
        st, stb = AF32.alloc((4,), "stw")
        for tt in range(NT):
            mt, mtb = mts[tt % 2]
            S.dma("sp", lambda e, mt=mt, tt=tt: e.dma_start(out=mt, in_=MT.rearrange("k p t -> p k t")[:, :, tt * 128:(tt + 1) * 128]),
                  reads=[dbufs["MT"]], writes=[mtb])
            S.dma("sp", lambda e, tt=tt: e.dma_start(out=xt, in_=XS[tt * 128:(tt + 1) * 128, :]), reads=[dbufs["XS"]], writes=[xb])
            src4 = []
            for nb in range(4):
                b = 4 + nb
                for k in range(16):
                    S.op("pe", lambda e, b=b, k=k, nb=nb, mt=mt: e.matmul(psum[b][:, 0:512], lhsT=mt[:, k, :],
                                                                        rhs=wo[:, k, nb * 512:(nb + 1) * 512], start=(k == 0),
                                                                        stop=(k == 15)), reads=[mtb, wob], writes=[pbuf[b]], sig=(k == 15))
                src4.append((psum[b][:, 0:512], pbuf[b]))
            g, gb_ = (gc, gcb) if is_ctx_tile(tt) else (gl, glb)
            deepnorm_tile(src4, xt, xb, t, tb_, sq, sqb, st, stb, g, gb_, lng, lngb, lnb, lnbb)
            S.dma("sp", lambda e, tt=tt: e.dma_start(out=XS[tt * 128:(tt + 1) * 128, :], in_=xt), reads=[xb], writes=[dbufs["XS"]])

    NR = NG + NE
    NSLOT = NE * CAP
    regs = {}

    def _init_pool(eng):
        regs["bc"] = eng.alloc_register("bc")
        eng.reg_mov(regs["bc"], NSLOT - 1)
    S.init.setdefault("pool", []).append(_init_pool)

    def phase_route(l):
        new_phase()
        S.op("pool", lambda e: e.memset(ebase[:], 0.0), writes=[cb["ebase"]])
        wr, wrb = AF32.alloc((16, NR), "wr")
        rb, rbb = AF32.alloc((NR,), "rb")
        S.dma("sp", lambda e: e.dma_start(out=wr[:, :, 0:NG], in_=I["w_grp"][l][:, 0:NG].rearrange("(k p) n -> p k n", p=128), allow_slow_non_contiguous=True), writes=[wrb])
        S.dma("sp", lambda e: e.dma_start(out=wr[:, :, NG:NR], in_=I["w_exp"][l][:, 0:NE].rearrange("(k p) n -> p k n", p=128), allow_slow_non_contiguous=True), writes=[wrb])
        S.dma("sp", lambda e: e.dma_start(out=rb[:, 0:NG], in_=I["b_grp"][l:l + 1, 0:NG].partition_broadcast(128)), writes=[rbb])
        S.dma("sp", lambda e: e.dma_start(out=rb[:, NG:NR], in_=I["b_exp"][l:l + 1, 0:NE].partition_broadcast(128)), writes=[rbb])
        xt, xb = AF32.alloc((D,), "xtr")
        xn, xnb = AF32.alloc((D,), "xnr")
        sq, sqb = AF32.alloc((D,), "sqr")
        vT, vTb = AF32.alloc((16, 128), "vT")
        st, stb = AF32.alloc((4,), "str")
        lg, lgb = AF32.alloc((NR,), "lg")
        ge, geb = AF32.alloc((NG,), "ge")
        pen, penb = AF32.alloc((NG,), "pen")
        msk, mskb = AF32.alloc((NE,), "msk")
        msk2, msk2b = AF32.alloc((NE,), "msk2")
        ohs = [AF32.alloc((NE,), f"oh{i}") for i in range(2)]
        pos, posb = AF32.alloc((NE,), "pos")
        tmpe, tmpeb = AF32.alloc((NE,), "tmpe")
        s_, sb_ = AF32.alloc((16,), "rs")
        vbs = [ABF.alloc((D,), f"vb{i}") for i in range(2)]
        for tt in range(NT):
            r = 1 if is_ctx_tile(tt) else 0
            S.dma("sp", lambda e, tt=tt: e.dma_start(out=xt, in_=XS[tt * 128:(tt + 1) * 128, :]), reads=[dbufs["XS"]], writes=[xb])
            layernorm(xt, xb, xn, xnb, sq, sqb, st, stb)
            for k in range(16):
                if k % 4 == 0:
                    ps, pb = P(4)
                S.op("pe", lambda e, ps=ps, k=k: e.transpose(ps[:, (k % 4) * 128:(k % 4 + 1) * 128], xn[:, k * 128:(k + 1) * 128],
                                                             ident[:]), reads=[xnb, cb["ident"]], writes=[pb])
                S.op("act", lambda e, ps=ps, k=k, r=r: e.activation(
                    out=vT[:, k, :], in_=ps[:, (k % 4) * 128:(k % 4 + 1) * 128], func=AF.Identity,
                    scale=mod1[:, l, 64 + k, r:r + 1], bias=mod[:, l, 48 + k, r:r + 1]), reads=[pb, cb["mod"]], writes=[vTb])
            pr, prb = P(2, 4)
            for k in range(16):
                S.op("pe", lambda e, k=k, pr=pr: e.matmul(pr[:, 0:NR], lhsT=vT[:, k, :], rhs=wr[:, k, :], start=(k == 0), stop=(k == 15)),
                     reads=[vTb, wrb], writes=[prb])
            S.op("dve", lambda e, pr=pr: e.tensor_tensor(out=lg, in0=pr[:, 0:NR], in1=rb, op=ALU.add), reads=[prb, rbb], writes=[lgb])
            vb_, vbb = vbs[tt % 2]
            for k in range(16):
                if k % 4 == 0:
                    ps, pb = P(4)
                S.op("pe", lambda e, ps=ps, k=k: e.transpose(ps[:, (k % 4) * 128:(k % 4 + 1) * 128], vT[:, k, :], ident[:]),
                     reads=[vTb, cb["ident"]], writes=[pb])
                if k % 4 == 3:
                    kb = k // 4
                    if kb % 2 == 0:
                        S.op("act", lambda e, ps=ps, kb=kb, vb_=vb_: e.activation(out=vb_[:, kb * 512:(kb + 1) * 512], in_=ps[:, 0:512],
                                                                                 func=AF.Identity), reads=[pb], writes=[vbb])
                    else:
                        S.op("dve", lambda e, ps=ps, kb=kb, vb_=vb_: e.tensor_copy(out=vb_[:, kb * 512:(kb + 1) * 512], in_=ps[:, 0:512]),
                             reads=[pb], writes=[vbb])
            S.op("dve", lambda e: e.tensor_reduce(out=s_[:, 0:1], in_=lg[:, 0:NG], axis=AX.X, op=ALU.max), reads=[lgb], writes=[sb_])
            S.op("dve", lambda e: e.tensor_scalar(out=ge, in0=lg[:, 0:NG], scalar1=s_[:, 0:1], scalar2=None, op0=ALU.subtract),
                 reads=[lgb, sb_], writes=[geb])
            S.op("act", lambda e: e.activation(out=ge, in_=ge, func=AF.Exp), reads=[geb], writes=[geb])
            S.op("dve", lambda e: e.tensor_reduce(out=s_[:, 1:2], in_=ge, axis=AX.X, op=ALU.add), reads=[geb], writes=[sb_])
            S.op("dve", lambda e: e.reciprocal(out=s_[:, 2:3], in_=s_[:, 1:2]), reads=[sb_], writes=[sb_])
            S.op("dve", lambda e: e.tensor_scalar(out=pen, in0=lg[:, 0:NG], scalar1=s_[:, 0:1], scalar2=None, op0=ALU.is_equal),
                 reads=[lgb, sb_], writes=[penb])
            S.op("dve", lambda e: e.tensor_scalar(out=pen, in0=pen, scalar1=-1.0, scalar2=BIG, op0=ALU.add, op1=ALU.mult),
                 reads=[penb], writes=[penb])
            for g in range(NG):
                S.op("dve", lambda e, g=g: e.tensor_scalar(out=msk[:, g * EPG:(g + 1) * EPG], in0=lg[:, NG + g * EPG:NG + (g + 1) * EPG],
                                                           scalar1=pen[:, g:g + 1], scalar2=None, op0=ALU.add),
                     reads=[lgb, penb], writes=[mskb])
            oh1, oh1b = ohs[0]
            oh2, oh2b = ohs[1]
            S.op("dve", lambda e: e.tensor_reduce(out=s_[:, 3:4], in_=msk, axis=AX.X, op=ALU.max), reads=[mskb], writes=[sb_])
            S.op("dve", lambda e: e.tensor_scalar(out=oh1, in0=msk, scalar1=s_[:, 3:4], scalar2=None, op0=ALU.is_equal),
                 reads=[mskb, sb_], writes=[oh1b])
            S.op("dve", lambda e: e.scalar_tensor_tensor(out=msk2, in0=oh1, scalar=-BIG, in1=msk, op0=ALU.mult, op1=ALU.add),
                 reads=[oh1b, mskb], writes=[msk2b])
            S.op("dve", lambda e: e.tensor_reduce(out=s_[:, 4:5], in_=msk2, axis=AX.X, op=ALU.max), reads=[msk2b], writes=[sb_])
            S.op("dve", lambda e: e.tensor_scalar(out=oh2, in0=msk2, scalar1=s_[:, 4:5], scalar2=None, op0=ALU.is_equal),
                 reads=[msk2b, sb_], writes=[oh2b])
            S.op("dve", lambda e: e.tensor_tensor(out=s_[:, 5:6], in0=s_[:, 3:4], in1=s_[:, 4:5], op=ALU.subtract), reads=[sb_], writes=[sb_])
            S.op("act", lambda e: e.activation(out=s_[:, 6:7], in_=s_[:, 5:6], func=AF.Sigmoid), reads=[sb_], writes=[sb_])
            S.op("act", lambda e: e.activation(out=s_[:, 7:8], in_=s_[:, 5:6], func=AF.Sigmoid, scale=-1.0), reads=[sb_], writes=[sb_])
            S.op("dve", lambda e, tt=tt: e.tensor_scalar(out=gatew[:, tt, :], in0=s_[:, 6:8], scalar1=s_[:, 2:3], scalar2=None,
                                                         op0=ALU.mult), reads=[sb_], writes=[cb["gatew"]])
            for kk, (oh, ohb) in enumerate(ohs):
                pp, ppb = P(2, 6)
                S.op("pe", lambda e, pp=pp, oh=oh: e.matmul(pp[:, 0:NE], lhsT=tril[:], rhs=oh, start=True, stop=True),
                     reads=[ohb, cb["tril"]], writes=[ppb])
                S.op("dve", lambda e, pp=pp: e.tensor_tensor(out=pos, in0=pp[:, 0:NE], in1=ebase[:], op=ALU.add),
                     reads=[ppb, cb["ebase"]], writes=[posb])
                S.op("dve", lambda e, oh=oh: e.tensor_tensor(out=tmpe, in0=oh, in1=pos, op=ALU.mult), reads=[ohb, posb], writes=[tmpeb])
                S.op("dve", lambda e: e.tensor_reduce(out=s_[:, 8:9], in_=tmpe, axis=AX.X, op=ALU.add), reads=[tmpeb], writes=[sb_])
                S.op("dve", lambda e, oh=oh: e.tensor_tensor(out=tmpe, in0=oh, in1=eoff[:], op=ALU.mult), reads=[ohb, cb["eoff"]],
                     writes=[tmpeb])
                S.op("dve", lambda e: e.tensor_reduce(out=s_[:, 9:10], in_=tmpe, axis=AX.X, op=ALU.add), reads=[tmpeb], writes=[sb_])
                S.op("dve", lambda e: e.tensor_scalar(out=s_[:, 10:11], in0=s_[:, 8:9], scalar1=float(CAP) - 0.5, scalar2=1.0e9,
                                                      op0=ALU.is_ge, op1=ALU.mult), reads=[sb_], writes=[sb_])
                S.op("dve", lambda e: e.tensor_tensor(out=s_[:, 8:9], in0=s_[:, 8:9], in1=s_[:, 9:10], op=ALU.add), reads=[sb_], writes=[sb_])
                S.op("dve", lambda e: e.tensor_tensor(out=s_[:, 8:9], in0=s_[:, 8:9], in1=s_[:, 10:11], op=ALU.add), reads=[sb_], writes=[sb_])
                S.op("dve", lambda e, tt=tt, kk=kk: e.tensor_copy(out=dest_i[:, tt, kk:kk + 1], in_=s_[:, 8:9]), reads=[sb_],
                     writes=[cb["dest"]])
                pt, ptb = P(2, 6)
                S.op("pe", lambda e, pt=pt, oh=oh: e.matmul(pt[:, 0:NE], lhsT=ones_f[:], rhs=oh, start=True, stop=True),
                     reads=[ohb, cb["ones"]], writes=[ptb])
                S.op("dve", lambda e, pt=pt: e.tensor_tensor(out=ebase[:], in0=ebase[:], in1=pt[:, 0:NE], op=ALU.add),
                     reads=[ptb, cb["ebase"]], writes=[cb["ebase"]])
                S.dma("pool", lambda e, tt=tt, kk=kk, vb_=vb_: e.indirect_dma_start(
                    out=XSORT, out_offset=bass.IndirectOffsetOnAxis(ap=dest_i[:, tt, kk:kk + 1], axis=0), in_=vb_, in_offset=None,
                    bounds_check=regs["bc"], oob_is_err=False), reads=[vbb, cb["dest"]], writes=[dbufs["XSORT"]])

    def phase_experts(l):
        new_phase()
        wgus = [ABF.alloc((16, 1024), f"wgu{i}") for i in range(2)]
        wdns = [ABF.alloc((4, D), f"wdn{i}") for i in range(2)]
        xes = [ABF.alloc((D,), f"xe{i}") for i in range(2)]
        xeTs = [ABF.alloc((16, 128), f"xeT{i}") for i in range(2)]
        a_s = [ABF.alloc((512,), f"aact{i}") for i in range(2)]
        aTs = [ABF.alloc((4, 128), f"aT{i}") for i in range(2)]
        sgs = [AF32.alloc((512,), f"sgl{i}") for i in range(2)]
        ys = [AF32.alloc((D,), f"ye{i}") for i in range(2)]
        NST = CAP // 128
        tiles = [(ex, s) for ex in range(NE) for s in range(NST)]
        n = len(tiles)
        dctr = [0]

        def load_gu(ex):
            if ex < NE:
                load_w_chunk(wgus[ex % 2][0], wgus[ex % 2][1], I["w_gu"][l, ex])

        def load_dn(ex):
            if ex < NE:
                load_w_chunk(wdns[ex % 2][0], wdns[ex % 2][1], I["w_dn"][l, ex])

        def stA(i):
            ex, s = tiles[i]
            r0 = ex * CAP + s * 128
            xe, xeb = xes[i % 2]
            xeT, xeTb = xeTs[i % 2]
            S.dma("sp", lambda e: e.dma_start(out=xe, in_=XSORT[r0:r0 + 128, :]), reads=[dbufs["XSORT"]], writes=[xeb])
            for kb in range(4):
                ps, pb = psum[kb % 2], pbuf[kb % 2]
                for kk in range(4):
                    k = kb * 4 + kk
                    S.op("pe", lambda e, ps=ps, k=k, kk=kk: e.matmul(ps[:, kk * 128:(kk + 1) * 128], lhsT=xe[:, k * 128:(k + 1) * 128],
                                                                    rhs=identb[:], start=True, stop=True),
                         reads=[xeb, cb["identb"]], writes=[pb], sig=(kk == 3))
                dstv = xeT[:, kb * 4:(kb + 1) * 4, :].rearrange("p a b -> p (a b)")
                if kb % 2 == 0:
                    S.op("act", lambda e, ps=ps, dstv=dstv: e.activation(out=dstv, in_=ps[:, 0:512], func=AF.Identity),
                         reads=[pb], writes=[xeTb])
                else:
                    S.op("dve", lambda e, ps=ps, dstv=dstv: e.tensor_copy(out=dstv, in_=ps[:, 0:512]), reads=[pb], writes=[xeTb])

        def stB(i):
            ex, s = tiles[i]
            wgu, wgub = wgus[ex % 2]
            xeT, xeTb = xeTs[i % 2]
            sg, sgb = sgs[i % 2]
            a_, ab_ = a_s[i % 2]
            for (bk, c0) in ((2, 0), (3, 512)):
                for k in range(16):
                    S.op("pe", lambda e, bk=bk, k=k, c0=c0: e.matmul(psum[bk][:, 0:512], lhsT=xeT[:, k, :], rhs=wgu[:, k, c0:c0 + 512],
                                                                    start=(k == 0), stop=(k == 15)),
                         reads=[xeTb, wgub], writes=[pbuf[bk]], sig=(k == 15))
            S.op("act", lambda e: e.activation(out=sg, in_=psum[2][:, 0:512], func=AF.Silu), reads=[pbuf[2]], writes=[sgb])
            S.op("dve", lambda e: e.tensor_tensor(out=a_, in0=psum[3][:, 0:512], in1=sg, op=ALU.mult), reads=[pbuf[3], sgb], writes=[ab_])
            if s == NST - 1:
                load_gu(ex + 2)

        def stC(i):
            a_, ab_ = a_s[i % 2]
            aT, aTb = aTs[i % 2]
            for k in range(4):
                S.op("pe", lambda e, k=k: e.matmul(psum[4][:, k * 128:(k + 1) * 128], lhsT=a_[:, k * 128:(k + 1) * 128], rhs=identb[:],
                                                  start=True, stop=True), reads=[ab_, cb["identb"]], writes=[pbuf[4]], sig=(k == 3))
            S.op("dve", lambda e: e.tensor_copy(out=aT.rearrange("p a b -> p (a b)"), in_=psum[4][:, 0:512]), reads=[pbuf[4]], writes=[aTb])

        def stD(i):
            ex, s = tiles[i]
            r0 = ex * CAP + s * 128
            wdn, wdnb = wdns[ex % 2]
            aT, aTb = aTs[i % 2]
            y, yb = ys[i % 2]
            for nb in range(4):
                bk = 5 + dctr[0] % 3
                dctr[0] += 1
                for k in range(4):
                    S.op("pe", lambda e, bk=bk, k=k, nb=nb: e.matmul(psum[bk][:, 0:512], lhsT=aT[:, k, :],
                                                                    rhs=wdn[:, k, nb * 512:(nb + 1) * 512], start=(k == 0), stop=(k == 3)),
                         reads=[aTb, wdnb], writes=[pbuf[bk]], sig=(k == 3))
                if nb % 2 == 0:
                    S.op("act", lambda e, bk=bk, nb=nb: e.activation(out=y[:, nb * 512:(nb + 1) * 512], in_=psum[bk][:, 0:512],
                                                                    func=AF.Identity), reads=[pbuf[bk]], writes=[yb])
                else:
                    S.op("dve", lambda e, bk=bk, nb=nb: e.tensor_copy(out=y[:, nb * 512:(nb + 1) * 512], in_=psum[bk][:, 0:512]),
                         reads=[pbuf[bk]], writes=[yb])
            S.dma("sp", lambda e: e.dma_start(out=YSORT[r0:r0 + 128, :], in_=y), reads=[yb], writes=[dbufs["YSORT"]])
            if s == NST - 1:
                load_dn(ex + 2)

        for ex in range(2):
            load_gu(ex)
            load_dn(ex)
        for it in range(n + 3):
            if it < n:
                stA(it)
            if 0 <= it - 1 < n:
                stB(it - 1)
            if 0 <= it - 2 < n:
                stC(it - 2)
            if 0 <= it - 3 < n:
                stD(it - 3)

    def phase_combine(l, last):
        new_phase()
        gl, glb = AF32.alloc((D,), "g2lat")
        gc, gcb = AF32.alloc((D,), "g2ctx")
        lng, lngb = AF32.alloc((D,), "lng2")
        lnb, lnbb = AF32.alloc((D,), "lnb2")
        S.dma("sp", lambda e: e.dma_start(out=gl, in_=GROW[l, 1, 0:1, :].partition_broadcast(128)), reads=[dbufs["GROW"]], writes=[glb])
        S.dma("sp", lambda e: e.dma_start(out=gc, in_=GROW[l, 1, 1:2, :].partition_broadcast(128)), reads=[dbufs["GROW"]], writes=[gcb])
        S.dma("sp", lambda e: e.dma_start(out=lng, in_=I["ln2_g"][l:l + 1, :].partition_broadcast(128)), writes=[lngb])
        S.dma("sp", lambda e: e.dma_start(out=lnb, in_=I["ln2_b"][l:l + 1, :].partition_broadcast(128)), writes=[lnbb])
        A, Ab = AF32.alloc((D,), "yA")
        B, Bb = AF32.alloc((D,), "yB")
        xt, xb = AF32.alloc((D,), "xtc")
        st, stb = AF32.alloc((4,), "stc")
        for tt in range(NT):
            S.op("pool", lambda e: e.memset(A, 0.0), writes=[Ab])
            S.op("pool", lambda e: e.memset(B, 0.0), writes=[Bb])
            for kk, (dst, dstb) in enumerate(((A, Ab), (B, Bb))):
                S.dma("pool", lambda e, tt=tt, kk=kk, dst=dst: e.indirect_dma_start(
                    out=dst, out_offset=None, in_=YSORT, in_offset=bass.IndirectOffsetOnAxis(ap=dest_i[:, tt, kk:kk + 1], axis=0),
                    bounds_check=regs["bc"], oob_is_err=False), reads=[dbufs["YSORT"], cb["dest"]], writes=[dstb])
            S.dma("sp", lambda e, tt=tt: e.dma_start(out=xt, in_=XS[tt * 128:(tt + 1) * 128, :]), reads=[dbufs["XS"]], writes=[xb])
            S.op("dve", lambda e, tt=tt: e.tensor_scalar(out=A, in0=A, scalar1=gatew[:, tt, 0:1], scalar2=None, op0=ALU.mult),
                 reads=[Ab, cb["gatew"]], writes=[Ab])
            S.op("dve", lambda e, tt=tt: e.scalar_tensor_tensor(out=A, in0=B, scalar=gatew[:, tt, 1:2], in1=A, op0=ALU.mult, op1=ALU.add),
                 reads=[Ab, Bb, cb["gatew"]], writes=[Ab])
            g, gb_ = (gc, gcb) if is_ctx_tile(tt) else (gl, glb)
            src4 = [(A[:, nb * 512:(nb + 1) * 512], Ab) for nb in range(4)]
            deepnorm_tile(src4, xt, xb, B, Bb, A, Ab, st, stb, g, gb_, lng, lngb, lnb, lnbb)
            S.dma("sp", lambda e, tt=tt: e.dma_start(out=XS[tt * 128:(tt + 1) * 128, :], in_=xt), reads=[xb], writes=[dbufs["XS"]])
            if last and not is_ctx_tile(tt):
                o0 = (tt - NTC) * 128
                S.dma("sp", lambda e, o0=o0: e.dma_start(out=OUT[o0:o0 + 128, :], in_=xt), reads=[xb], writes=[dbufs["OUT"]])

    def dump(dst_ap, src_ap, srcname):
        new_phase()
        n = src_ap.shape[1]
        t_, tb_ = ABF.alloc((n,), "dumpst")
        nr = src_ap.shape[0]
        S.dma("sp", lambda e: e.dma_start(out=t_[0:nr], in_=src_ap), reads=[dbufs[srcname]], writes=[tb_])
        S.dma("pool", lambda e: e.dma_start(out=dst_ap, in_=t_[0:nr]), reads=[tb_], writes=[dbufs["DBG"]])

    S.dma("sp", lambda e: e.dma_start(out=XS[0:T_CTX, :], in_=I["ctx"]), writes=[dbufs["XS"]])
    S.dma("sp", lambda e: e.dma_start(out=XS[T_CTX:T, :], in_=I["x"]), writes=[dbufs["XS"]])
    done = False
    if stop == "const":
        S.dma("sp", lambda e: e.dma_start(out=DBG[0:128, 0:128], in_=tril[:]), reads=[cb["tril"]], writes=[dbufs["DBG"]])
        done = True
    else:
        phase_adaln()
    if stop == "adaln":
        S.dma("sp", lambda e: e.dma_start(out=DBG[0:128, 0:192], in_=mod[:, 0].rearrange("p j r -> p (j r)")),
              reads=[cb["mod"]], writes=[dbufs["DBG"]])
        S.dma("sp", lambda e: e.dma_start(out=DBG[128:256, 0:64], in_=smallv[:, 0]), reads=[cb["smallv"]], writes=[dbufs["DBG"]])
        S.dma("sp", lambda e: e.dma_start(out=DBG[256:384, 0:8], in_=lamt[:, 0]), reads=[cb["lamt"]], writes=[dbufs["DBG"]])
        S.dma("sp", lambda e: e.dma_start(out=DBG[384:388, 0:D], in_=GROW[0].rearrange("a b d -> (a b) d")),
              reads=[dbufs["GROW"]], writes=[dbufs["DBG"]])
        done = True
    for l in range(0 if done else NL):
        r = phase_inproj(l)
        if stop == "uT":
            uT_, uTb_ = r
            for k in range(16):
                S.dma("pool", lambda e, k=k: e.dma_start(out=DBG[k * 128:(k + 1) * 128, 0:T], in_=uT_[:, k, :]),
                      reads=[uTb_], writes=[dbufs["DBG"]])
            done = True
            break
        if stop == "inproj":
            o = 0
            for nm, ap, rows in (("KD", KD[0], 128), ("KR", KR2, 128), ("CKG", CKG[1], 128), ("QD", QD[3], 128),
                                 ("GT", GT[5], 128), ("OP", OP[2], 128)):
                if nm in cfg.get("dumps", ("KD", "KR", "CKG", "QD", "GT", "OP")):
                    dump(DBG[o:o + rows, 0:T], ap, nm)
                o += rows
            if "VD" in cfg.get("dumps", ("VD",)):
                dump(DBG[o:o + 128, 0:516], VD[1], "VD")
            done = True
            break
        phase_upproj(l, *r)
        phase_attn(l)
        if stop == "attn":
            o = 0
            for nm, ap in (("OM", OM[0]), ("OM", OM[7]), ("OD", OD[0]), ("OD", OD[3]), ("KN", KN[2]), ("QN", QN[5]), ("QR", QR[3])):
                rows = ap.shape[0]
                dump(DBG[o:o + rows, 0:T], ap, nm)
                o += 128
            dump(DBG[o:o + 128, 0:1032], VM[1], "VM")
            done = True
            break
        phase_merge(l)
        phase_wo(l)
        if stop == "mixer":
            S.dma("sp", lambda e: e.dma_start(out=DBG[0:T, 0:D], in_=XS), reads=[dbufs["XS"]], writes=[dbufs["DBG"]])
            done = True
            break
        phase_route(l)
        if stop == "route":
            S.dma("sp", lambda e: e.dma_start(out=DBG[0:128, 0:NT * 2], in_=gatew[:].rearrange("p a b -> p (a b)")),
                  reads=[cb["gatew"]], writes=[dbufs["DBG"]])
            df, dfb = AF32.alloc((NT * 2,), "destf")
            S.op("dve", lambda e: e.tensor_copy(out=df, in_=dest_i[:].rearrange("p a b -> p (a b)")), reads=[cb["dest"]], writes=[dfb])
            S.dma("sp", lambda e: e.dma_start(out=DBG[128:256, 0:NT * 2], in_=df), reads=[dfb], writes=[dbufs["DBG"]])
            done = True
            break
        phase_experts(l)
        phase_combine(l, l == NL - 1)
        if stop == "layer" and l == 0:
            S.dma("sp", lambda e: e.dma_start(out=DBG[0:T, 0:D], in_=XS), reads=[dbufs["XS"]], writes=[dbufs["DBG"]])
            done = True
            break
    S.finish(list(dbufs.values()))
    barrier()
    S.emit()
    return nc, es, S


def host_consts(T_CTX, T_LAT, NE, CAP):
    T = T_CTX + T_LAT
    quarter = ROPE // 4
    inv_freq = (10000.0 ** (-np.arange(quarter, dtype=np.float32) / quarter)).astype(np.float32)
    rows = T_LAT // GRID_W
    row = np.repeat(np.arange(rows, dtype=np.float32), GRID_W)
    col = (np.arange(rows * GRID_W) % GRID_W).astype(np.float32)
    ang = np.stack([row[:, None] * inv_freq, col[:, None] * inv_freq], axis=1)
    cos, sin = np.cos(ang).astype(np.float32), np.sin(ang).astype(np.float32)
    c64 = np.ones((T, 2, 2, quarter), np.float32)
    s64 = np.zeros((T, 2, 2, quarter), np.float32)
    c64[T_CTX:] = cos[:, :, None, :]
    s64[T_CTX:] = sin[:, :, None, :]
    c64 = c64.reshape(T, 64).T
    s64 = s64.reshape(T, 64).T
    ropc = np.ascontiguousarray(np.concatenate([c64, c64], axis=0))
    rops = np.ascontiguousarray(np.concatenate([s64, s64], axis=0))
    rcnt = np.zeros((4, T), np.float32)
    for gi, w in enumerate(POOL_WINDOWS):
        half = w // 2
        for (s0, n) in ((0, T_CTX), (T_CTX, T_LAT)):
            t = np.arange(n)
            cnt = np.minimum(t + half, n) - np.maximum(t - half, 0)
            rcnt[gi, s0:s0 + n] = 1.0 / cnt.astype(np.float32)
    tril = (np.arange(128)[:, None] < np.arange(128)[None, :]).astype(np.float32)
    eoff = np.broadcast_to((np.arange(NE, dtype=np.float32) * CAP)[None, :], (128, NE)).copy()
    return dict(ident=np.eye(128, dtype=np.float32), ropc=ropc, rops=rops, rcnt=rcnt, tril=tril, eoff=eoff)


N_ACTIVE = 4
MOE_CAP = 384


def _per_core_inputs(b, x, c, ctx, c_ctx, weights, consts):
    m = dict(x=np.ascontiguousarray(x[b]), ctx=np.ascontiguousarray(ctx[b]),
             cvec=np.ascontiguousarray(np.stack([c[b], c_ctx])))
    m.update(weights)
    m.update(consts)
    return m


def kernel(x, c, ctx, c_ctx, w_ada, b_ada, w_in, b_gate, g_q, g_kv, w_uq, w_ukv, lam, g_sub, w_pool, pool_scale,
           w_br_mla, w_br_diff, w_br_pool, w_o, ln1_g, ln1_b, w_grp, b_grp, w_exp, b_exp, w_gu, w_dn, ln2_g, ln2_b):
    B, T_LAT, _ = x.shape
    T_CTX = ctx.shape[1]
    weights = dict(w_ada=w_ada, b_ada=b_ada, w_in=w_in, b_gate=np.reshape(b_gate, (DEPTH, 3 * D)), g_q=g_q, g_kv=g_kv,
                   w_uq=w_uq, w_ukv=w_ukv, lam=np.reshape(lam, (DEPTH, 4 * DIFF_HD)), g_sub=g_sub, w_pool=w_pool,
                   pool_scale=pool_scale, w_br_mla=w_br_mla, w_br_diff=w_br_diff, w_br_pool=w_br_pool, w_o=w_o,
                   ln1_g=ln1_g, ln1_b=ln1_b, w_grp=w_grp, b_grp=b_grp, w_exp=w_exp, b_exp=b_exp, w_gu=w_gu, w_dn=w_dn,
                   ln2_g=ln2_g, ln2_b=ln2_b)
    weights = {k: np.ascontiguousarray(np.asarray(v, np.float32)) for k, v in weights.items()}
    cfg = dict(T_CTX=T_CTX, T_LAT=T_LAT, NL=DEPTH, NG=N_GROUPS, CAP=MOE_CAP, stop="end")
    nc, es, S = build_program(cfg)
    consts = host_consts(T_CTX, T_LAT, N_EXPERTS, MOE_CAP)
    in_maps = [_per_core_inputs(b, x, c, ctx, c_ctx, weights, consts) for b in range(B)]
    res = run_bass_kernel_spmd(nc, in_maps, core_ids=list(range(N_ACTIVE)))
    return np.stack([res.results[b]["out"] for b in range(B)], axis=0).astype(np.float32)
```
